# Optimizing a Trainium2 kernel written in Bass

```python
import math
import jax, jax.numpy as jnp
from jax import lax
import numpy as np

D_MODEL = 1024
BATCH = 4
SEQ = 4096
DEPTH = 2

CTX_LEN = 256
GRID_W = 64
HEAD_DIM = 64
MIX_HEADS = D_MODEL // HEAD_DIM
BLOCK = 128
WINDOW = 128
ROPE_THETA = 10000.0
LN_EPS = 1e-6
RMS_EPS = 1e-6
NEG_INF = -1e30
N_MOD = 6

A_GROUPS = MIX_HEADS // 4
A_W = A_GROUPS * HEAD_DIM
B_Q_HEADS = MIX_HEADS - A_GROUPS
B_KV_HEADS = B_Q_HEADS // 3
B_GROUP = B_Q_HEADS // B_KV_HEADS
AB_Q_W = A_W + B_Q_HEADS * HEAD_DIM
AB_KV_W = 2 * B_KV_HEADS * HEAD_DIM
AB_IN_W = AB_Q_W + AB_KV_W
AB_OUT_W = A_W + B_Q_HEADS * HEAD_DIM

C_HEADS = MIX_HEADS // 4
D_Q_HEADS = MIX_HEADS // 2
D_KV_HEADS = D_Q_HEADS // 4
D_GROUP = D_Q_HEADS // D_KV_HEADS
C_QK_W = C_HEADS * 2 * HEAD_DIM
D_Q_W = D_Q_HEADS * HEAD_DIM
D_KV_W = D_KV_HEADS * HEAD_DIM
C_V_W = C_HEADS * 2 * HEAD_DIM
CD_Q_W = C_QK_W + D_Q_W
CD_KV_W = C_QK_W + D_KV_W + C_V_W + D_KV_W
CD_IN_W = CD_Q_W + CD_KV_W
CD_OUT_W = C_V_W + D_Q_W

N_EXPERTS = 32
TOP_K = 4
EXPERT_FF = D_MODEL
SWIGLU_LIMIT = 7.0
SWIGLU_ALPHA = 1.702
MOE_BLOCK = 128

DEEPNORM_ALPHA = (2 * DEPTH) ** 0.25
DEEPNORM_BETA = (8 * DEPTH) ** -0.25
N_EVEN = (DEPTH + 1) // 2
N_ODD = DEPTH // 2

kernel_name = 'hybrid_fourier_window_diff_axial_moe_dit'


def _layernorm(x, g, b):
    xf = x.astype(jnp.float32)
    mu = jnp.mean(xf, axis=-1, keepdims=True)
    var = jnp.mean(jnp.square(xf - mu), axis=-1, keepdims=True)
    return ((xf - mu) * lax.rsqrt(var + LN_EPS) * g + b).astype(x.dtype)


def _rmsnorm(x, g):
    xf = x.astype(jnp.float32)
    y = xf * lax.rsqrt(jnp.mean(jnp.square(xf), axis=-1, keepdims=True) + RMS_EPS)
    return (y * g).astype(x.dtype)


def _post_norm(x, y, gate, g, b):
    return _layernorm(DEEPNORM_ALPHA * x + gate * y, g, b)


def _modulate(x, shift, scale):
    return x * (1 + scale) + shift


def _axial_rope_tables(rows, d):
    t = jnp.arange(rows * GRID_W)
    row = (t // GRID_W).astype(jnp.float32)
    col = (t % GRID_W).astype(jnp.float32)
    nf = d // 4
    inv = ROPE_THETA ** (-jnp.arange(nf, dtype=jnp.float32) / nf)
    ar = row[:, None] * inv[None, :]
    ac = col[:, None] * inv[None, :]
    ang = jnp.concatenate([ar, ar, ac, ac], axis=-1)
    return jnp.cos(ang), jnp.sin(ang)


def _apply_rope(x, cos, sin):
    d = x.shape[-1]
    nf = d // 4
    shp = (cos.shape[0],) + (1,) * (x.ndim - 3) + (d,)
    cs, sn = cos.reshape(shp), sin.reshape(shp)
    xf = x.astype(jnp.float32)
    rot = jnp.concatenate([-xf[..., nf:2 * nf], xf[..., :nf], -xf[..., 3 * nf:], xf[..., 2 * nf:3 * nf]], axis=-1)
    return (xf * cs + rot * sn).astype(x.dtype)


def _gqa_scores(q, k):
    return jnp.einsum('bqhgd,bkhd->bhgqk', q, k).astype(jnp.float32) * (q.shape[-1] ** -0.5)


def _gqa_apply(p, v):
    return jnp.einsum('bhgqk,bkhd->bqhgd', p.astype(v.dtype), v)


def _gqa_attention(q, k, v):
    return _gqa_apply(jax.nn.softmax(_gqa_scores(q, k), axis=-1), v)


def _sink_attention(q, k, v, sink):
    s = _gqa_scores(q, k)
    s_sink = jnp.broadcast_to(sink.reshape(B_KV_HEADS, B_GROUP).astype(jnp.float32)[None, :, :, None, None], s.shape[:-1] + (1,))
    p = jax.nn.softmax(jnp.concatenate([s, s_sink], axis=-1), axis=-1)
    return _gqa_apply(p[..., :-1], v)


def _window_attention(q, k, v, kc, vc, sink):
    B, S, Hkv, G, d = q.shape
    n_ctx = kc.shape[1]
    pad = ((0, 0), (BLOCK, BLOCK), (0, 0), (0, 0))
    kp, vp = jnp.pad(k, pad), jnp.pad(v, pad)
    sink_hg = sink.reshape(Hkv, G).astype(jnp.float32)[None, :, :, None, None]
    offs_q = jnp.arange(BLOCK)
    offs_k = jnp.arange(3 * BLOCK) - BLOCK

    def block(n):
        q0 = n * BLOCK
        qb = lax.dynamic_slice_in_dim(q, q0, BLOCK, axis=1)
        kb = lax.dynamic_slice_in_dim(kp, q0, 3 * BLOCK, axis=1)
        vb = lax.dynamic_slice_in_dim(vp, q0, 3 * BLOCK, axis=1)
        qpos = q0 + offs_q
        kpos = q0 + offs_k
        allowed = (jnp.abs(qpos[:, None] - kpos[None, :]) <= WINDOW) & (kpos >= 0)[None, :] & (kpos < S)[None, :]
        s_loc = jnp.where(allowed, _gqa_scores(qb, kb), NEG_INF)
        s_ctx = _gqa_scores(qb, kc)
        s_sink = jnp.broadcast_to(sink_hg, s_ctx.shape[:-1] + (1,))
        p = jax.nn.softmax(jnp.concatenate([s_loc, s_ctx, s_sink], axis=-1), axis=-1)
        return _gqa_apply(p[..., :3 * BLOCK], vb) + _gqa_apply(p[..., 3 * BLOCK:3 * BLOCK + n_ctx], vc)

    o = lax.map(block, jnp.arange(S // BLOCK))
    return jnp.moveaxis(o, 0, 1).reshape(B, S, Hkv * G * d)


def _fourier_mix(a):
    B, T, _ = a.shape
    g = a.reshape(B, T, A_GROUPS, HEAD_DIM).astype(jnp.float32)
    f = jnp.fft.fft2(g, axes=(1, 3), norm='ortho').real
    return f.reshape(B, T, A_W).astype(a.dtype)


def _ab_mixer(h_lat, h_ctx, w_in, sink, w_out, cos, sin, need_ctx_out):
    B, S, _ = h_lat.shape
    n_ctx = h_ctx.shape[1]
    p_lat = h_lat @ w_in
    kv_ctx = h_ctx @ w_in[:, AB_Q_W:]

    def kv_split(kv, T):
        k = kv[..., :AB_KV_W // 2].reshape(B, T, B_KV_HEADS, HEAD_DIM)
        v = kv[..., AB_KV_W // 2:].reshape(B, T, B_KV_HEADS, HEAD_DIM)
        return k, v

    k_lat, v_lat = kv_split(p_lat[..., AB_Q_W:], S)
    k_ctx, v_ctx = kv_split(kv_ctx, n_ctx)
    k_lat = _apply_rope(k_lat, cos, sin)
    q_lat = _apply_rope(p_lat[..., A_W:AB_Q_W].reshape(B, S, B_KV_HEADS, B_GROUP, HEAD_DIM), cos, sin)
    o_lat = jnp.concatenate([_fourier_mix(p_lat[..., :A_W]),
                             _window_attention(q_lat, k_lat, v_lat, k_ctx, v_ctx, sink)], axis=-1)
    y_lat = o_lat @ w_out
    y_ctx = None
    if need_ctx_out:
        qs_ctx = h_ctx @ w_in[:, :AB_Q_W]
        q_ctx = qs_ctx[..., A_W:].reshape(B, n_ctx, B_KV_HEADS, B_GROUP, HEAD_DIM)
        o_ctx = jnp.concatenate([_fourier_mix(qs_ctx[..., :A_W]),
                                 _sink_attention(q_ctx, k_ctx, v_ctx, sink).reshape(B, n_ctx, B_Q_HEADS * HEAD_DIM)], axis=-1)
        y_ctx = o_ctx @ w_out
    return y_lat, y_ctx


def _split_cd_q(qs):
    B, T, _ = qs.shape
    q_c = qs[..., :C_QK_W].reshape(B, T, C_HEADS, 2, HEAD_DIM)
    q_d = qs[..., C_QK_W:].reshape(B, T, D_KV_HEADS, D_GROUP, HEAD_DIM)
    return q_c, q_d


def _split_cd_kv(kv):
    B, T, _ = kv.shape
    o1 = C_QK_W
    o2 = o1 + D_KV_W
    o3 = o2 + C_V_W
    k_c = kv[..., :o1].reshape(B, T, C_HEADS, 2, HEAD_DIM)
    k_d = kv[..., o1:o2].reshape(B, T, D_KV_HEADS, HEAD_DIM)
    v_c = kv[..., o2:o3].reshape(B, T, C_HEADS, 2 * HEAD_DIM)
    v_d = kv[..., o3:].reshape(B, T, D_KV_HEADS, HEAD_DIM)
    return k_c, k_d, v_c, v_d


def _diff_attention(q, k, v, lam, subln_g, lambda_init):
    s = jnp.einsum('bqhmd,bkhmd->bhmqk', q, k).astype(jnp.float32) * (HEAD_DIM ** -0.5)
    p = jax.nn.softmax(s, axis=-1)
    w = p[:, :, 0] - lam * p[:, :, 1]
    o = jnp.einsum('bhqk,bkhd->bqhd', w.astype(v.dtype), v)
    return _rmsnorm(o, subln_g) * (1.0 - lambda_init)


def _lambda_init(layer):
    return 0.8 - 0.6 * math.exp(-0.3 * layer)


def _cd_mixer(h_lat, h_ctx, w_in, lam_vec, subln_g, q_norm_g, k_norm_g, w_out, lambda_init, cos, sin, need_ctx_out):
    B, S, _ = h_lat.shape
    n_ctx = h_ctx.shape[1]
    lv = lam_vec.astype(jnp.float32)
    lam = jnp.exp(jnp.sum(lv[0] * lv[1])) - jnp.exp(jnp.sum(lv[2] * lv[3])) + lambda_init
    p_lat = h_lat @ w_in
    q_c, q_d = _split_cd_q(p_lat[..., :CD_Q_W])
    k_c, k_d, v_c, v_d = _split_cd_kv(p_lat[..., CD_Q_W:])
    k_c_ctx, k_d_ctx, v_c_ctx, v_d_ctx = _split_cd_kv(h_ctx @ w_in[:, CD_Q_W:])
    q_c = _apply_rope(q_c, cos, sin)
    k_c = _apply_rope(k_c, cos, sin)
    q_d = _apply_rope(_rmsnorm(q_d, q_norm_g), cos, sin)
    k_d = _apply_rope(_rmsnorm(k_d, k_norm_g), cos, sin)
    k_d_ctx = _rmsnorm(k_d_ctx, k_norm_g)
    k_c_all = jnp.concatenate([k_c_ctx, k_c], axis=1)
    v_c_all = jnp.concatenate([v_c_ctx, v_c], axis=1)
    k_d_all = jnp.concatenate([k_d_ctx, k_d], axis=1)
    v_d_all = jnp.concatenate([v_d_ctx, v_d], axis=1)

    def block(n):
        q0 = n * BLOCK
        qcb = lax.dynamic_slice_in_dim(q_c, q0, BLOCK, axis=1)
        qdb = lax.dynamic_slice_in_dim(q_d, q0, BLOCK, axis=1)
        oc = _diff_attention(qcb, k_c_all, v_c_all, lam, subln_g, lambda_init)
        od = _gqa_attention(qdb, k_d_all, v_d_all)
        return jnp.concatenate([oc.reshape(B, BLOCK, C_V_W), od.reshape(B, BLOCK, D_Q_W)], axis=-1)

    o = lax.map(block, jnp.arange(S // BLOCK))
    y_lat = jnp.moveaxis(o, 0, 1).reshape(B, S, CD_OUT_W) @ w_out
    y_ctx = None
    if need_ctx_out:
        q_c_ctx, q_d_ctx = _split_cd_q(h_ctx @ w_in[:, :CD_Q_W])
        q_d_ctx = _rmsnorm(q_d_ctx, q_norm_g)
        o_ctx = jnp.concatenate([
            _diff_attention(q_c_ctx, k_c_ctx, v_c_ctx, lam, subln_g, lambda_init).reshape(B, n_ctx, C_V_W),
            _gqa_attention(q_d_ctx, k_d_ctx, v_d_ctx).reshape(B, n_ctx, D_Q_W)], axis=-1)
        y_ctx = o_ctx @ w_out
    return y_lat, y_ctx


def _moe(h, router_w, router_b, w_gu, b_gu, w_down, b_down):
    n_tok, d = h.shape
    logits = (h @ router_w + router_b).astype(jnp.float32)
    top_logit, top_e = lax.top_k(logits, TOP_K)
    gates = jax.nn.softmax(top_logit, axis=-1)
    n_asg = n_tok * TOP_K
    flat_e = top_e.reshape(n_asg)
    order = jnp.argsort(flat_e)
    sorted_e = flat_e[order]
    counts = jnp.bincount(flat_e, length=N_EXPERTS)
    padded = (counts + MOE_BLOCK - 1) // MOE_BLOCK * MOE_BLOCK
    start = jnp.cumsum(counts) - counts
    pend = jnp.cumsum(padded)
    pstart = pend - padded
    dest = (pstart[sorted_e] + jnp.arange(n_asg) - start[sorted_e]).astype(jnp.int32)
    n_blocks = -(-n_asg // MOE_BLOCK) + N_EXPERTS
    buf_tok = jnp.full((n_blocks * MOE_BLOCK,), n_tok, jnp.int32).at[dest].set((order // TOP_K).astype(jnp.int32))
    block_e = jnp.minimum(jnp.searchsorted(pend, jnp.arange(n_blocks) * MOE_BLOCK, side='right'), N_EXPERTS - 1)
    xb = jnp.concatenate([h, jnp.zeros((1, d), h.dtype)], axis=0)[buf_tok].reshape(n_blocks, MOE_BLOCK, d)

    def expert_block(args):
        xblk, e = args
        gu = xblk @ w_gu[e] + b_gu[e]
        g = jnp.minimum(gu[..., :EXPERT_FF], SWIGLU_LIMIT)
        u = jnp.clip(gu[..., EXPERT_FF:], -SWIGLU_LIMIT, SWIGLU_LIMIT)
        return ((u + 1.0) * (g * jax.nn.sigmoid(SWIGLU_ALPHA * g))) @ w_down[e] + b_down[e]

    yb = lax.map(expert_block, (xb, block_e)).reshape(n_blocks * MOE_BLOCK, d)
    slot = jnp.zeros((n_asg,), jnp.int32).at[order].set(dest)
    y = yb[slot].reshape(n_tok, TOP_K, d)
    return jnp.einsum('nk,nkd->nd', gates.astype(y.dtype), y)


def setup_inputs(seed: int = 0) -> dict:
    key = jax.random.key(seed)
    ks = jax.random.split(key, 23)
    f32 = jnp.float32
    D = D_MODEL

    def nrm(k, shape, scale):
        return jax.random.normal(k, shape, f32) * scale

    return {
        'x': nrm(ks[0], (BATCH, SEQ, D), 1.0),
        'c': nrm(ks[1], (BATCH, D), 1.0),
        'ctx': nrm(ks[2], (BATCH, CTX_LEN, D), 1.0),
        'c_ctx': nrm(ks[3], (D,), 1.0),
        'mod_w': nrm(ks[4], (DEPTH, D, N_MOD * D), 0.5 * D ** -0.5),
        'mod_b': nrm(ks[5], (DEPTH, N_MOD * D), 0.02),
        'ln_g': 1.0 + nrm(ks[6], (DEPTH, 2, D), 0.02),
        'ln_b': nrm(ks[7], (DEPTH, 2, D), 0.02),
        'ab_w_in': nrm(ks[8], (N_EVEN, D, AB_IN_W), D ** -0.5),
        'ab_sink': nrm(ks[9], (N_EVEN, B_Q_HEADS), 0.5),
        'ab_w_out': nrm(ks[10], (N_EVEN, AB_OUT_W, D), DEEPNORM_BETA * AB_OUT_W ** -0.5),
        'cd_w_in': nrm(ks[11], (N_ODD, D, CD_IN_W), D ** -0.5),
        'cd_lambda': nrm(ks[12], (N_ODD, 4, HEAD_DIM), 0.1),
        'cd_subln_g': 1.0 + nrm(ks[13], (N_ODD, 2 * HEAD_DIM), 0.02),
        'cd_q_norm_g': 1.0 + nrm(ks[14], (N_ODD, HEAD_DIM), 0.02),
        'cd_k_norm_g': 1.0 + nrm(ks[15], (N_ODD, HEAD_DIM), 0.02),
        'cd_w_out': nrm(ks[16], (N_ODD, CD_OUT_W, D), DEEPNORM_BETA * CD_OUT_W ** -0.5),
        'router_w': nrm(ks[17], (DEPTH, D, N_EXPERTS), D ** -0.5),
        'router_b': nrm(ks[18], (DEPTH, N_EXPERTS), 0.01),
        'expert_w_gu': nrm(ks[19], (DEPTH, N_EXPERTS, D, 2 * EXPERT_FF), D ** -0.5),
        'expert_b_gu': nrm(ks[20], (DEPTH, N_EXPERTS, 2 * EXPERT_FF), 0.02),
        'expert_w_down': nrm(ks[21], (DEPTH, N_EXPERTS, EXPERT_FF, D), DEEPNORM_BETA * EXPERT_FF ** -0.5),
        'expert_b_down': nrm(ks[22], (DEPTH, N_EXPERTS, D), 0.02),
    }


def reference(x, c, ctx, c_ctx, mod_w, mod_b, ln_g, ln_b, ab_w_in, ab_sink, ab_w_out,
              cd_w_in, cd_lambda, cd_subln_g, cd_q_norm_g, cd_k_norm_g, cd_w_out,
              router_w, router_b, expert_w_gu, expert_b_gu, expert_w_down, expert_b_down):
    B, S, D = x.shape
    n_ctx = ctx.shape[1]
    ROWS = S // GRID_W
    cos, sin = _axial_rope_tables(ROWS, HEAD_DIM)
    silu_c = jax.nn.silu(c)
    silu_c_ctx = jax.nn.silu(c_ctx)
    x_lat, x_ctx = x, ctx
    for l in range(DEPTH):
        last = l == DEPTH - 1
        m_lat = (silu_c @ mod_w[l] + mod_b[l])[:, None, :]
        m_ctx = (silu_c_ctx @ mod_w[l] + mod_b[l])[None, None, :]
        sh1, sc1, g1, sh2, sc2, g2 = jnp.split(m_lat, N_MOD, axis=-1)
        csh1, csc1, cg1, csh2, csc2, cg2 = jnp.split(m_ctx, N_MOD, axis=-1)
        h_lat = _modulate(x_lat, sh1, sc1)
        h_ctx = _modulate(x_ctx, csh1, csc1)
        i = l // 2
        if l % 2 == 0:
            y_lat, y_ctx = _ab_mixer(h_lat, h_ctx, ab_w_in[i], ab_sink[i], ab_w_out[i], cos, sin, not last)
        else:
            y_lat, y_ctx = _cd_mixer(h_lat, h_ctx, cd_w_in[i], cd_lambda[i], cd_subln_g[i], cd_q_norm_g[i],
                                     cd_k_norm_g[i], cd_w_out[i], _lambda_init(l), cos, sin, not last)
        x_lat = _post_norm(x_lat, y_lat, g1, ln_g[l, 0], ln_b[l, 0])
        h_lat = _modulate(x_lat, sh2, sc2)
        if not last:
            x_ctx = _post_norm(x_ctx, y_ctx, cg1, ln_g[l, 0], ln_b[l, 0])
            h_ctx = _modulate(x_ctx, csh2, csc2)
            h_all = jnp.concatenate([h_ctx.reshape(B * n_ctx, D), h_lat.reshape(B * S, D)], axis=0)
            f = _moe(h_all, router_w[l], router_b[l], expert_w_gu[l], expert_b_gu[l], expert_w_down[l], expert_b_down[l])
            x_ctx = _post_norm(x_ctx, f[:B * n_ctx].reshape(B, n_ctx, D), cg2, ln_g[l, 1], ln_b[l, 1])
            f_lat = f[B * n_ctx:].reshape(B, S, D)
        else:
            f_lat = _moe(h_lat.reshape(B * S, D), router_w[l], router_b[l], expert_w_gu[l], expert_b_gu[l],
                         expert_w_down[l], expert_b_down[l]).reshape(B, S, D)
        x_lat = _post_norm(x_lat, f_lat, g2, ln_g[l, 1], ln_b[l, 1])
    return x_lat
```

```python
import numpy as np
import ml_dtypes
from contextlib import ExitStack
import concourse.bass as bass
import concourse.mybir as mybir
from concourse.bass_utils import run_bass_kernel_spmd

F32 = mybir.dt.float32
BF16 = mybir.dt.bfloat16
ALU = mybir.AluOpType
ACTF = mybir.ActivationFunctionType
AX = mybir.AxisListType

ENGS = ["sp", "act", "dve", "pool", "pe"]
DQ = ("sp", "act", "pool")
NDSEM = 8


class T:
    __slots__ = ("ap", "w", "r", "name")

    def __init__(self, ap, name=""):
        self.ap = ap
        self.w = None
        self.r = []
        self.name = name


def _ta(x):
    if isinstance(x, T):
        return x, x.ap
    if isinstance(x, tuple):
        return x
    return None, x


def _key(ev):
    return ev[:2] if ev[0] == "e" else ev[:3]


class Sched:
    def __init__(self, nc, stack):
        self.nc = nc
        self.q = {e: [] for e in ENGS}
        self.cnt = {e: 0 for e in ENGS}
        self.sem = {e: stack.enter_context(nc.semaphore(f"s_{e}")) for e in ENGS}
        self.dsem = {e: [stack.enter_context(nc.semaphore(f"d_{e}{k}")) for k in range(NDSEM)] for e in DQ}
        self.dcnt = {e: [0] * NDSEM for e in DQ}
        self.dnext = {e: 0 for e in DQ}
        self.seen = {e: {} for e in ENGS}
        self.ninstr = 0

    def _wait(self, eng, ev):
        if ev[0] == "e":
            _, f, n = ev
            if f == eng and eng == "pe":
                return
            key, val, sem = ("e", f), n, self.sem[f]
        else:
            _, f, k, n = ev
            key, val, sem = ("d", f, k), 16 * n, self.dsem[f][k]
        if self.seen[eng].get(key, 0) >= val:
            return
        self.seen[eng][key] = val
        self.q[eng].append(lambda E, sem=sem, val=val: E.wait_ge(sem, val))
        self.ninstr += 1

    def _deps(self, eng, reads, writes):
        best = {}
        for t in reads:
            if t.w is not None:
                k = _key(t.w)
                if k not in best or best[k][-1] < t.w[-1]:
                    best[k] = t.w
        for t in writes:
            for ev in ([t.w] if t.w is not None else []) + t.r:
                k = _key(ev)
                if k not in best or best[k][-1] < ev[-1]:
                    best[k] = ev
        for ev in best.values():
            self._wait(eng, ev)

    def _commit(self, ev, reads, writes):
        for t in reads:
            t.r.append(ev)
            if len(t.r) > 16:
                best = {}
                for e2 in t.r:
                    k = _key(e2)
                    if k not in best or best[k][-1] < e2[-1]:
                        best[k] = e2
                t.r = list(best.values())
        for t in writes:
            t.w = ev
            t.r = []

    def op(self, eng, fns, reads=(), writes=()):
        if callable(fns):
            fns = [fns]
        reads = [t for t in reads if t is not None]
        writes = [t for t in writes if t is not None]
        self._deps(eng, reads, writes)
        self.cnt[eng] += 1
        n = self.cnt[eng]
        sem = self.sem[eng]
        last = len(fns) - 1
        for i, fn in enumerate(fns):
            if i == last:
                self.q[eng].append(lambda E, fn=fn, sem=sem: fn(E).then_inc(sem, 1))
            else:
                self.q[eng].append(lambda E, fn=fn: fn(E))
            self.ninstr += 1
        ev = ("e", eng, n)
        self._commit(ev, reads, writes)
        return ev

    def dma(self, eng, out, in_):
        ot, oa = _ta(out)
        it, ia = _ta(in_)
        k = self.dnext[eng]
        self.dnext[eng] = (k + 1) % NDSEM
        if self.dcnt[eng][k] > 0:
            self._wait(eng, ("d", eng, k, self.dcnt[eng][k]))
        reads = [t for t in [it] if t is not None]
        writes = [t for t in [ot] if t is not None]
        self._deps(eng, reads, writes)
        self.dcnt[eng][k] += 1
        n = self.dcnt[eng][k]
        sem = self.dsem[eng][k]
        self.q[eng].append(lambda E, oa=oa, ia=ia, sem=sem: E.dma_start(out=oa, in_=ia).then_inc(sem, 16))
        self.ninstr += 1
        ev = ("d", eng, k, n)
        self._commit(ev, reads, writes)
        return ev

    def _all_events(self):
        evs = [("e", f, self.cnt[f]) for f in ENGS if self.cnt[f] > 0]
        for f in DQ:
            for k in range(NDSEM):
                if self.dcnt[f][k] > 0:
                    evs.append(("d", f, k, self.dcnt[f][k]))
        return evs

    def barrier(self):
        evs = self._all_events()
        for e in ENGS:
            for ev in evs:
                self._wait(e, ev)

    def wait_all_on(self, eng):
        for ev in self._all_events():
            self._wait(eng, ev)

    def mm(self, out, pairs, start=True, stop=True, extra_reads=()):
        ot, oa = _ta(out)
        reads = list(extra_reads)
        fns = []
        n = len(pairs)
        for i, (l, r) in enumerate(pairs):
            lt, la = _ta(l)
            rt, ra = _ta(r)
            reads += [lt, rt]
            fns.append(lambda E, la=la, ra=ra, st=(start and i == 0), sp=(stop and i == n - 1):
                       E.matmul(oa, la, ra, start=st, stop=sp))
        return self.op("pe", fns, reads, [ot])

    def transpose(self, out, in_, ident):
        ot, oa = _ta(out)
        it, ia = _ta(in_)
        dt, da = _ta(ident)
        return self.op("pe", lambda E: E.transpose(oa, ia, da), [it, dt], [ot])

    def act(self, out, in_, func, bias=0.0, scale=1.0, accum_out=None, eng="act"):
        ot, oa = _ta(out)
        it, ia = _ta(in_)
        bt, ba = _ta(bias)
        st_, sa = _ta(scale)
        at, aa = _ta(accum_out) if accum_out is not None else (None, None)
        if aa is None:
            fn = lambda E: E.activation(out=oa, in_=ia, func=func, bias=ba, scale=sa)
        else:
            fn = lambda E: E.activation(out=oa, in_=ia, func=func, bias=ba, scale=sa, accum_out=aa)
        return self.op("act", fn, [it, bt, st_], [ot, at])

    def tt(self, eng, out, in0, in1, op):
        ot, oa = _ta(out)
        at, aa = _ta(in0)
        bt, ba = _ta(in1)
        return self.op(eng, lambda E: E.tensor_tensor(out=oa, in0=aa, in1=ba, op=op), [at, bt], [ot])

    def ts(self, eng, out, in0, s1, s2, op0, op1=None):
        ot, oa = _ta(out)
        at, aa = _ta(in0)
        t1, a1 = _ta(s1)
        t2, a2 = _ta(s2)
        if op1 is None:
            fn = lambda E: E.tensor_scalar(out=oa, in0=aa, scalar1=a1, scalar2=None, op0=op0)
        else:
            fn = lambda E: E.tensor_scalar(out=oa, in0=aa, scalar1=a1, scalar2=a2, op0=op0, op1=op1)
        return self.op(eng, fn, [at, t1, t2], [ot])

    def stt(self, eng, out, in0, scalar, in1, op0, op1):
        ot, oa = _ta(out)
        at, aa = _ta(in0)
        st_, sa = _ta(scalar)
        bt, ba = _ta(in1)
        return self.op(eng, lambda E: E.scalar_tensor_tensor(out=oa, in0=aa, scalar=sa, in1=ba, op0=op0, op1=op1),
                       [at, st_, bt], [ot])

    def copy(self, eng, out, in_):
        ot, oa = _ta(out)
        it, ia = _ta(in_)
        if eng == "act":
            return self.op("act", lambda E: E.activation(out=oa, in_=ia, func=ACTF.Copy), [it], [ot])
        return self.op(eng, lambda E: E.tensor_copy(out=oa, in_=ia), [it], [ot])

    def recip(self, out, in_):
        ot, oa = _ta(out)
        it, ia = _ta(in_)
        return self.op("dve", lambda E: E.reciprocal(out=oa, in_=ia), [it], [ot])

    def memset(self, eng, out, val):
        ot, oa = _ta(out)
        return self.op(eng, lambda E: E.memset(oa, val), [], [ot])

    def emit(self):
        nc = self.nc
        q = self.q
        with nc.Block() as block:
            @block.sync
            def _(E):
                for f in q["sp"]:
                    f(E)

            @block.scalar
            def _(E):
                for f in q["act"]:
                    f(E)

            @block.vector
            def _(E):
                for f in q["dve"]:
                    f(E)

            @block.gpsimd
            def _(E):
                for f in q["pool"]:
                    f(E)

            @block.tensor
            def _(E):
                for f in q["pe"]:
                    f(E)


class Arena:
    def __init__(self, nc, stack, name, nbytes):
        self.n = nbytes // 4
        self.t = stack.enter_context(nc.sbuf_tensor(name, [128, self.n], F32))
        self.off = 0
        self.peak = 0
        self.top = self.n

    def alloc(self, free_shape, dt, name="", top=False):
        nel = int(np.prod(free_shape))
        nw = nel if dt == F32 else (nel + 1) // 2
        if top:
            self.top = (self.top - nw) // 8 * 8
            assert self.off <= self.top, f"arena overflow (top) allocating {name}"
            ap = self.t[:, self.top:self.top + nw]
        else:
            self.off = (self.off + 7) // 8 * 8
            assert self.off + nw <= self.top, f"arena overflow allocating {name}: {self.off + nw} > {self.top}"
            ap = self.t[:, self.off:self.off + nw]
            self.off += nw
        self.peak = max(self.peak, self.off + (self.n - self.top))
        if dt != F32:
            ap = ap.bitcast(dt)
            if nel != 2 * nw:
                ap = ap[:, 0:nel]
        if len(free_shape) == 2:
            ap = ap.rearrange("p (a b) -> p a b", a=free_shape[0])
        elif len(free_shape) == 3:
            ap = ap.rearrange("p (a b c) -> p a b c", a=free_shape[0], b=free_shape[1])
        return T(ap, name)

    def mark(self):
        return self.off

    def release(self, m):
        self.off = m

    def release_top(self):
        self.top = self.n


import math

D = 1024
HD = 64
ALPHA = 4.0 ** 0.25
LN_EPS = 1e-6
RMS_EPS = 1e-6
NCTX = 256


class Cfg:
    def __init__(self, S=4096, NE=32, L=2):
        self.S, self.NE, self.L = S, NE, L
        self.NOWN = S // 2
        self.NB = self.NOWN // 512
        self.NCOL = NCTX + self.NOWN
        self.NT_OWN = self.NOWN // 128
        self.NT_ALL = S // 128
        self.blocks = [(0, NCTX, 1)] + [(NCTX + 512 * i, 512, 0) for i in range(self.NB)]


def dram_inputs(cfg, mode="A"):
    L, NE, NOWN = cfg.L, cfg.NE, cfg.NOWN
    NKO, NK = NCTX + NOWN, NCTX + 2 * NOWN
    common = {
        "cvec": ([128, 16], F32), "mod_w": ([L, D, 6 * D], F32), "mod_bT": ([128, L * 48], F32),
        "lnT": ([128, L * 2 * 2 * 8], F32),
        "router_w": ([L, D, NE], F32), "router_b": ([L, 1, NE], F32),
        "e_bguT": ([128, L * NE * 16], F32), "e_bd": ([L, NE, D], F32),
    }
    a_only = {
        "xT_own": ([D, NOWN], F32), "xT_par": ([D, NOWN], F32), "xT_halo": ([D, 256], F32), "cT": ([D, NCTX], F32),
        "w_in0": ([D, 1536], F32), "w_out0": ([D, D], F32), "sink": ([1, 12], F32),
        "w_in1": ([D, 2304], F32), "qkng": ([128, 2], F32),
        "e_wgu0": ([NE, D, 2 * D], F32), "e_wd0": ([NE, D, D], F32),
        "cos_own": ([128, NOWN], F32), "sin_own": ([128, NOWN], F32),
        "cos_halo": ([128, 256], F32), "sin_halo": ([128, 256], F32), "valid_halo": ([128, 2], F32),
        "Rm": ([128, 128], F32), "masks": ([128, 768], F32), "CS2": ([128, 256], F32),
        "blk1": ([128, 128], F32),
        "tabC": ([cfg.NB, 128, cfg.NT_ALL, 512], BF16), "tabS": ([cfg.NB, 128, cfg.NT_ALL, 512], BF16),
        "tabCc": ([128, 2, NCTX], BF16), "tabSc": ([128, 2, NCTX], BF16),
    }
    b_only = {
        "w_out1": ([D, D], F32), "lam": ([1, 256], F32), "subln": ([1, 128], F32),
        "e_wgu1": ([NE, D, 2 * D], F32), "e_wd1": ([NE, D, D], F32),
    }
    b_state = {
        "xstate": ([128, 8, cfg.NCOL], F32), "q1": ([8, 128, NOWN], BF16), "kT_all": ([5, 128, NK], BF16),
        "vc_all": ([4, NK, 129], BF16), "vd_all": ([2, NK, 65], BF16),
    }
    d = dict(common)
    if mode in ("A", "F"):
        d.update(a_only)
    if mode in ("B", "F"):
        d.update(b_only)
    if mode == "B":
        d.update(b_state)
    return d


class Prog:
    def __init__(self, cfg, mode="A", stop_after=None, dbg=()):
        self.cfg = cfg
        self.mode = mode
        self.stop_after = stop_after
        self.dbg = dbg
        self.nc = bass.Bass("TRN2", target_bir_lowering=False)
        self.st = ExitStack()

    def build(self):
        cfg, nc, st, mode = self.cfg, self.nc, self.st, self.mode
        NOWN = cfg.NOWN
        NKO, NK = NCTX + NOWN, NCTX + 2 * NOWN
        with st:
            self.S = S = Sched(nc, st)
            self.din = {}
            for name, (shape, dt) in dram_inputs(cfg, mode).items():
                self.din[name] = nc.dram_tensor(name, shape, dt, kind="ExternalInput").ap()
            self.dbg_aps = {}
            self.DT = {k: T(v, k) for k, v in self.din.items()}
            pers_b = 8 * cfg.NCOL * 6 + 6144
            self.pers = Arena(nc, st, "pers", pers_b)
            self.work = Arena(nc, st, "work", (210000 - pers_b) // 32 * 32)
            self.PS = [T(st.enter_context(nc.psum_tensor(f"ps{i}", [128, 512], F32))[:], f"ps{i}") for i in range(8)]
            self.setup_persistent()
            self.phase0_mod()
            done = False
            if mode in ("A", "F"):
                self.layer0()
                done = self.stop_after is not None
            if mode == "A" and not done:
                outs = {}
                for name, shape, dt in (("xstate", [128, 8, cfg.NCOL], F32), ("q1", [8, 128, NOWN], BF16),
                                        ("kT_own", [5, 128, NKO], BF16), ("vc_own", [4, NKO, 129], BF16), ("vd_own", [2, NKO, 65], BF16)):
                    outs[name] = nc.dram_tensor(name, shape, dt, kind="ExternalOutput").ap()
                self.layer1_proj(outs["q1"], outs["kT_own"], outs["vc_own"], outs["vd_own"])
                XS = T(outs["xstate"], "xstate_o")
                for c in range(8):
                    for bi, (c0, n, v) in enumerate(cfg.blocks):
                        S.dma("sp" if c % 2 == 0 else "act", (XS, outs["xstate"][:, c, c0:c0 + n]), self.xT[c][bi])
            if mode == "B":
                self.layer1_attn(self.din["q1"], self.din["kT_all"], self.din["vc_all"], self.din["vd_all"])
                self.moe(1)
                out_ap = nc.dram_tensor("outT", [128, 8, NOWN], F32, kind="ExternalOutput").ap()
                OUT = T(out_ap, "outT")
                for c in range(8):
                    for bi, (c0, n, v) in enumerate(cfg.blocks):
                        if v == 1:
                            continue
                        S.dma("sp" if c % 2 == 0 else "act", (OUT, out_ap[:, c, c0 - NCTX:c0 - NCTX + n]), self.xT[c][bi])
            S.wait_all_on("sp")
            S.wait_all_on("pool")
            S.wait_all_on("act")
            print("mode", mode, "instructions:", S.ninstr, "pers peak", self.pers.peak * 4, "work peak", self.work.peak * 4)
            S.emit()
        return nc

    def dbg_out(self, name, src_T, src_ap, shape, dt=F32):
        if name not in self.dbg:
            return
        ap = self.nc.dram_tensor("dbg_" + name, list(shape), dt, kind="ExternalOutput").ap()
        self.dbg_aps[name] = ap
        self.S.barrier()
        self.S.dma("sp", (T(ap), ap), (src_T, src_ap))

    def setup_persistent(self):
        cfg, S, P = self.cfg, self.S, self.pers
        DT = self.DT
        self.xT = [[None] * len(cfg.blocks) for _ in range(8)]
        xall = P.alloc([8, cfg.NCOL], F32, "xT")
        self.xall = xall
        for c in range(8):
            for bi, (c0, n, v) in enumerate(cfg.blocks):
                self.xT[c][bi] = T(xall.ap[:, c, c0:c0 + n], f"xT{c}_{bi}")
        self.hT2 = P.alloc([8, cfg.NCOL], BF16, "hbuf")
        self.hT2b = [[T(self.hT2.ap[:, c, c0:c0 + n], f"h2_{c}_{bi}") for bi, (c0, n, v) in enumerate(cfg.blocks)] for c in range(8)]
        self.ident_f = P.alloc([128], F32, "ident_f")
        self.ident_b = P.alloc([128], BF16, "ident_b")
        self.ones_f = P.alloc([128], F32, "ones_f")
        self.lnT = P.alloc([cfg.L * 32], F32, "lnT")
        self.mv = [P.alloc([cfg.L * 48], F32, f"mv{v}") for v in range(2)]
        self.scal = P.alloc([cfg.L * 2 * 2 * 3 * 8 + 32], F32, "scal")
        self._scal_off = 0
        self._scal_map = {}
        S.memset("pool", self.ident_f, 1.0)
        S.op("pool", lambda E: E.affine_select(out=self.ident_f.ap, in_=self.ident_f.ap, pattern=[[-1, 128]],
                                                compare_op=ALU.is_equal, fill=0.0, base=0, channel_multiplier=1),
             [self.ident_f], [self.ident_f])
        S.copy("dve", self.ident_b, self.ident_f)
        S.memset("dve", self.ones_f, 1.0)
        S.dma("sp", self.lnT, DT["lnT"])
        for bi, (c0, n, v) in enumerate(cfg.blocks):
            if self.mode == "B":
                for c in range(8):
                    S.dma("sp" if c % 2 == 0 else "act", self.xT[c][bi], (DT["xstate"], self.din["xstate"][:, c, c0:c0 + n]))
                continue
            src = self.din["cT"] if v == 1 else self.din["xT_own"][:, c0 - NCTX:c0 - NCTX + n]
            srcT = DT["cT"] if v == 1 else DT["xT_own"]
            for c in range(8):
                S.dma("sp", self.xT[c][bi], (srcT, src[c * 128:(c + 1) * 128, :]))

    def sc(self, key):
        if key not in self._scal_map:
            self._scal_map[key] = self._scal_off
            self._scal_off += 8
        o = self._scal_map[key]
        return (self.scal, self.scal.ap[:, o:o + 8])

    def lnp(self, l, i, gb):
        o = ((l * 2 + i) * 2 + gb) * 8
        return (self.lnT, self.lnT.ap[:, o:o + 8])

    def mcol(self, v, l, j):
        o = l * 48 + j * 8
        return (self.mv[v], self.mv[v].ap[:, o:o + 8])

    def phase0_mod(self):
        cfg, S, W, DT = self.cfg, self.S, self.work, self.DT
        m0 = W.mark()
        cv = W.alloc([16], F32, "cv")
        sT = W.alloc([16], F32, "sT")
        mbT = W.alloc([cfg.L * 48], F32, "mbT")
        mw = [W.alloc([8, 512], F32, f"mw{i}") for i in range(2)]
        S.dma("sp", cv, DT["cvec"])
        S.dma("sp", mbT, DT["mod_bT"])
        S.act(sT, cv, ACTF.Silu)
        sview = sT.ap.rearrange("p (v k) -> p k v", v=2)
        mps = self.PS[0]
        it = 0
        for l in range(cfg.L):
            for cb in range(12):
                buf = mw[it % 2]
                it += 1
                for kc in range(8):
                    S.dma("sp" if kc % 2 == 0 else "act", (buf, buf.ap[:, kc, :]),
                          (DT["mod_w"], self.din["mod_w"][l, kc * 128:(kc + 1) * 128, cb * 512:(cb + 1) * 512]))
                for sub in range(4):
                    ch = l * 48 + cb * 4 + sub
                    S.mm((mps, mps.ap[:, ch * 2:ch * 2 + 2]),
                         [((buf, buf.ap[:, kc, sub * 128:(sub + 1) * 128]), (sT, sview[:, kc, :])) for kc in range(8)])
        nch = cfg.L * 48
        for v in range(2):
            S.tt("dve", self.mv[v], (mps, mps.ap[:, 0:2 * nch].rearrange("p (c v) -> p c v", v=2)[:, :, v]), mbT, ALU.add)
        for v in range(2):
            S.ts("dve", self.sc(("A0s", v)), self.mcol(v, 0, 1), 1.0, None, ALU.add)
            S.copy("dve", self.sc(("A0b", v)), self.mcol(v, 0, 0))
            for l in range(cfg.L):
                for i in range(2):
                    S.ts("dve", self.sc(("gp", l, i, v)), self.mcol(v, l, 2 + 3 * i), 1.0 / ALPHA, None, ALU.mult)
                    if i == 0:
                        nsc, nsh = self.mcol(v, l, 4), self.mcol(v, l, 3)
                    elif l + 1 < cfg.L:
                        nsc, nsh = self.mcol(v, l + 1, 1), self.mcol(v, l + 1, 0)
                    else:
                        continue
                    tmp = self.sc(("tmp", v))
                    S.ts("dve", tmp, nsc, 1.0, None, ALU.add)
                    S.tt("dve", self.sc(("hG", l, i, v)), self.lnp(l, i, 0), tmp, ALU.mult)
                    S.tt("dve", self.sc(("hB", l, i, v)), self.lnp(l, i, 1), tmp, ALU.mult)
                    S.tt("dve", self.sc(("hB", l, i, v)), self.sc(("hB", l, i, v)), nsh, ALU.add)
        self.dbg_out("mv0", self.mv[0], self.mv[0].ap, [128, cfg.L * 48])
        self.dbg_out("mv1", self.mv[1], self.mv[1].ap, [128, cfg.L * 48])
        S.barrier()
        W.release(m0)

    def load_w_bf16(self, dst, src_name, src_ap, ncols, nk=8):
        S = self.S
        for kc in range(nk):
            S.dma("pool", (dst, dst.ap[:, kc, :]), (self.DT[src_name], src_ap[kc * 128:(kc + 1) * 128, :]))

    def layer0(self):
        cfg, S, W, DT, PS = self.cfg, self.S, self.work, self.DT, self.PS
        NOWN, NT_OWN, NT_ALL = cfg.NOWN, cfg.NT_OWN, cfg.NT_ALL
        m_layer = W.mark()
        hflat = self.hT2.ap.rearrange("p c n -> p (c n)")
        AcAs = [T(hflat[:, t * 512:(t + 1) * 512], f"AcAs{t}") for t in range(NT_ALL)]
        AcAs_c = [T(hflat[:, (NT_ALL + t) * 512:(NT_ALL + t + 1) * 512], f"AcAsc{t}") for t in range(2)]
        qd0 = self.nc.dram_tensor("qd0", [6, 128, NOWN], BF16, kind="Internal").ap()
        qd0c = self.nc.dram_tensor("qd0c", [6, 128, NCTX], BF16, kind="Internal").ap()
        QD0 = [T(qd0[i], f"qd0_{i}") for i in range(6)]
        QD0c = [T(qd0c[i], f"qd0c_{i}") for i in range(6)]
        m_attn = W.mark()
        KT = [W.alloc([NOWN], BF16, f"KT{i}") for i in range(2)]
        KTh = [W.alloc([256], BF16, f"KTh{i}") for i in range(2)]
        KTc = [W.alloc([NCTX], BF16, f"KTc{i}") for i in range(2)]
        Vo = [W.alloc([4, 65], BF16, f"Vo{t}") for t in range(NT_OWN)]
        Vh = [W.alloc([4, 65], BF16, f"Vh{t}") for t in range(2)]
        Vc = [W.alloc([4, 65], BF16, f"Vc{t}") for t in range(2)]
        m_proj = W.mark()
        Win = W.alloc([8, 1536], BF16, "Win")
        csb = [W.alloc([2, 512], F32, f"csb{i}") for i in range(2)]
        cosH = W.alloc([256], F32, "cosH")
        sinH = W.alloc([256], F32, "sinH")
        vh = W.alloc([2], F32, "vh")
        Rm = W.alloc([128], BF16, "Rm")
        CS2 = W.alloc([256], BF16, "CS2")
        xs = [W.alloc([512], F32, f"xs{i}") for i in range(3)]
        hT = [W.alloc([8, 512], BF16, "hT0")]
        qs = [W.alloc([512], BF16, "qs0")]
        qst = [W.alloc([512], BF16, f"qst{i}") for i in range(3)]
        t1 = [W.alloc([512], F32, f"t1{i}") for i in range(2)]
        t2 = [W.alloc([512], F32, f"t2{i}") for i in range(2)]
        aTb = [W.alloc([2, 512], BF16, f"aTb{i}") for i in range(2)]
        self.load_w_bf16(Win, "w_in0", self.din["w_in0"], 1536)
        S.dma("sp", cosH, DT["cos_halo"]); S.dma("sp", sinH, DT["sin_halo"])
        S.dma("sp", vh, DT["valid_halo"])
        S.dma("pool", Rm, DT["Rm"]); S.dma("pool", CS2, DT["CS2"])

        tblocks = [("ctx", 0, NCTX)] + [("own", i, 512) for i in range(cfg.NB)] + [("halo", 0, 256)] + \
                  [("par", i, 512) for i in range(cfg.NB)]
        cnt = {"h": 0, "q": 0, "a": 0, "a2": 0, "pp": 0, "pr": 0, "pv": 0, "pa": 0, "xs": 0, "cs": 0, "qst": 0}

        def nxt(k, m=2):
            v = cnt[k] % m
            cnt[k] += 1
            return v

        for (kind, bi, n) in tblocks:
            v = 1 if kind == "ctx" else 0
            hb = hT[0]
            if kind == "own":
                cb_ = csb[nxt("cs")]
                S.dma("sp", (cb_, cb_.ap[:, 0, :]), (DT["cos_own"], self.din["cos_own"][:, bi * 512:(bi + 1) * 512]))
                S.dma("act", (cb_, cb_.ap[:, 1, :]), (DT["sin_own"], self.din["sin_own"][:, bi * 512:(bi + 1) * 512]))
            for c in range(8):
                if kind == "ctx":
                    src = self.xT[c][0]
                elif kind == "own":
                    src = self.xT[c][1 + bi]
                else:
                    dn = "xT_halo" if kind == "halo" else "xT_par"
                    dap = self.din[dn][c * 128:(c + 1) * 128, (0 if kind == "halo" else bi * 512):(0 if kind == "halo" else bi * 512) + n]
                    xb_ = xs[nxt("xs", 3)]
                    S.dma("sp" if c % 2 == 0 else "act", (xb_, xb_.ap[:, 0:n]), (DT[dn], dap))
                    src = (xb_, xb_.ap[:, 0:n])
                sT_, sA = self.sc(("A0s", v))
                bT_, bA = self.sc(("A0b", v))
                S.act((hb, hb.ap[:, c, 0:n]), src, ACTF.Identity, bias=(bT_, bA[:, c:c + 1]), scale=(sT_, sA[:, c:c + 1]))

            def proj_fm(col0):
                ps = PS[nxt("pp")]
                S.mm((ps, ps.ap[:, 0:n]), [((Win, Win.ap[:, kc, col0:col0 + 128]), (hb, hb.ap[:, kc, 0:n])) for kc in range(8)])
                return ps

            def rope_to(ps, dst, cos_, sin_):
                q_ = qs[0]
                S.copy("act", (q_, q_.ap[:, 0:n]), (ps, ps.ap[:, 0:n]))
                pr = PS[2 + nxt("pr")]
                S.mm((pr, pr.ap[:, 0:n]), [(Rm, (q_, q_.ap[:, 0:n]))])
                i_ = nxt("a")
                a1, a2 = t1[i_], t2[i_]
                S.tt("pool", (a1, a1.ap[:, 0:n]), (q_, q_.ap[:, 0:n]), cos_, ALU.mult)
                S.tt("dve", (a2, a2.ap[:, 0:n]), (pr, pr.ap[:, 0:n]), sin_, ALU.mult)
                S.tt("pool", dst, (a1, a1.ap[:, 0:n]), (a2, a2.ap[:, 0:n]), ALU.add)

            if kind in ("ctx", "own"):
                for ti in range(6):
                    ps = proj_fm(256 + ti * 128)
                    qb_ = qst[nxt("qst", 3)]
                    if kind == "ctx":
                        S.copy("act", (qb_, qb_.ap[:, 0:n]), (ps, ps.ap[:, 0:n]))
                        S.dma("sp", QD0c[ti], (qb_, qb_.ap[:, 0:n]))
                    else:
                        rope_to(ps, (qb_, qb_.ap[:, 0:n]), (cb_, cb_.ap[:, 0, :]), (cb_, cb_.ap[:, 1, :]))
                        S.dma("sp", (QD0[ti], qd0[ti][:, bi * 512:bi * 512 + n]), (qb_, qb_.ap[:, 0:n]))
            if kind in ("ctx", "own", "halo"):
                for ti in range(2):
                    ps = proj_fm(1024 + ti * 128)
                    if kind == "ctx":
                        S.copy("act", KTc[ti], (ps, ps.ap[:, 0:n]))
                    elif kind == "own":
                        rope_to(ps, (KT[ti], KT[ti].ap[:, bi * 512:bi * 512 + n]), (cb_, cb_.ap[:, 0, :]), (cb_, cb_.ap[:, 1, :]))
                    else:
                        rope_to(ps, KTh[ti], cosH, sinH)
                for tt_ in range(n // 128):
                    pv = PS[4 + nxt("pv")]
                    S.mm((pv, pv.ap[:, 0:256]),
                         [((hb, hb.ap[:, kc, tt_ * 128:(tt_ + 1) * 128]), (Win, Win.ap[:, kc, 1280:1536])) for kc in range(8)])
                    if kind == "ctx":
                        vt = Vc[tt_]
                    elif kind == "own":
                        vt = Vo[bi * 4 + tt_]
                    else:
                        vt = Vh[tt_]
                    pvv = pv.ap[:, 0:256].rearrange("p (g d) -> p g d", g=4)
                    if kind == "halo":
                        S.ts("dve", (vt, vt.ap[:, :, 0:64]), (pv, pvv), (vh, vh.ap[:, tt_:tt_ + 1]), None, ALU.mult)
                        S.copy("pool", (vt, vt.ap[:, :, 64]), (vh, vh.ap[:, tt_:tt_ + 1].to_broadcast([128, 4])))
                    else:
                        S.copy("act", (vt, vt.ap[:, :, 0:64]), (pv, pvv))
                        S.memset("pool", (vt, vt.ap[:, :, 64]), 1.0)
            if kind in ("ctx", "own", "par"):
                ab = aTb[nxt("a2")]
                for cc in range(2):
                    ps = proj_fm(cc * 128)
                    S.copy("act" if cc == 0 else "dve", (ab, ab.ap[:, cc, 0:n]), (ps, ps.ap[:, 0:n]))
                for tt_ in range(n // 128):
                    pa = PS[6 + nxt("pa")]
                    for cc in range(2):
                        S.mm((pa, pa.ap[:, cc * 256:(cc + 1) * 256]), [((ab, ab.ap[:, cc, tt_ * 128:(tt_ + 1) * 128]), CS2)])
                    if kind == "ctx":
                        dst = AcAs_c[tt_]
                    elif kind == "own":
                        dst = AcAs[bi * 4 + tt_]
                    else:
                        dst = AcAs[NT_OWN + bi * 4 + tt_]
                    S.copy("act" if tt_ % 2 == 0 else "dve", dst, pa)
        self.dbg_out("KT0", KT[0], KT[0].ap, [128, NOWN], BF16)
        self.dbg_out("KTh0", KTh[0], KTh[0].ap, [128, 256], BF16)
        self.dbg_out("Vo0", Vo[0], Vo[0].ap, [128, 4, 65], BF16)
        self.dbg_out("Vh0", Vh[0], Vh[0].ap, [128, 4, 65], BF16)
        self.dbg_out("AcAs0", AcAs[0], AcAs[0].ap, [128, 512], BF16)
        if self.stop_after == "proj0":
            return
        S.barrier()
        W.release(m_proj)

        oT = W.alloc([8, cfg.NCOL], BF16, "oT", top=True)
        masks = W.alloc([768], BF16, "masks")
        esink = W.alloc([12], F32, "esink")
        o_tok = [W.alloc([768], BF16, f"otok{i}") for i in range(2)]
        PTl = [W.alloc([3, 384], BF16, f"PTl{i}") for i in range(2)]
        PTc = [W.alloc([2, 384], BF16, f"PTc{i}") for i in range(2)]
        zs = [W.alloc([4], F32, f"zs{i}") for i in range(2)]
        S.dma("pool", masks, DT["masks"])
        S.dma("sp", esink, (DT["sink"], self.din["sink"].partition_broadcast(128)))
        S.act(esink, esink, ACTF.Exp)
        psO = PS[5]
        psT = T(PS[6].ap.bitcast(BF16), "psT_bf")
        ac = {"pt": 0, "o": 0, "z": 0, "ot": 0}

        oT_att = [T(oT.ap[:, 2:8, c0:c0 + n], f"oTatt{bi}") for bi, (c0, n, v) in enumerate(cfg.blocks)]
        self.oT_att = oT_att
        def kfn(tiles, c0):
            return lambda ti: tiles[ti].ap[:, c0:c0 + 128]
        ctx_keys = [((KTc[0], KTc[1]), kfn(KTc, t * 128), Vc[t], "ctx") for t in range(2)]

        def attend_impl(Qsrc, klist, dst):
            ot = o_tok[ac["ot"] % 2]
            ac["ot"] += 1
            loc = [k for k in klist if k[3] != "ctx"]
            ctxk = [k for k in klist if k[3] == "ctx"]
            for g in range(4):
                half = g % 2
                rows = slice(half * 64, half * 64 + 64)
                ti = g // 2
                q_tiles = [(0 if g < 2 else 3) + j for j in range(3)]
                pl = PTl[ac["pt"] % 2]
                pc = PTc[ac["pt"] % 2]
                ac["pt"] += 1
                for grp, banks, ptile in ((loc, (0, 1, 2), pl), (ctxk, (3, 4), pc)):
                    for ci, (kTs, kap, vt, mt) in enumerate(grp):
                        ps = PS[banks[ci]]
                        for j in range(3):
                            qT_, qap = Qsrc[q_tiles[j]]
                            S.mm((ps, ps.ap[:, j * 128:(j + 1) * 128]), [((kTs[ti], kap(ti)[rows, :]), (qT_, qap[rows, :]))])
                        S.act((ptile, ptile.ap[:, ci, :]), (ps, ps.ap[:, 0:384]), ACTF.Exp, scale=0.125)
                        if mt == "lo":
                            S.tt("pool", (ptile, ptile.ap[:, ci, :]), (ptile, ptile.ap[:, ci, :]), (masks, masks.ap[:, 0:384]), ALU.mult)
                        elif mt == "hi":
                            S.tt("pool", (ptile, ptile.ap[:, ci, :]), (ptile, ptile.ap[:, ci, :]), (masks, masks.ap[:, 384:768]), ALU.mult)
                oo = (ac["o"] % 2) * 256
                ac["o"] += 1
                allk = [(pl, ci, k) for ci, k in enumerate(loc)] + [(pc, ci, k) for ci, k in enumerate(ctxk)]
                for j in range(3):
                    S.mm((psO, psO.ap[:, oo + j * 65: oo + (j + 1) * 65]),
                         [((pt_, pt_.ap[:, ci, j * 128:(j + 1) * 128]), (k[2], k[2].ap[:, g, :])) for (pt_, ci, k) in allk])
                z = zs[ac["z"] % 2]
                ac["z"] += 1
                ov = psO.ap[:, oo:oo + 195].rearrange("p (j d) -> p j d", j=3)
                S.tt("dve", (z, z.ap[:, 0:3]), (psO, ov[:, :, 64]), (esink, esink.ap[:, 3 * g:3 * g + 3]), ALU.add)
                S.recip((z, z.ap[:, 0:3]), (z, z.ap[:, 0:3]))
                S.tt("dve", (ot, ot.ap[:, 192 * g:192 * g + 192].rearrange("p (j d) -> p j d", j=3)), (psO, ov[:, :, 0:64]),
                     (z, z.ap[:, 0:3].unsqueeze(2).to_broadcast([128, 3, 64])), ALU.mult)
            for c in range(6):
                S.transpose((psT, psT.ap[:, c * 128:(c + 1) * 128]), (ot, ot.ap[:, c * 128:(c + 1) * 128]), self.ident_b)
            dstT, dap = dst
            S.copy("act", (dstT, dap), (psT, psT.ap[:, 0:768].rearrange("p (c q) -> p c q", c=6)))

        Qb = [W.alloc([6, 512], BF16, f"Qb{i}") for i in range(2)]
        for i in range(6):
            S.dma("sp" if i % 2 == 0 else "act", (Qb[1], Qb[1].ap[:, i, 0:NCTX]), QD0c[i])
        for t in range(2):
            attend_impl([(Qb[1], Qb[1].ap[:, i, t * 128:(t + 1) * 128]) for i in range(6)], ctx_keys,
                        (oT_att[0], oT.ap[:, 2:8, t * 128:(t + 1) * 128]))
        for n_ in range(NT_OWN):
            if n_ % 4 == 0:
                qb_ = Qb[(n_ // 4) % 2]
                for i in range(6):
                    S.dma("sp" if i % 2 == 0 else "act", (qb_, qb_.ap[:, i, :]), (QD0[i], qd0[i][:, n_ * 128:n_ * 128 + 512]))
            kl = []
            if n_ == 0:
                kl.append(((KTh[0], KTh[1]), kfn(KTh, 0), Vh[0], "lo"))
            else:
                kl.append(((KT[0], KT[1]), kfn(KT, (n_ - 1) * 128), Vo[n_ - 1], "lo"))
            kl.append(((KT[0], KT[1]), kfn(KT, n_ * 128), Vo[n_], "mid"))
            if n_ == NT_OWN - 1:
                kl.append(((KTh[0], KTh[1]), kfn(KTh, 128), Vh[1], "hi"))
            else:
                kl.append(((KT[0], KT[1]), kfn(KT, (n_ + 1) * 128), Vo[n_ + 1], "hi"))
            kl += ctx_keys
            bi = 1 + n_ // 4
            c0 = NCTX + n_ * 128
            attend_impl([(qb_, qb_.ap[:, i, (n_ % 4) * 128:(n_ % 4 + 1) * 128]) for i in range(6)], kl,
                        (oT_att[bi], oT.ap[:, 2:8, c0:c0 + 128]))
        self.dbg_out("oT_att", oT, oT.ap[:, 2:8, :], [128, 6, cfg.NCOL], BF16)
        if self.stop_after == "attn0":
            return
        S.barrier()
        W.release(m_attn)
        NPIECE = 8
        tb = [[W.alloc([NPIECE, 512], BF16, f"tab{cs}{i}") for i in range(2)] for cs in range(2)]
        tcx = [W.alloc([2, NCTX], BF16, f"tabc{cs}") for cs in range(2)]
        S.dma("sp", tcx[0], DT["tabCc"]); S.dma("act", tcx[1], DT["tabSc"])
        oT_f = [[T(oT.ap[:, cc, c0:c0 + n], f"oTf{cc}_{bi}") for bi, (c0, n, v) in enumerate(cfg.blocks)] for cc in range(2)]
        self.oT_f = oT_f
        for cc in range(2):
            ps = PS[cc]
            S.mm((ps, ps.ap[:, 0:NCTX]),
                 [((AcAs_c[t], AcAs_c[t].ap[:, cc * 256 + cs * 128: cc * 256 + cs * 128 + 128]), (tcx[cs], tcx[cs].ap[:, t, :]))
                  for t in range(2) for cs in range(2)])
            S.copy("act", oT_f[cc][0], (ps, ps.ap[:, 0:NCTX]))
        pi = 0
        for b in range(cfg.NB):
            npieces = NT_ALL // NPIECE
            for p_ in range(npieces):
                bufs = [tb[0][pi % 2], tb[1][pi % 2]]
                pi += 1
                for cs, nm in ((0, "tabC"), (1, "tabS")):
                    S.dma("sp" if cs == 0 else "act", bufs[cs], (DT[nm], self.din[nm][b, :, p_ * NPIECE:(p_ + 1) * NPIECE, :]))
                for cc in range(2):
                    ps = PS[cc]
                    pairs = []
                    for tl in range(NPIECE):
                        t = p_ * NPIECE + tl
                        for cs in range(2):
                            pairs.append(((AcAs[t], AcAs[t].ap[:, cc * 256 + cs * 128: cc * 256 + cs * 128 + 128]),
                                          (bufs[cs], bufs[cs].ap[:, tl, :])))
                    S.mm(ps, pairs, start=(p_ == 0), stop=(p_ == npieces - 1))
            for cc in range(2):
                S.copy("act" if cc == 0 else "dve", oT_f[cc][1 + b], PS[cc])
        self.dbg_out("oT_f", oT, oT.ap[:, 0:2, :], [128, 2, cfg.NCOL], BF16)
        if self.stop_after == "fourier0":
            return
        S.barrier()
        W.release(m_attn)
        self.oT = oT
        self.outproj_ln(0, "w_out0")
        if self.stop_after == "mix0":
            return
        S.barrier()
        W.release(m_layer)
        W.release_top()
        self.moe(0)
        if self.stop_after == "moe0":
            return

    def outproj_ln(self, l, wname):
        cfg, S, W, DT, PS = self.cfg, self.S, self.work, self.DT, self.PS
        oT = self.oT
        m0 = W.mark()
        Wo = W.alloc([8, D], BF16, "Wo")
        self.load_w_bf16(Wo, wname, self.din[wname], D)
        oTall = T(oT.ap, "oTall")
        k = 0
        for bi, (c0, n, v) in enumerate(cfg.blocks):
            for dc in range(8):
                ps = PS[k % 2]
                k += 1
                S.mm((ps, ps.ap[:, 0:n]), [((Wo, Wo.ap[:, oc, dc * 128:(dc + 1) * 128]), (oTall, oT.ap[:, oc, c0:c0 + n])) for oc in range(8)])
                gT_, gA = self.sc(("gp", l, 0, v))
                xt = self.xT[dc][bi]
                S.stt("dve", xt, (ps, ps.ap[:, 0:n]), (gT_, gA[:, dc:dc + 1]), xt, ALU.mult, ALU.add)
        self.dbg_out(f"z{l}0", self.xall, self.xall.ap, [128, 8, cfg.NCOL])
        self.layernorm(l, 0, self.hT2b)
        W.release(m0)

    def layernorm(self, l, i, hdst, skip_ctx=False):
        cfg, S, W, PS = self.cfg, self.S, self.work, self.PS
        m0 = W.mark()
        zsq = [W.alloc([512], F32, f"zsq{j}") for j in range(2)]
        mean = W.alloc([512], F32, "mean")
        var = W.alloc([512], F32, "var")
        rstd = W.alloc([512], F32, "rstd")
        mr = W.alloc([512], F32, "mr")
        u = [W.alloc([512], F32, f"u{j}") for j in range(2)]
        eps = LN_EPS / (ALPHA * ALPHA)
        last = (l == cfg.L - 1 and i == 1)
        for bi, (c0, n, v) in enumerate(cfg.blocks):
            if (last or skip_ctx) and v == 1:
                continue
            s1, s2 = PS[2], PS[3]
            S.mm((s1, s1.ap[:, 0:n]), [(self.ones_f, self.xT[c][bi]) for c in range(8)])
            for c in range(8):
                zq = zsq[c % 2]
                S.act((zq, zq.ap[:, 0:n]), self.xT[c][bi], ACTF.Square)
                S.mm((s2, s2.ap[:, 0:n]), [(self.ones_f, (zq, zq.ap[:, 0:n]))], start=(c == 0), stop=(c == 7))
            S.act((mean, mean.ap[:, 0:n]), (s1, s1.ap[:, 0:n]), ACTF.Copy, scale=1.0 / D)
            S.tt("pool", (mr, mr.ap[:, 0:n]), (mean, mean.ap[:, 0:n]), (mean, mean.ap[:, 0:n]), ALU.mult)
            S.stt("dve", (var, var.ap[:, 0:n]), (s2, s2.ap[:, 0:n]), 1.0 / D, (mr, mr.ap[:, 0:n]), ALU.mult, ALU.subtract)
            S.ts("dve", (var, var.ap[:, 0:n]), (var, var.ap[:, 0:n]), eps, None, ALU.add)
            S.act((var, var.ap[:, 0:n]), (var, var.ap[:, 0:n]), ACTF.Sqrt)
            S.recip((rstd, rstd.ap[:, 0:n]), (var, var.ap[:, 0:n]))
            S.tt("pool", (mr, mr.ap[:, 0:n]), (mean, mean.ap[:, 0:n]), (rstd, rstd.ap[:, 0:n]), ALU.mult)
            for c in range(8):
                uu = u[c % 2]
                eng = "dve" if c % 2 == 0 else "pool"
                S.tt(eng, (uu, uu.ap[:, 0:n]), self.xT[c][bi], (rstd, rstd.ap[:, 0:n]), ALU.mult)
                S.tt(eng, (uu, uu.ap[:, 0:n]), (uu, uu.ap[:, 0:n]), (mr, mr.ap[:, 0:n]), ALU.subtract)
                gT_, gA = self.lnp(l, i, 0)
                bT_, bA = self.lnp(l, i, 1)
                S.act(self.xT[c][bi], (uu, uu.ap[:, 0:n]), ACTF.Identity, bias=(bT_, bA[:, c:c + 1]), scale=(gT_, gA[:, c:c + 1]))
                if hdst is not None and not last:
                    hgT, hgA = self.sc(("hG", l, i, v))
                    hbT, hbA = self.sc(("hB", l, i, v))
                    S.act(hdst[c][bi], (uu, uu.ap[:, 0:n]), ACTF.Identity, bias=(hbT, hbA[:, c:c + 1]), scale=(hgT, hgA[:, c:c + 1]))
        self.dbg_out(f"x{l}{i}", self.xall, self.xall.ap, [128, 8, cfg.NCOL])
        S.barrier()
        W.release(m0)

    def moe(self, l):
        cfg, S, W, DT, PS = self.cfg, self.S, self.work, self.DT, self.PS
        NE, NCOL = cfg.NE, cfg.NCOL
        last = (l == cfg.L - 1)
        hT2, hT2b = self.hT2, self.hT2b
        m0 = W.mark()
        rw = W.alloc([8, NE], BF16, "rw")
        rb = W.alloc([NE], F32, "rb")
        GT = W.alloc([NCOL], F32, "GT")
        bgu = W.alloc([NE * 16], F32, "bgu")
        bd = W.alloc([D], F32, "bd")
        for kc in range(8):
            S.dma("pool", (rw, rw.ap[:, kc, :]), (DT["router_w"], self.din["router_w"][l, kc * 128:(kc + 1) * 128, :]))
        S.dma("sp", rb, (DT["router_b"], self.din["router_b"][l].partition_broadcast(128)))
        S.dma("sp", bgu, (DT["e_bguT"], self.din["e_bguT"][:, l * NE * 16:(l + 1) * NE * 16]))
        S.dma("sp", (bd, bd.ap[0:NE, :]), (DT["e_bd"], self.din["e_bd"][l]))
        Lg = [W.alloc([NE], F32, f"Lg{i}") for i in range(2)]
        Ex = [W.alloc([NE], F32, f"Ex{i}") for i in range(2)]
        Mk = [W.alloc([NE], F32, f"Mk{i}") for i in range(2)]
        t8 = [W.alloc([8], F32, f"t8{i}") for i in range(2)]
        sm = [W.alloc([4], F32, f"sm{i}") for i in range(2)]
        k = 0
        for bi, (c0, n, v) in enumerate(cfg.blocks):
            if last and v == 1:
                continue
            for tt_ in range(n // 128):
                i = k % 2
                k += 1
                pr, pt = PS[6], PS[7]
                S.mm((pr, pr.ap[:, 0:NE]), [((hT2b[kc][bi], hT2b[kc][bi].ap[:, tt_ * 128:(tt_ + 1) * 128]), (rw, rw.ap[:, kc, :])) for kc in range(8)])
                S.tt("dve", Lg[i], (pr, pr.ap[:, 0:NE]), rb, ALU.add)
                S.op("dve", lambda E, o=t8[i].ap, a=Lg[i].ap: E.max(out=o, in_=a), [Lg[i]], [t8[i]])
                S.ts("dve", Mk[i], Lg[i], (t8[i], t8[i].ap[:, 3:4]), None, ALU.is_ge)
                S.ts("dve", (sm[i], sm[i].ap[:, 0:1]), (t8[i], t8[i].ap[:, 0:1]), -1.0, None, ALU.mult)
                S.act(Ex[i], Lg[i], ACTF.Exp, bias=(sm[i], sm[i].ap[:, 0:1]))
                S.tt("dve", Ex[i], Ex[i], Mk[i], ALU.mult)
                S.op("dve", lambda E, o=sm[i].ap[:, 1:2], a=Ex[i].ap: E.reduce_sum(out=o, in_=a, axis=AX.X), [Ex[i]], [sm[i]])
                S.recip((sm[i], sm[i].ap[:, 2:3]), (sm[i], sm[i].ap[:, 1:2]))
                S.ts("dve", Ex[i], Ex[i], (sm[i], sm[i].ap[:, 2:3]), None, ALU.mult)
                S.transpose((pt, pt.ap[0:NE, 0:128]), Ex[i], self.ident_f)
                S.copy("act", (GT, GT.ap[0:NE, c0 + tt_ * 128:c0 + (tt_ + 1) * 128]), (pt, pt.ap[0:NE, 0:128]))
        self.dbg_out(f"GT{l}", GT, GT.ap[0:NE, :], [NE, NCOL])
        Wg = [[W.alloc([512], BF16, f"Wg{b}_{kc}") for kc in range(8)] for b in range(2)]
        Wu = [[W.alloc([512], BF16, f"Wu{b}_{kc}") for kc in range(8)] for b in range(2)]
        Wdn = [[W.alloc([D], BF16, f"Wd{b}_{jj}") for jj in range(4)] for b in range(2)]
        actT = [W.alloc([4, 512], BF16, f"actT{i}") for i in range(2)]
        g1 = [W.alloc([512], F32, f"g1{i}") for i in range(2)]
        u1 = [W.alloc([512], F32, f"u1{i}") for i in range(2)]
        sg = [W.alloc([512], BF16, f"sg{i}") for i in range(2)]
        gsb = [W.alloc([512], F32, f"gsb{i}") for i in range(2)]
        wgu = self.din[f"e_wgu{l}"]
        wd = self.din[f"e_wd{l}"]
        WGU, WD_ = DT[f"e_wgu{l}"], DT[f"e_wd{l}"]

        def load_half(e, hh, b):
            for kc in range(8):
                S.dma("pool", Wg[b][kc], (WGU, wgu[e, kc * 128:(kc + 1) * 128, hh * 512:(hh + 1) * 512]))
                S.dma("pool", Wu[b][kc], (WGU, wgu[e, kc * 128:(kc + 1) * 128, 1024 + hh * 512:1024 + (hh + 1) * 512]))
            for jj in range(4):
                S.dma("pool", Wdn[b][jj], (WD_, wd[e, (hh * 4 + jj) * 128:(hh * 4 + jj + 1) * 128, :]))

        halves = [(e, hh) for e in range(NE) for hh in range(2)]
        load_half(0, 0, 0)
        cq = {"q": 0, "a": 0, "y": 0, "g": 0}
        gpT, gpA = None, None
        for it, (e, hh) in enumerate(halves):
            b = it % 2
            if it + 1 < len(halves):
                load_half(halves[it + 1][0], halves[it + 1][1], (it + 1) % 2)
            for bi, (c0, n, v) in enumerate(cfg.blocks):
                if last and v == 1:
                    continue
                gpT, gpA = self.sc(("gp", l, 1, v))
                gps = PS[6]
                gs = gsb[cq["g"] % 2]
                cq["g"] += 1
                S.mm((gps, gps.ap[:, 0:n]), [((self.ident_f, self.ident_f.ap[0:NE, e:e + 1].to_broadcast([NE, 128])), (GT, GT.ap[0:NE, c0:c0 + n]))])
                S.copy("act", (gs, gs.ap[:, 0:n]), (gps, gps.ap[:, 0:n]))
                at = actT[cq["a"] % 2]
                cq["a"] += 1
                for jj in range(4):
                    j = hh * 4 + jj
                    q = cq["q"] % 2
                    cq["q"] += 1
                    pg, pu = PS[2 * q], PS[2 * q + 1]
                    S.mm((pg, pg.ap[:, 0:n]), [((Wg[b][kc], Wg[b][kc].ap[:, jj * 128:(jj + 1) * 128]), hT2b[kc][bi]) for kc in range(8)])
                    S.mm((pu, pu.ap[:, 0:n]), [((Wu[b][kc], Wu[b][kc].ap[:, jj * 128:(jj + 1) * 128]), hT2b[kc][bi]) for kc in range(8)])
                    G1, U1, SG = g1[q], u1[q], sg[q]
                    S.ts("dve", (G1, G1.ap[:, 0:n]), (pg, pg.ap[:, 0:n]), (bgu, bgu.ap[:, e * 16 + j:e * 16 + j + 1]), 7.0, ALU.add, ALU.min)
                    S.act((SG, SG.ap[:, 0:n]), (G1, G1.ap[:, 0:n]), ACTF.Sigmoid, scale=1.702)
                    S.ts("dve", (U1, U1.ap[:, 0:n]), (pu, pu.ap[:, 0:n]), (bgu, bgu.ap[:, e * 16 + 8 + j:e * 16 + 8 + j + 1]), 7.0, ALU.add, ALU.min)
                    S.ts("pool", (U1, U1.ap[:, 0:n]), (U1, U1.ap[:, 0:n]), -7.0, 1.0, ALU.max, ALU.add)
                    S.tt("pool", (G1, G1.ap[:, 0:n]), (G1, G1.ap[:, 0:n]), (SG, SG.ap[:, 0:n]), ALU.mult)
                    S.tt("pool", (G1, G1.ap[:, 0:n]), (G1, G1.ap[:, 0:n]), (U1, U1.ap[:, 0:n]), ALU.mult)
                    S.tt("dve", (at, at.ap[:, jj, 0:n]), (G1, G1.ap[:, 0:n]), (gs, gs.ap[:, 0:n]), ALU.mult)
                for dc in range(8):
                    py = PS[4 + cq["y"] % 2]
                    cq["y"] += 1
                    pairs = [((Wdn[b][jj], Wdn[b][jj].ap[:, dc * 128:(dc + 1) * 128]), (at, at.ap[:, jj, 0:n])) for jj in range(4)]
                    if it == 0:
                        pairs.append(((bd, bd.ap[0:NE, dc * 128:(dc + 1) * 128]), (GT, GT.ap[0:NE, c0:c0 + n])))
                    S.mm((py, py.ap[:, 0:n]), pairs)
                    xt = self.xT[dc][bi]
                    S.stt("dve", xt, (py, py.ap[:, 0:n]), (gpT, gpA[:, dc:dc + 1]), xt, ALU.mult, ALU.add)
        self.dbg_out(f"z{l}1", self.xall, self.xall.ap, [128, 8, NCOL])
        S.barrier()
        W.release(m0)
        self.layernorm(l, 1, None if last else hT2b)
        if not last:
            self.dbg_out(f"h1in", hT2, hT2.ap, [128, 8, NCOL], BF16)

    def layer1_proj(self, q1, kT_own, vc_own, vd_own):
        cfg, S, W, DT, PS = self.cfg, self.S, self.work, self.DT, self.PS
        NOWN = cfg.NOWN
        m0 = W.mark()
        Win = W.alloc([8, 2304], BF16, "Win1")
        csb = [W.alloc([2, 512], F32, f"csb{i}") for i in range(2)]
        Rm = W.alloc([128], BF16, "Rm")
        blk1 = W.alloc([128], BF16, "blk1")
        qkng = W.alloc([2], F32, "qkng")
        qs = [W.alloc([512], BF16, f"qs{i}") for i in range(2)]
        sq = W.alloc([512], BF16, "sq")
        rs = W.alloc([512], F32, "rs")
        qn = [W.alloc([512], BF16, f"qn{i}") for i in range(2)]
        qst = [W.alloc([512], BF16, f"qst{i}") for i in range(3)]
        t1 = [W.alloc([512], F32, f"t1{i}") for i in range(2)]
        t2 = [W.alloc([512], F32, f"t2{i}") for i in range(2)]
        vcs = [W.alloc([4, 129], BF16, f"vcs{i}") for i in range(2)]
        vds = [W.alloc([2, 65], BF16, f"vds{i}") for i in range(2)]
        self.load_w_bf16(Win, "w_in1", self.din["w_in1"], 2304)
        S.dma("pool", Rm, DT["Rm"]); S.dma("pool", blk1, DT["blk1"])
        S.dma("sp", qkng, DT["qkng"])
        for i in range(2):
            S.memset("pool", (vcs[i], vcs[i].ap[:, :, 128]), 1.0)
            S.memset("pool", (vds[i], vds[i].ap[:, :, 64]), 1.0)
        Q1 = [T(q1[i], f"q1_{i}") for i in range(8)]
        KTO = [T(kT_own[i], f"kTo_{i}") for i in range(5)]
        VCO, VDO = T(vc_own, "vco"), T(vd_own, "vdo")
        cnt = {}

        def nxt(k, m=2):
            v = cnt.get(k, 0)
            cnt[k] = v + 1
            return v % m

        for bi, (c0, n, v) in enumerate(cfg.blocks):
            own = (v == 0)
            k0 = c0
            if own:
                ob = bi - 1
                cb_ = csb[nxt("cs")]
                S.dma("sp", (cb_, cb_.ap[:, 0, :]), (DT["cos_own"], self.din["cos_own"][:, ob * 512:(ob + 1) * 512]))
                S.dma("act", (cb_, cb_.ap[:, 1, :]), (DT["sin_own"], self.din["sin_own"][:, ob * 512:(ob + 1) * 512]))

            def proj_fm(col0):
                ps = PS[nxt("pp")]
                S.mm((ps, ps.ap[:, 0:n]), [((Win, Win.ap[:, kc, col0:col0 + 128]), self.hT2b[kc][bi]) for kc in range(8)])
                return ps

            def finish(ps, dst_T, dst_ap, norm_col, rope):
                q_ = qs[nxt("q")]
                S.copy("act", (q_, q_.ap[:, 0:n]), (ps, ps.ap[:, 0:n]))
                cur = q_
                if norm_col is not None:
                    S.act((sq, sq.ap[:, 0:n]), (ps, ps.ap[:, 0:n]), ACTF.Square)
                    pn = PS[2 + nxt("pr")]
                    S.mm((pn, pn.ap[:, 0:n]), [(blk1, (sq, sq.ap[:, 0:n]))])
                    S.ts("dve", (rs, rs.ap[:, 0:n]), (pn, pn.ap[:, 0:n]), 1.0 / 64, RMS_EPS, ALU.mult, ALU.add)
                    S.act((rs, rs.ap[:, 0:n]), (rs, rs.ap[:, 0:n]), ACTF.Sqrt)
                    S.recip((rs, rs.ap[:, 0:n]), (rs, rs.ap[:, 0:n]))
                    qn_ = qn[nxt("qn")]
                    S.stt("dve", (qn_, qn_.ap[:, 0:n]), (q_, q_.ap[:, 0:n]), (qkng, qkng.ap[:, norm_col:norm_col + 1]),
                          (rs, rs.ap[:, 0:n]), ALU.mult, ALU.mult)
                    cur = qn_
                if rope:
                    pr = PS[2 + nxt("pr")]
                    S.mm((pr, pr.ap[:, 0:n]), [(Rm, (cur, cur.ap[:, 0:n]))])
                    i_ = nxt("a")
                    a1, a2 = t1[i_], t2[i_]
                    st_ = qst[nxt("qst", 3)]
                    S.tt("pool", (a1, a1.ap[:, 0:n]), (cur, cur.ap[:, 0:n]), (cb_, cb_.ap[:, 0, 0:n]), ALU.mult)
                    S.tt("dve", (a2, a2.ap[:, 0:n]), (pr, pr.ap[:, 0:n]), (cb_, cb_.ap[:, 1, 0:n]), ALU.mult)
                    S.tt("pool", (st_, st_.ap[:, 0:n]), (a1, a1.ap[:, 0:n]), (a2, a2.ap[:, 0:n]), ALU.add)
                    cur = st_
                S.dma("sp", (dst_T, dst_ap), (cur, cur.ap[:, 0:n]))

            if own:
                for ti in range(4):
                    finish(proj_fm(ti * 128), Q1[ti], q1[ti][:, ob * 512:ob * 512 + n], None, True)
                for ti in range(4):
                    finish(proj_fm(512 + ti * 128), Q1[4 + ti], q1[4 + ti][:, ob * 512:ob * 512 + n], 0, True)
            for ti in range(4):
                finish(proj_fm(1024 + ti * 128), KTO[ti], kT_own[ti][:, k0:k0 + n], None, own)
            finish(proj_fm(1536), KTO[4], kT_own[4][:, k0:k0 + n], 1, own)
            for tt_ in range(n // 128):
                hsl = [(self.hT2b[kc][bi], self.hT2b[kc][bi].ap[:, tt_ * 128:(tt_ + 1) * 128]) for kc in range(8)]
                pv, pd = PS[4 + nxt("pv")], PS[6 + nxt("pd")]
                S.mm(pv, [(hsl[kc], (Win, Win.ap[:, kc, 1664:2176])) for kc in range(8)])
                S.mm((pd, pd.ap[:, 0:128]), [(hsl[kc], (Win, Win.ap[:, kc, 2176:2304])) for kc in range(8)])
                vc_, vd_ = vcs[nxt("vc")], vds[nxt("vd")]
                S.copy("act", (vc_, vc_.ap[:, :, 0:128]), (pv, pv.ap.rearrange("p (h d) -> p h d", h=4)))
                S.copy("dve", (vd_, vd_.ap[:, :, 0:64]), (pd, pd.ap[:, 0:128].rearrange("p (h d) -> p h d", h=2)))
                r0 = k0 + tt_ * 128
                S.dma("sp", (VCO, vc_own[:, r0:r0 + 128, :].rearrange("h p d -> p h d")), vc_)
                S.dma("act", (VDO, vd_own[:, r0:r0 + 128, :].rearrange("h p d -> p h d")), vd_)
        S.barrier()
        W.release(m0)

    def layer1_attn(self, q1, kT_all, vc_all, vd_all):
        cfg, S, W, DT, PS = self.cfg, self.S, self.work, self.DT, self.PS
        NOWN = cfg.NOWN
        NK = NCTX + 2 * NOWN
        NKC = NK // 128
        l = 1
        m0 = W.mark()
        Q1 = [T(q1[i], f"q1r_{i}") for i in range(8)]
        KTA = [T(kT_all[i], f"kTa_{i}") for i in range(5)]
        VCA = [T(vc_all[i], f"vca_{i}") for i in range(4)]
        VDA = [T(vd_all[i], f"vda_{i}") for i in range(2)]
        Wo = W.alloc([8, D], BF16, "Wo1")
        self.load_w_bf16(Wo, "w_out1", self.din["w_out1"], D)
        Kt = [W.alloc([NK], BF16, f"Kt{i}") for i in range(2)]
        Vt = [W.alloc([NKC, 129], BF16, f"Vt{i}") for i in range(2)]
        Qt = [W.alloc([4, 512], BF16, f"Qt{i}") for i in range(2)]
        PT = [W.alloc([512], BF16, f"PT{i}") for i in range(4)]
        o_tok = W.alloc([4, D], BF16, "otok1")
        oTb = W.alloc([8, 512], BF16, "oTb1")
        lamt = W.alloc([256], F32, "lamt")
        lam = W.alloc([8], F32, "lam")
        sgb = W.alloc([128], F32, "sgb")
        oa = [W.alloc([128], F32, f"oa{i}") for i in range(2)]
        ob_ = [W.alloc([128], F32, f"ob{i}") for i in range(2)]
        rz = [W.alloc([8], F32, f"rz{i}") for i in range(2)]
        lam_init = 0.8 - 0.6 * math.exp(-0.3 * 1)
        S.dma("sp", lamt, (DT["lam"], self.din["lam"].partition_broadcast(128)))
        S.dma("sp", sgb, (DT["subln"], self.din["subln"].partition_broadcast(128)))
        S.tt("dve", (lamt, lamt.ap[:, 0:64]), (lamt, lamt.ap[:, 0:64]), (lamt, lamt.ap[:, 64:128]), ALU.mult)
        S.tt("dve", (lamt, lamt.ap[:, 128:192]), (lamt, lamt.ap[:, 128:192]), (lamt, lamt.ap[:, 192:256]), ALU.mult)
        S.op("dve", lambda E: E.reduce_sum(out=lam.ap[:, 0:1], in_=lamt.ap[:, 0:64], axis=AX.X), [lamt], [lam])
        S.op("dve", lambda E: E.reduce_sum(out=lam.ap[:, 1:2], in_=lamt.ap[:, 128:192], axis=AX.X), [lamt], [lam])
        S.act((lam, lam.ap[:, 0:2]), (lam, lam.ap[:, 0:2]), ACTF.Exp)
        S.tt("dve", (lam, lam.ap[:, 2:3]), (lam, lam.ap[:, 0:1]), (lam, lam.ap[:, 1:2]), ALU.subtract)
        S.ts("dve", (lam, lam.ap[:, 3:4]), (lam, lam.ap[:, 2:3]), lam_init, -1.0, ALU.add, ALU.mult)
        S.ts("dve", sgb, sgb, 1.0 - lam_init, None, ALU.mult)
        psT = T(PS[7].ap.bitcast(BF16), "psT1")
        cq = {}

        def nxt(k, m=2):
            v = cq.get(k, 0)
            cq[k] = v + 1
            return v % m

        for qb in range(cfg.NB):
            bi = 1 + qb
            units = [("C", h) for h in range(4)] + [("D", g) for g in range(2)]
            for (ut, ui) in units:
                kt, vt, qt = Kt[nxt("kt")], Vt[nxt("vt")], Qt[nxt("qt")]
                if ut == "C":
                    S.dma("sp", kt, KTA[ui])
                    S.dma("act", vt, (VCA[ui], vc_all[ui].rearrange("(c p) d -> p c d", p=128)))
                    S.dma("sp", (qt, qt.ap[:, 0, :]), (Q1[ui], q1[ui][:, qb * 512:(qb + 1) * 512]))
                    maps = [(slice(64 * m, 64 * m + 64), 0) for m in range(2)]
                    dv = 128
                else:
                    S.dma("sp", kt, KTA[4])
                    S.dma("act", (vt, vt.ap[:, :, 0:65]), (VDA[ui], vd_all[ui].rearrange("(c p) d -> p c d", p=128)))
                    for j in range(4):
                        S.dma("sp" if j % 2 == 0 else "act", (qt, qt.ap[:, j, :]), (Q1[4 + j], q1[4 + j][:, qb * 512:(qb + 1) * 512]))
                    maps = [(slice(64 * ui, 64 * ui + 64), j) for j in range(4)]
                    dv = 64
                nm = len(maps)
                w = dv + 1
                per_bank = 512 // w
                def acc(mi, st):
                    i = mi * 4 + st
                    return PS[4 + i // per_bank], (i % per_bank) * w
                for kc in range(NKC):
                    pts = []
                    for mi, (rows, qi) in enumerate(maps):
                        ps = PS[(nxt("sc", 4) if nm == 2 else mi)]
                        S.mm(ps, [((kt, kt.ap[rows, kc * 128:(kc + 1) * 128]), (qt, qt.ap[rows, qi, :]))])
                        pt = PT[nxt("pt", 4)]
                        S.act(pt, ps, ACTF.Exp, scale=0.125)
                        pts.append(pt)
                    for mi in range(nm):
                        for st in range(4):
                            pb, off = acc(mi, st)
                            S.mm((pb, pb.ap[:, off:off + w]), [((pts[mi], pts[mi].ap[:, st * 128:(st + 1) * 128]), (vt, vt.ap[:, kc, 0:w]))],
                                 start=(kc == 0), stop=(kc == NKC - 1))
                for st in range(4):
                    if ut == "C":
                        (p1, o1), (p2, o2) = acc(0, st), acc(1, st)
                        r = rz[nxt("rz")]
                        S.recip((r, r.ap[:, 0:1]), (p1, p1.ap[:, o1 + 128:o1 + 129]))
                        S.recip((r, r.ap[:, 1:2]), (p2, p2.ap[:, o2 + 128:o2 + 129]))
                        S.tt("dve", (r, r.ap[:, 1:2]), (r, r.ap[:, 1:2]), (lam, lam.ap[:, 3:4]), ALU.mult)
                        a_, b_ = oa[nxt("oa")], ob_[nxt("ob")]
                        S.ts("dve", a_, (p1, p1.ap[:, o1:o1 + 128]), (r, r.ap[:, 0:1]), None, ALU.mult)
                        S.stt("dve", a_, (p2, p2.ap[:, o2:o2 + 128]), (r, r.ap[:, 1:2]), a_, ALU.mult, ALU.add)
                        S.act(b_, a_, ACTF.Square, accum_out=(r, r.ap[:, 2:3]))
                        S.ts("dve", (r, r.ap[:, 2:3]), (r, r.ap[:, 2:3]), 1.0 / 128, RMS_EPS, ALU.mult, ALU.add)
                        S.act((r, r.ap[:, 2:3]), (r, r.ap[:, 2:3]), ACTF.Sqrt)
                        S.recip((r, r.ap[:, 2:3]), (r, r.ap[:, 2:3]))
                        S.stt("dve", (o_tok, o_tok.ap[:, st, ui * 128:(ui + 1) * 128]), a_, (r, r.ap[:, 2:3]), sgb, ALU.mult, ALU.mult)
                    else:
                        for j in range(4):
                            pb, off = acc(j, st)
                            r = rz[nxt("rz")]
                            S.recip((r, r.ap[:, 0:1]), (pb, pb.ap[:, off + 64:off + 65]))
                            hq = 4 * ui + j
                            S.ts("dve", (o_tok, o_tok.ap[:, st, 512 + 64 * hq:512 + 64 * hq + 64]), (pb, pb.ap[:, off:off + 64]),
                                 (r, r.ap[:, 0:1]), None, ALU.mult)
            for st in range(4):
                for c in range(8):
                    S.transpose((psT, psT.ap[:, c * 128:(c + 1) * 128]), (o_tok, o_tok.ap[:, st, c * 128:(c + 1) * 128]), self.ident_b)
                S.copy("act", (oTb, oTb.ap[:, :, st * 128:(st + 1) * 128]), (psT, psT.ap.rearrange("p (c q) -> p c q", c=8)))
            c0, n, v = cfg.blocks[bi]
            for dc in range(8):
                ps = PS[nxt("op")]
                S.mm(ps, [((Wo, Wo.ap[:, oc, dc * 128:(dc + 1) * 128]), (oTb, oTb.ap[:, oc, :])) for oc in range(8)])
                gT_, gA = self.sc(("gp", l, 0, 0))
                xt = self.xT[dc][bi]
                S.stt("dve", xt, ps, (gT_, gA[:, dc:dc + 1]), xt, ALU.mult, ALU.add)
        self.dbg_out("z10", self.xall, self.xall.ap, [128, 8, cfg.NCOL])
        S.barrier()
        W.release(m0)
        self.layernorm(l, 0, self.hT2b, skip_ctx=True)


BF = ml_dtypes.bfloat16
D = 1024
NCTX = 256
Q0_PERM = [0, 3, 1, 4, 2, 5, 6, 9, 7, 10, 8, 11]
QD_PERM = [0, 4, 1, 5, 2, 6, 3, 7]


def rope_tables(S):
    t = np.arange(S)
    row = (t // 64).astype(np.float32)
    col = (t % 64).astype(np.float32)
    nf = 16
    inv = (np.float32(10000.0) ** (-np.arange(nf, dtype=np.float32) / nf)).astype(np.float32)
    ar = row[:, None] * inv[None, :]
    ac = col[:, None] * inv[None, :]
    ang = np.concatenate([ar, ar, ac, ac], axis=-1).astype(np.float32)
    return np.cos(ang).astype(np.float32), np.sin(ang).astype(np.float32)


def consts(cfg):
    S = cfg.S
    c = {}
    Rm = np.zeros((128, 128), np.float32)
    for h in range(2):
        o = 64 * h
        for m in range(64):
            if m < 16:
                Rm[o + m + 16, o + m] = -1
            elif m < 32:
                Rm[o + m - 16, o + m] = 1
            elif m < 48:
                Rm[o + m + 16, o + m] = -1
            else:
                Rm[o + m - 16, o + m] = 1
    c["Rm"] = Rm
    kl = np.arange(128)[:, None]
    ql = np.arange(128)[None, :]
    lo = (kl >= ql).astype(np.float32)
    hi = (kl <= ql).astype(np.float32)
    c["masks"] = np.concatenate([lo, lo, lo, hi, hi, hi], axis=1)
    cc = np.arange(64)
    m = (cc[:, None] * cc[None, :]) % 64
    C64 = np.cos(2 * np.pi * m / 64)
    S64 = np.sin(2 * np.pi * m / 64)
    Z = np.zeros((64, 64))
    Cc2 = np.block([[C64, Z], [Z, C64]])
    Sc2 = np.block([[S64, Z], [Z, S64]])
    c["CS2"] = np.concatenate([Cc2, Sc2], axis=1).astype(np.float32)
    b1 = np.zeros((128, 128), np.float32)
    b1[:64, :64] = 1
    b1[64:, 64:] = 1
    c["blk1"] = b1
    t = np.arange(NCTX)
    mm = (t[:, None] * t[None, :]) % NCTX
    nrm = 1.0 / np.sqrt(NCTX * 64.0)
    Cc = (np.cos(2 * np.pi * mm / NCTX) * nrm)
    Sc = (-np.sin(2 * np.pi * mm / NCTX) * nrm)
    c["tabCc"] = np.ascontiguousarray(Cc.reshape(2, 128, NCTX).transpose(1, 0, 2)).astype(BF)
    c["tabSc"] = np.ascontiguousarray(Sc.reshape(2, 128, NCTX).transpose(1, 0, 2)).astype(BF)
    return c


def core_consts(cfg, s):
    S, NOWN = cfg.S, cfg.NOWN
    c = {}
    cos, sin = rope_tables(S)
    own0 = s * NOWN
    def fm(tab, pos):
        valid = (pos >= 0) & (pos < S)
        p = np.clip(pos, 0, S - 1)
        a = tab[p].T * valid[None, :]
        return np.ascontiguousarray(np.concatenate([a, a], axis=0)).astype(np.float32)
    pos_own = np.arange(own0, own0 + NOWN)
    pos_halo = np.concatenate([np.arange(own0 - 128, own0), np.arange(own0 + NOWN, own0 + NOWN + 128)])
    c["cos_own"], c["sin_own"] = fm(cos, pos_own), fm(sin, pos_own)
    c["cos_halo"], c["sin_halo"] = fm(cos, pos_halo), fm(sin, pos_halo)
    vh = np.zeros((128, 2), np.float32)
    vh[:, 0] = 1.0 if own0 - 128 >= 0 else 0.0
    vh[:, 1] = 1.0 if own0 + NOWN + 128 <= S else 0.0
    c["valid_halo"] = vh
    par0 = (1 - s) * NOWN
    pos_all = np.concatenate([pos_own, np.arange(par0, par0 + NOWN)])
    nrm = 1.0 / np.sqrt(S * 64.0)
    tabC = np.empty((cfg.NB, 128, cfg.NT_ALL, 512), BF)
    tabS = np.empty((cfg.NB, 128, cfg.NT_ALL, 512), BF)
    for b in range(cfg.NB):
        tp = own0 + b * 512 + np.arange(512)
        mm = (pos_all[:, None].astype(np.int64) * tp[None, :]) % S
        ang = (2 * np.pi / S) * mm
        Cm = (np.cos(ang) * nrm).astype(np.float32).reshape(cfg.NT_ALL, 128, 512).transpose(1, 0, 2)
        Sm = (-np.sin(ang) * nrm).astype(np.float32).reshape(cfg.NT_ALL, 128, 512).transpose(1, 0, 2)
        tabC[b] = Cm.astype(BF)
        tabS[b] = Sm.astype(BF)
    c["tabC"], c["tabS"] = tabC, tabS
    return c


def pl(vec):
    v = np.asarray(vec, np.float32)
    lead = v.shape[:-1]
    n = v.shape[-1] // 128
    v = v.reshape(*lead, n, 128)
    v = np.moveaxis(v, -1, 0)
    return np.ascontiguousarray(v.reshape(128, -1))


def prep_inputs(inp, cfg, n_batch):
    L, NE, NOWN, S = cfg.L, cfg.NE, cfg.NOWN, cfg.S
    f32 = np.float32
    x = np.asarray(inp["x"], f32)
    ctx = np.asarray(inp["ctx"], f32)
    c = np.asarray(inp["c"], f32)
    c_ctx = np.asarray(inp["c_ctx"], f32)
    shared = {}
    shared["mod_w"] = np.ascontiguousarray(np.asarray(inp["mod_w"], f32))
    shared["mod_bT"] = pl(np.asarray(inp["mod_b"], f32))
    lnT = np.zeros((128, L * 32), f32)
    for l in range(L):
        for i in range(2):
            for gb, arr in enumerate((inp["ln_g"], inp["ln_b"])):
                o = ((l * 2 + i) * 2 + gb) * 8
                lnT[:, o:o + 8] = np.asarray(arr, f32)[l, i].reshape(8, 128).T
    shared["lnT"] = lnT
    w0 = np.asarray(inp["ab_w_in"], f32)[0]
    qcols = np.concatenate([256 + 64 * h + np.arange(64) for h in Q0_PERM])
    shared["w_in0"] = np.ascontiguousarray(np.concatenate([w0[:, :256], w0[:, qcols], w0[:, 1024:]], axis=1))
    shared["w_out0"] = np.ascontiguousarray(np.asarray(inp["ab_w_out"], f32)[0])
    shared["sink"] = np.asarray(inp["ab_sink"], f32)[0].reshape(1, 12)
    w1 = np.asarray(inp["cd_w_in"], f32)[0]
    qd = np.concatenate([512 + 64 * h + np.arange(64) for h in QD_PERM])
    shared["w_in1"] = np.ascontiguousarray(np.concatenate([w1[:, :512], w1[:, qd], w1[:, 1024:]], axis=1))
    shared["w_out1"] = np.ascontiguousarray(np.asarray(inp["cd_w_out"], f32)[0])
    shared["lam"] = np.asarray(inp["cd_lambda"], f32)[0].reshape(1, 256)
    shared["subln"] = np.asarray(inp["cd_subln_g"], f32)[0].reshape(1, 128)
    qn = np.asarray(inp["cd_q_norm_g"], f32)[0]
    kn = np.asarray(inp["cd_k_norm_g"], f32)[0]
    shared["qkng"] = np.ascontiguousarray(np.stack([np.concatenate([qn, qn]), np.concatenate([kn, kn])], axis=1))
    shared["router_w"] = np.ascontiguousarray(np.asarray(inp["router_w"], f32))
    shared["router_b"] = np.asarray(inp["router_b"], f32).reshape(L, 1, NE)
    wgu_ = np.asarray(inp["expert_w_gu"], f32)
    shared["e_wgu0"], shared["e_wgu1"] = wgu_[0], wgu_[1]
    bgu = np.asarray(inp["expert_b_gu"], f32)
    shared["e_bguT"] = np.ascontiguousarray(bgu.reshape(L * NE * 16, 128).T)
    wd_ = np.asarray(inp["expert_w_down"], f32)
    shared["e_wd0"], shared["e_wd1"] = wd_[0], wd_[1]
    shared["e_bd"] = np.asarray(inp["expert_b_down"], f32)
    shared.update(consts(cfg))
    cc = [core_consts(cfg, s) for s in range(2)]
    maps = []
    for b in range(n_batch):
        for s in range(2):
            m = dict(shared)
            own0 = s * NOWN
            par0 = (1 - s) * NOWN
            m["xT_own"] = np.ascontiguousarray(x[b, own0:own0 + NOWN].T)
            m["xT_par"] = np.ascontiguousarray(x[b, par0:par0 + NOWN].T)
            halo = np.zeros((256, D), f32)
            if own0 - 128 >= 0:
                halo[:128] = x[b, own0 - 128:own0]
            if own0 + NOWN + 128 <= S:
                halo[128:] = x[b, own0 + NOWN:own0 + NOWN + 128]
            m["xT_halo"] = np.ascontiguousarray(halo.T)
            m["cT"] = np.ascontiguousarray(ctx[b].T)
            cv = np.zeros((128, 16), f32)
            cv[:, 0:8] = c[b].reshape(8, 128).T
            cv[:, 8:16] = c_ctx.reshape(8, 128).T
            m["cvec"] = cv
            m.update(cc[s])
            maps.append(m)
    return maps


def glue_b(maps_a, res_a, cfg, n_batch):
    NOWN = cfg.NOWN
    maps = []
    for b in range(n_batch):
        for s in range(2):
            me, par = res_a[2 * b + s], res_a[2 * b + (1 - s)]
            m = dict(maps_a[2 * b + s])
            m["xstate"] = me["xstate"]
            m["q1"] = me["q1"]
            m["kT_all"] = np.concatenate([me["kT_own"], par["kT_own"][:, :, 256:]], axis=2)
            m["vc_all"] = np.concatenate([me["vc_own"], par["vc_own"][:, 256:]], axis=1)
            m["vd_all"] = np.concatenate([me["vd_own"], par["vd_own"][:, 256:]], axis=1)
            maps.append(m)
    return maps


def select(m, names):
    return {k: m[k] for k in names}


def kernel(**inputs):
    cfg = Cfg(S=4096, NE=32, L=2)
    inp = {k: np.asarray(v) for k, v in inputs.items()}
    nb = inp["x"].shape[0]
    ncores = 2 * nb
    maps = prep_inputs(inp, cfg, nb)
    ncA = Prog(cfg, mode="A").build()
    namesA = list(dram_inputs(cfg, "A").keys())
    resA = run_bass_kernel_spmd(ncA, [select(m, namesA) for m in maps], core_ids=list(range(ncores)))
    mapsB = glue_b(maps, resA.results, cfg, nb)
    del resA
    ncB = Prog(cfg, mode="B").build()
    namesB = list(dram_inputs(cfg, "B").keys())
    resB = run_bass_kernel_spmd(ncB, [select(m, namesB) for m in mapsB], core_ids=list(range(ncores)))
    NOWN = cfg.NOWN
    out = np.empty((nb, cfg.S, D), np.float32)
    for b in range(nb):
        for s in range(2):
            o = np.asarray(resB.results[2 * b + s]["outT"])
            out[b, s * NOWN:(s + 1) * NOWN] = o.transpose(2, 1, 0).reshape(NOWN, D)
    return out
```

```python
import numpy as np
import ml_dtypes
from contextlib import ExitStack
import concourse.bass as bass
import concourse.mybir as mybir
from concourse.bass_utils import run_bass_kernel_spmd

F32 = mybir.dt.float32
BF16 = mybir.dt.bfloat16
ALU = mybir.AluOpType
ACTF = mybir.ActivationFunctionType
AX = mybir.AxisListType

ENGS = ["sp", "act", "dve", "pool", "pe"]
DQ = ("sp", "act", "pool")
NDSEM = 8


class T:
    __slots__ = ("ap", "w", "r", "name")

    def __init__(self, ap, name=""):
        self.ap = ap
        self.w = None
        self.r = []
        self.name = name


def _ta(x):
    if isinstance(x, T):
        return x, x.ap
    if isinstance(x, tuple):
        return x
    return None, x


def _key(ev):
    return ev[:2] if ev[0] == "e" else ev[:3]


class Sched:
    def __init__(self, nc, stack):
        self.nc = nc
        self.q = {e: [] for e in ENGS}
        self.cnt = {e: 0 for e in ENGS}
        self.sem = {e: stack.enter_context(nc.semaphore(f"s_{e}")) for e in ENGS}
        self.dsem = {e: [stack.enter_context(nc.semaphore(f"d_{e}{k}")) for k in range(NDSEM)] for e in DQ}
        self.dcnt = {e: [0] * NDSEM for e in DQ}
        self.dnext = {e: 0 for e in DQ}
        self.seen = {e: {} for e in ENGS}
        self.ninstr = 0

    def _wait(self, eng, ev):
        if ev[0] == "e":
            _, f, n = ev
            if f == eng and eng == "pe":
                return
            key, val, sem = ("e", f), n, self.sem[f]
        else:
            _, f, k, n = ev
            key, val, sem = ("d", f, k), 16 * n, self.dsem[f][k]
        if self.seen[eng].get(key, 0) >= val:
            return
        self.seen[eng][key] = val
        self.q[eng].append(lambda E, sem=sem, val=val: E.wait_ge(sem, val))
        self.ninstr += 1

    def _deps(self, eng, reads, writes):
        best = {}
        for t in reads:
            if t.w is not None:
                k = _key(t.w)
                if k not in best or best[k][-1] < t.w[-1]:
                    best[k] = t.w
        for t in writes:
            for ev in ([t.w] if t.w is not None else []) + t.r:
                k = _key(ev)
                if k not in best or best[k][-1] < ev[-1]:
                    best[k] = ev
        for ev in best.values():
            self._wait(eng, ev)

    def _commit(self, ev, reads, writes):
        for t in reads:
            t.r.append(ev)
            if len(t.r) > 16:
                best = {}
                for e2 in t.r:
                    k = _key(e2)
                    if k not in best or best[k][-1] < e2[-1]:
                        best[k] = e2
                t.r = list(best.values())
        for t in writes:
            t.w = ev
            t.r = []

    def op(self, eng, fns, reads=(), writes=()):
        if callable(fns):
            fns = [fns]
        reads = [t for t in reads if t is not None]
        writes = [t for t in writes if t is not None]
        self._deps(eng, reads, writes)
        self.cnt[eng] += 1
        n = self.cnt[eng]
        sem = self.sem[eng]
        last = len(fns) - 1
        for i, fn in enumerate(fns):
            if i == last:
                self.q[eng].append(lambda E, fn=fn, sem=sem: fn(E).then_inc(sem, 1))
            else:
                self.q[eng].append(lambda E, fn=fn: fn(E))
            self.ninstr += 1
        ev = ("e", eng, n)
        self._commit(ev, reads, writes)
        return ev

    def dma(self, eng, out, in_):
        ot, oa = _ta(out)
        it, ia = _ta(in_)
        k = self.dnext[eng]
        self.dnext[eng] = (k + 1) % NDSEM
        if self.dcnt[eng][k] > 0:
            self._wait(eng, ("d", eng, k, self.dcnt[eng][k]))
        reads = [t for t in [it] if t is not None]
        writes = [t for t in [ot] if t is not None]
        self._deps(eng, reads, writes)
        self.dcnt[eng][k] += 1
        n = self.dcnt[eng][k]
        sem = self.dsem[eng][k]
        self.q[eng].append(lambda E, oa=oa, ia=ia, sem=sem: E.dma_start(out=oa, in_=ia).then_inc(sem, 16))
        self.ninstr += 1
        ev = ("d", eng, k, n)
        self._commit(ev, reads, writes)
        return ev

    def _all_events(self):
        evs = [("e", f, self.cnt[f]) for f in ENGS if self.cnt[f] > 0]
        for f in DQ:
            for k in range(NDSEM):
                if self.dcnt[f][k] > 0:
                    evs.append(("d", f, k, self.dcnt[f][k]))
        return evs

    def barrier(self):
        evs = self._all_events()
        for e in ENGS:
            for ev in evs:
                self._wait(e, ev)

    def wait_all_on(self, eng):
        for ev in self._all_events():
            self._wait(eng, ev)

    def mm(self, out, pairs, start=True, stop=True, extra_reads=()):
        ot, oa = _ta(out)
        reads = list(extra_reads)
        fns = []
        n = len(pairs)
        for i, (l, r) in enumerate(pairs):
            lt, la = _ta(l)
            rt, ra = _ta(r)
            reads += [lt, rt]
            fns.append(lambda E, la=la, ra=ra, st=(start and i == 0), sp=(stop and i == n - 1):
                       E.matmul(oa, la, ra, start=st, stop=sp))
        return self.op("pe", fns, reads, [ot])

    def transpose(self, out, in_, ident):
        ot, oa = _ta(out)
        it, ia = _ta(in_)
        dt, da = _ta(ident)
        return self.op("pe", lambda E: E.transpose(oa, ia, da), [it, dt], [ot])

    def act(self, out, in_, func, bias=0.0, scale=1.0, accum_out=None, eng="act"):
        ot, oa = _ta(out)
        it, ia = _ta(in_)
        bt, ba = _ta(bias)
        st_, sa = _ta(scale)
        at, aa = _ta(accum_out) if accum_out is not None else (None, None)
        if aa is None:
            fn = lambda E: E.activation(out=oa, in_=ia, func=func, bias=ba, scale=sa)
        else:
            fn = lambda E: E.activation(out=oa, in_=ia, func=func, bias=ba, scale=sa, accum_out=aa)
        return self.op("act", fn, [it, bt, st_], [ot, at])

    def tt(self, eng, out, in0, in1, op):
        ot, oa = _ta(out)
        at, aa = _ta(in0)
        bt, ba = _ta(in1)
        return self.op(eng, lambda E: E.tensor_tensor(out=oa, in0=aa, in1=ba, op=op), [at, bt], [ot])

    def ts(self, eng, out, in0, s1, s2, op0, op1=None):
        ot, oa = _ta(out)
        at, aa = _ta(in0)
        t1, a1 = _ta(s1)
        t2, a2 = _ta(s2)
        if op1 is None:
            fn = lambda E: E.tensor_scalar(out=oa, in0=aa, scalar1=a1, scalar2=None, op0=op0)
        else:
            fn = lambda E: E.tensor_scalar(out=oa, in0=aa, scalar1=a1, scalar2=a2, op0=op0, op1=op1)
        return self.op(eng, fn, [at, t1, t2], [ot])

    def stt(self, eng, out, in0, scalar, in1, op0, op1):
        ot, oa = _ta(out)
        at, aa = _ta(in0)
        st_, sa = _ta(scalar)
        bt, ba = _ta(in1)
        return self.op(eng, lambda E: E.scalar_tensor_tensor(out=oa, in0=aa, scalar=sa, in1=ba, op0=op0, op1=op1),
                       [at, st_, bt], [ot])

    def copy(self, eng, out, in_):
        ot, oa = _ta(out)
        it, ia = _ta(in_)
        if eng == "act":
            return self.op("act", lambda E: E.activation(out=oa, in_=ia, func=ACTF.Copy), [it], [ot])
        return self.op(eng, lambda E: E.tensor_copy(out=oa, in_=ia), [it], [ot])

    def recip(self, out, in_):
        ot, oa = _ta(out)
        it, ia = _ta(in_)
        return self.op("dve", lambda E: E.reciprocal(out=oa, in_=ia), [it], [ot])

    def memset(self, eng, out, val):
        ot, oa = _ta(out)
        return self.op(eng, lambda E: E.memset(oa, val), [], [ot])

    def emit(self):
        nc = self.nc
        q = self.q
        with nc.Block() as block:
            @block.sync
            def _(E):
                for f in q["sp"]:
                    f(E)

            @block.scalar
            def _(E):
                for f in q["act"]:
                    f(E)

            @block.vector
            def _(E):
                for f in q["dve"]:
                    f(E)

            @block.gpsimd
            def _(E):
                for f in q["pool"]:
                    f(E)

            @block.tensor
            def _(E):
                for f in q["pe"]:
                    f(E)


class Arena:
    def __init__(self, nc, stack, name, nbytes):
        self.n = nbytes // 4
        self.t = stack.enter_context(nc.sbuf_tensor(name, [128, self.n], F32))
        self.off = 0
        self.peak = 0
        self.top = self.n

    def alloc(self, free_shape, dt, name="", top=False):
        nel = int(np.prod(free_shape))
        nw = nel if dt == F32 else (nel + 1) // 2
        if top:
            self.top = (self.top - nw) // 8 * 8
            assert self.off <= self.top, f"arena overflow (top) allocating {name}"
            ap = self.t[:, self.top:self.top + nw]
        else:
            self.off = (self.off + 7) // 8 * 8
            assert self.off + nw <= self.top, f"arena overflow allocating {name}: {self.off + nw} > {self.top}"
            ap = self.t[:, self.off:self.off + nw]
            self.off += nw
        self.peak = max(self.peak, self.off + (self.n - self.top))
        if dt != F32:
            ap = ap.bitcast(dt)
            if nel != 2 * nw:
                ap = ap[:, 0:nel]
        if len(free_shape) == 2:
            ap = ap.rearrange("p (a b) -> p a b", a=free_shape[0])
        elif len(free_shape) == 3:
            ap = ap.rearrange("p (a b c) -> p a b c", a=free_shape[0], b=free_shape[1])
        return T(ap, name)

    def mark(self):
        return self.off

    def release(self, m):
        self.off = m

    def release_top(self):
        self.top = self.n


import math

D = 1024
HD = 64
ALPHA = 4.0 ** 0.25
LN_EPS = 1e-6
RMS_EPS = 1e-6
NCTX = 256


class Cfg:
    def __init__(self, S=4096, NE=32, L=2):
        self.S, self.NE, self.L = S, NE, L
        self.NOWN = S // 2
        self.NB = self.NOWN // 512
        self.NCOL = NCTX + self.NOWN
        self.NT_OWN = self.NOWN // 128
        self.NT_ALL = S // 128
        self.blocks = [(0, NCTX, 1)] + [(NCTX + 512 * i, 512, 0) for i in range(self.NB)]


def dram_inputs(cfg, mode="A"):
    L, NE, NOWN = cfg.L, cfg.NE, cfg.NOWN
    NKO, NK = NCTX + NOWN, NCTX + 2 * NOWN
    common = {
        "cvec": ([128, 16], F32), "mod_w": ([L, D, 6 * D], F32), "mod_bT": ([128, L * 48], F32),
        "lnT": ([128, L * 2 * 2 * 8], F32),
        "router_w": ([L, D, NE], F32), "router_b": ([L, 1, NE], F32),
        "e_bguT": ([128, L * NE * 16], F32), "e_bd": ([L, NE, D], F32), "ident": ([128, 128], F32),
    }
    a_only = {
        "xT_own": ([D, NOWN], F32), "xT_par": ([D, NOWN], F32), "xT_halo": ([D, 256], F32), "cT": ([D, NCTX], F32),
        "w_in0": ([D, 1536], F32), "w_out0": ([D, D], F32), "sink": ([1, 12], F32),
        "w_in1": ([D, 2304], F32), "qkng": ([128, 2], F32),
        "e_wgu0": ([NE, D, 2 * D], F32), "e_wd0": ([NE, D, D], F32),
        "cos_own": ([128, NOWN], F32), "sin_own": ([128, NOWN], F32),
        "cos_halo": ([128, 256], F32), "sin_halo": ([128, 256], F32), "valid_halo": ([128, 2], F32),
        "Rm": ([128, 128], F32), "masks": ([128, 768], F32), "CS2": ([128, 256], F32),
        "blk1": ([128, 128], F32),
        "tabC": ([cfg.NB, 128, cfg.NT_ALL, 512], BF16), "tabS": ([cfg.NB, 128, cfg.NT_ALL, 512], BF16),
        "tabCc": ([128, 2, NCTX], BF16), "tabSc": ([128, 2, NCTX], BF16),
    }
    b_only = {
        "w_out1": ([D, D], F32), "lam": ([1, 256], F32), "subln": ([1, 128], F32),
        "e_wgu1": ([NE, D, 2 * D], F32), "e_wd1": ([NE, D, D], F32),
    }
    b_state = {
        "xstate": ([128, 8, cfg.NCOL], F32), "q1": ([8, 128, NOWN], BF16), "kT_all": ([5, 128, NK], BF16),
        "vc_all": ([4, NK, 129], BF16), "vd_all": ([2, NK, 65], BF16),
    }
    d = dict(common)
    if mode in ("A", "F"):
        d.update(a_only)
    if mode in ("B", "F"):
        d.update(b_only)
    if mode == "B":
        d.update(b_state)
    if mode == "F":
        d["gidx"] = ([128, 22], mybir.dt.uint32)
    return d


class Prog:
    def __init__(self, cfg, mode="A", stop_after=None, dbg=()):
        self.cfg = cfg
        self.mode = mode
        self.stop_after = stop_after
        self.dbg = dbg
        self.nc = bass.Bass("TRN2", target_bir_lowering=False)
        self.st = ExitStack()

    def build(self):
        cfg, nc, st, mode = self.cfg, self.nc, self.st, self.mode
        NOWN = cfg.NOWN
        NKO, NK = NCTX + NOWN, NCTX + 2 * NOWN
        with st:
            self.S = S = Sched(nc, st)
            self.din = {}
            for name, (shape, dt) in dram_inputs(cfg, mode).items():
                self.din[name] = nc.dram_tensor(name, shape, dt, kind="ExternalInput").ap()
            self.dbg_aps = {}
            self.DT = {k: T(v, k) for k, v in self.din.items()}
            pers_b = 8 * cfg.NCOL * 6 + 6144
            self.pers = Arena(nc, st, "pers", pers_b)
            self.work = Arena(nc, st, "work", (210000 - pers_b) // 32 * 32)
            self.PS = [T(st.enter_context(nc.psum_tensor(f"ps{i}", [128, 512], F32))[:], f"ps{i}") for i in range(8)]
            self.setup_persistent()
            self.phase0_mod()
            done = False
            if mode in ("A", "F"):
                self.layer0()
                done = self.stop_after is not None and self.stop_after != "cc"
            if mode == "A" and not done:
                outs = {}
                for name, shape, dt in (("xstate", [128, 8, cfg.NCOL], F32), ("q1", [8, 128, NOWN], BF16),
                                        ("kT_own", [5, 128, NKO], BF16), ("vc_own", [4, NKO, 129], BF16), ("vd_own", [2, NKO, 65], BF16)):
                    outs[name] = nc.dram_tensor(name, shape, dt, kind="ExternalOutput").ap()
                self.layer1_proj(outs["q1"], outs["kT_own"], outs["vc_own"], outs["vd_own"])
                XS = T(outs["xstate"], "xstate_o")
                for c in range(8):
                    for bi, (c0, n, v) in enumerate(cfg.blocks):
                        S.dma("sp" if c % 2 == 0 else "act", (XS, outs["xstate"][:, c, c0:c0 + n]), self.xT[c][bi])
            if mode == "F" and not done:
                NKOC = NKO // 128
                ROW = NKOC * 129
                snd_t = nc.dram_tensor("kv_snd", [11 * 128, ROW], BF16)
                rcv_t = nc.dram_tensor("kv_rcv", [8 * 11 * 128, ROW], BF16)
                q1_t = nc.dram_tensor("q1_scr", [8, 128, NOWN], BF16)
                snd, rcv, q1 = snd_t.ap(), rcv_t.ap(), q1_t.ap()
                SND, RCV = T(snd, "kv_snd"), T(rcv, "kv_rcv")
                self.Q1T = [T(q1[i], f"q1_{i}") for i in range(8)]
                kdst = [snd[i * 128:(i + 1) * 128, 0:NKO] for i in range(5)]
                vcdst = lambda r0: snd[5 * 128:9 * 128, (r0 // 128) * 129:(r0 // 128 + 1) * 129].rearrange("(h p) d -> p h d", p=128)
                vddst = lambda r0: snd[9 * 128:11 * 128, (r0 // 128) * 65:(r0 // 128 + 1) * 65].rearrange("(h p) d -> p h d", p=128)
                self.layer1_proj(q1, kdst, None, None, vcdst=vcdst, vddst=vddst, kvT=SND)
                gix = self.pers.alloc([22], mybir.dt.uint32, "gidx") if False else None
                gi_sb = T(st.enter_context(nc.sbuf_tensor("gidx_sb", [128, 22], mybir.dt.uint32))[:], "gidx_sb")
                S.dma("sp", gi_sb, self.DT["gidx"])
                cc_sem = st.enter_context(nc.semaphore("cc_sem"))
                S.barrier()
                CCSTEP = 9
                if CCSTEP >= 1:
                    S.q["pool"].append(lambda E: E.collective_compute("AllGather", ALU.bypass, replica_groups=[list(range(8))],
                                                                      ins=[snd_t.ap().opt()], outs=[rcv_t.ap().opt()]).then_inc(cc_sem))
                    S.q["pool"].append(lambda E: E.wait_ge(cc_sem, 1))

                last_g = [None]

                def gather(dst_T, dst_ap, rl, tile):
                    eng = "pool"
                    if last_g[0] is not None:
                        S._wait(eng, last_g[0])
                    k = S.dnext[eng]
                    S.dnext[eng] = (k + 1) % NDSEM
                    if S.dcnt[eng][k] > 0:
                        S._wait(eng, ("d", eng, k, S.dcnt[eng][k]))
                    S._deps(eng, [gi_sb], [dst_T])
                    S.dcnt[eng][k] += 1
                    n_ = S.dcnt[eng][k]
                    sem = S.dsem[eng][k]
                    col = rl * 11 + tile
                    S.q[eng].append(lambda E, dst_ap=dst_ap, col=col, sem=sem: E.indirect_dma_start(
                        out=dst_ap, out_offset=None, in_=rcv,
                        in_offset=bass.IndirectOffsetOnAxis(ap=gi_sb.ap[:, col:col + 1], axis=0)).then_inc(sem, 16))
                    S.ninstr += 1
                    S._commit(("d", eng, k, n_), [gi_sb], [dst_T])
                    last_g[0] = ("d", eng, k, n_)

                if self.stop_after == "cc":
                    tmpk = self.work.alloc([2, ROW], BF16, "tmpk")
                    if CCSTEP >= 2:
                        gather(tmpk, tmpk.ap[:, 0, :], 0, 0)
                        gather(tmpk, tmpk.ap[:, 1, :], 1, 5)
                    done = True
                else:
                    self.layer1_attn(q1, None, None, None, gather=gather)
            if mode == "B":
                self.layer1_attn(self.din["q1"], self.din["kT_all"], self.din["vc_all"], self.din["vd_all"])
            if mode in ("B", "F") and not done:
                self.moe(1)
            if mode in ("B", "F"):
                out_ap = nc.dram_tensor("outT", [128, 8, NOWN], F32, kind="ExternalOutput").ap()
                OUT = T(out_ap, "outT")
                for c in range(8):
                    for bi, (c0, n, v) in enumerate(cfg.blocks):
                        if v == 1:
                            continue
                        S.dma("sp" if c % 2 == 0 else "act", (OUT, out_ap[:, c, c0 - NCTX:c0 - NCTX + n]), self.xT[c][bi])
            S.wait_all_on("sp")
            S.wait_all_on("pool")
            S.wait_all_on("act")
            print("mode", mode, "instructions:", S.ninstr, "pers peak", self.pers.peak * 4, "work peak", self.work.peak * 4)
            S.emit()
        return nc

    def dbg_out(self, name, src_T, src_ap, shape, dt=F32):
        if name not in self.dbg:
            return
        ap = self.nc.dram_tensor("dbg_" + name, list(shape), dt, kind="ExternalOutput").ap()
        self.dbg_aps[name] = ap
        self.S.barrier()
        self.S.dma("sp", (T(ap), ap), (src_T, src_ap))

    def setup_persistent(self):
        cfg, S, P = self.cfg, self.S, self.pers
        DT = self.DT
        self.xT = [[None] * len(cfg.blocks) for _ in range(8)]
        xall = P.alloc([8, cfg.NCOL], F32, "xT")
        self.xall = xall
        for c in range(8):
            for bi, (c0, n, v) in enumerate(cfg.blocks):
                self.xT[c][bi] = T(xall.ap[:, c, c0:c0 + n], f"xT{c}_{bi}")
        self.hT2 = P.alloc([8, cfg.NCOL], BF16, "hbuf")
        self.hT2b = [[T(self.hT2.ap[:, c, c0:c0 + n], f"h2_{c}_{bi}") for bi, (c0, n, v) in enumerate(cfg.blocks)] for c in range(8)]
        self.ident_f = P.alloc([128], F32, "ident_f")
        self.ident_b = P.alloc([128], BF16, "ident_b")
        self.ones_f = P.alloc([128], F32, "ones_f")
        self.lnT = P.alloc([cfg.L * 32], F32, "lnT")
        self.mv = [P.alloc([cfg.L * 48], F32, f"mv{v}") for v in range(2)]
        self.scal = P.alloc([cfg.L * 2 * 2 * 3 * 8 + 32], F32, "scal")
        self._scal_off = 0
        self._scal_map = {}
        S.dma("sp", self.ident_f, DT["ident"])
        S.copy("dve", self.ident_b, self.ident_f)
        S.memset("dve", self.ones_f, 1.0)
        S.dma("sp", self.lnT, DT["lnT"])
        for bi, (c0, n, v) in enumerate(cfg.blocks):
            if self.mode == "B":
                for c in range(8):
                    S.dma("sp" if c % 2 == 0 else "act", self.xT[c][bi], (DT["xstate"], self.din["xstate"][:, c, c0:c0 + n]))
                continue
            src = self.din["cT"] if v == 1 else self.din["xT_own"][:, c0 - NCTX:c0 - NCTX + n]
            srcT = DT["cT"] if v == 1 else DT["xT_own"]
            for c in range(8):
                S.dma("sp", self.xT[c][bi], (srcT, src[c * 128:(c + 1) * 128, :]))

    def sc(self, key):
        if key not in self._scal_map:
            self._scal_map[key] = self._scal_off
            self._scal_off += 8
        o = self._scal_map[key]
        return (self.scal, self.scal.ap[:, o:o + 8])

    def lnp(self, l, i, gb):
        o = ((l * 2 + i) * 2 + gb) * 8
        return (self.lnT, self.lnT.ap[:, o:o + 8])

    def mcol(self, v, l, j):
        o = l * 48 + j * 8
        return (self.mv[v], self.mv[v].ap[:, o:o + 8])

    def phase0_mod(self):
        cfg, S, W, DT = self.cfg, self.S, self.work, self.DT
        m0 = W.mark()
        cv = W.alloc([16], F32, "cv")
        sT = W.alloc([16], F32, "sT")
        mbT = W.alloc([cfg.L * 48], F32, "mbT")
        mw = [W.alloc([8, 512], F32, f"mw{i}") for i in range(2)]
        S.dma("sp", cv, DT["cvec"])
        S.dma("sp", mbT, DT["mod_bT"])
        S.act(sT, cv, ACTF.Silu)
        sview = sT.ap.rearrange("p (v k) -> p k v", v=2)
        mps = self.PS[0]
        it = 0
        for l in range(cfg.L):
            for cb in range(12):
                buf = mw[it % 2]
                it += 1
                for kc in range(8):
                    S.dma("sp" if kc % 2 == 0 else "act", (buf, buf.ap[:, kc, :]),
                          (DT["mod_w"], self.din["mod_w"][l, kc * 128:(kc + 1) * 128, cb * 512:(cb + 1) * 512]))
                for sub in range(4):
                    ch = l * 48 + cb * 4 + sub
                    S.mm((mps, mps.ap[:, ch * 2:ch * 2 + 2]),
                         [((buf, buf.ap[:, kc, sub * 128:(sub + 1) * 128]), (sT, sview[:, kc, :])) for kc in range(8)])
        nch = cfg.L * 48
        for v in range(2):
            S.tt("dve", self.mv[v], (mps, mps.ap[:, 0:2 * nch].rearrange("p (c v) -> p c v", v=2)[:, :, v]), mbT, ALU.add)
        for v in range(2):
            S.ts("dve", self.sc(("A0s", v)), self.mcol(v, 0, 1), 1.0, None, ALU.add)
            S.copy("dve", self.sc(("A0b", v)), self.mcol(v, 0, 0))
            for l in range(cfg.L):
                for i in range(2):
                    S.ts("dve", self.sc(("gp", l, i, v)), self.mcol(v, l, 2 + 3 * i), 1.0 / ALPHA, None, ALU.mult)
                    if i == 0:
                        nsc, nsh = self.mcol(v, l, 4), self.mcol(v, l, 3)
                    elif l + 1 < cfg.L:
                        nsc, nsh = self.mcol(v, l + 1, 1), self.mcol(v, l + 1, 0)
                    else:
                        continue
                    tmp = self.sc(("tmp", v))
                    S.ts("dve", tmp, nsc, 1.0, None, ALU.add)
                    S.tt("dve", self.sc(("hG", l, i, v)), self.lnp(l, i, 0), tmp, ALU.mult)
                    S.tt("dve", self.sc(("hB", l, i, v)), self.lnp(l, i, 1), tmp, ALU.mult)
                    S.tt("dve", self.sc(("hB", l, i, v)), self.sc(("hB", l, i, v)), nsh, ALU.add)
        self.dbg_out("mv0", self.mv[0], self.mv[0].ap, [128, cfg.L * 48])
        self.dbg_out("mv1", self.mv[1], self.mv[1].ap, [128, cfg.L * 48])
        S.barrier()
        W.release(m0)

    def load_w_bf16(self, dst, src_name, src_ap, ncols, nk=8):
        S = self.S
        for kc in range(nk):
            S.dma("pool", (dst, dst.ap[:, kc, :]), (self.DT[src_name], src_ap[kc * 128:(kc + 1) * 128, :]))

    def layer0(self):
        cfg, S, W, DT, PS = self.cfg, self.S, self.work, self.DT, self.PS
        NOWN, NT_OWN, NT_ALL = cfg.NOWN, cfg.NT_OWN, cfg.NT_ALL
        m_layer = W.mark()
        hflat = self.hT2.ap.rearrange("p c n -> p (c n)")
        AcAs = [T(hflat[:, t * 512:(t + 1) * 512], f"AcAs{t}") for t in range(NT_ALL)]
        AcAs_c = [T(hflat[:, (NT_ALL + t) * 512:(NT_ALL + t + 1) * 512], f"AcAsc{t}") for t in range(2)]
        qd0 = self.nc.dram_tensor("qd0", [6, 128, NOWN], BF16, kind="Internal").ap()
        qd0c = self.nc.dram_tensor("qd0c", [6, 128, NCTX], BF16, kind="Internal").ap()
        QD0 = [T(qd0[i], f"qd0_{i}") for i in range(6)]
        QD0c = [T(qd0c[i], f"qd0c_{i}") for i in range(6)]
        m_attn = W.mark()
        KT = [W.alloc([NOWN], BF16, f"KT{i}") for i in range(2)]
        KTh = [W.alloc([256], BF16, f"KTh{i}") for i in range(2)]
        KTc = [W.alloc([NCTX], BF16, f"KTc{i}") for i in range(2)]
        Vo = [W.alloc([4, 65], BF16, f"Vo{t}") for t in range(NT_OWN)]
        Vh = [W.alloc([4, 65], BF16, f"Vh{t}") for t in range(2)]
        Vc = [W.alloc([4, 65], BF16, f"Vc{t}") for t in range(2)]
        m_proj = W.mark()
        Win = W.alloc([8, 1536], BF16, "Win")
        csb = [W.alloc([2, 512], F32, f"csb{i}") for i in range(2)]
        cosH = W.alloc([256], F32, "cosH")
        sinH = W.alloc([256], F32, "sinH")
        vh = W.alloc([2], F32, "vh")
        Rm = W.alloc([128], BF16, "Rm")
        CS2 = W.alloc([256], BF16, "CS2")
        xs = [W.alloc([512], F32, f"xs{i}") for i in range(3)]
        hT = [W.alloc([8, 512], BF16, "hT0")]
        qs = [W.alloc([512], BF16, "qs0")]
        qst = [W.alloc([512], BF16, f"qst{i}") for i in range(3)]
        t1 = [W.alloc([512], F32, f"t1{i}") for i in range(2)]
        t2 = [W.alloc([512], F32, f"t2{i}") for i in range(2)]
        aTb = [W.alloc([2, 512], BF16, f"aTb{i}") for i in range(2)]
        self.load_w_bf16(Win, "w_in0", self.din["w_in0"], 1536)
        S.dma("sp", cosH, DT["cos_halo"]); S.dma("sp", sinH, DT["sin_halo"])
        S.dma("sp", vh, DT["valid_halo"])
        S.dma("pool", Rm, DT["Rm"]); S.dma("pool", CS2, DT["CS2"])

        tblocks = [("ctx", 0, NCTX)] + [("own", i, 512) for i in range(cfg.NB)] + [("halo", 0, 256)] + \
                  [("par", i, 512) for i in range(cfg.NB)]
        cnt = {"h": 0, "q": 0, "a": 0, "a2": 0, "pp": 0, "pr": 0, "pv": 0, "pa": 0, "xs": 0, "cs": 0, "qst": 0}

        def nxt(k, m=2):
            v = cnt[k] % m
            cnt[k] += 1
            return v

        for (kind, bi, n) in tblocks:
            v = 1 if kind == "ctx" else 0
            hb = hT[0]
            if kind == "own":
                cb_ = csb[nxt("cs")]
                S.dma("sp", (cb_, cb_.ap[:, 0, :]), (DT["cos_own"], self.din["cos_own"][:, bi * 512:(bi + 1) * 512]))
                S.dma("act", (cb_, cb_.ap[:, 1, :]), (DT["sin_own"], self.din["sin_own"][:, bi * 512:(bi + 1) * 512]))
            for c in range(8):
                if kind == "ctx":
                    src = self.xT[c][0]
                elif kind == "own":
                    src = self.xT[c][1 + bi]
                else:
                    dn = "xT_halo" if kind == "halo" else "xT_par"
                    dap = self.din[dn][c * 128:(c + 1) * 128, (0 if kind == "halo" else bi * 512):(0 if kind == "halo" else bi * 512) + n]
                    xb_ = xs[nxt("xs", 3)]
                    S.dma("sp" if c % 2 == 0 else "act", (xb_, xb_.ap[:, 0:n]), (DT[dn], dap))
                    src = (xb_, xb_.ap[:, 0:n])
                sT_, sA = self.sc(("A0s", v))
                bT_, bA = self.sc(("A0b", v))
                S.act((hb, hb.ap[:, c, 0:n]), src, ACTF.Identity, bias=(bT_, bA[:, c:c + 1]), scale=(sT_, sA[:, c:c + 1]))

            def proj_fm(col0):
                ps = PS[nxt("pp")]
                S.mm((ps, ps.ap[:, 0:n]), [((Win, Win.ap[:, kc, col0:col0 + 128]), (hb, hb.ap[:, kc, 0:n])) for kc in range(8)])
                return ps

            def rope_to(ps, dst, cos_, sin_):
                q_ = qs[0]
                S.copy("act", (q_, q_.ap[:, 0:n]), (ps, ps.ap[:, 0:n]))
                pr = PS[2 + nxt("pr")]
                S.mm((pr, pr.ap[:, 0:n]), [(Rm, (q_, q_.ap[:, 0:n]))])
                i_ = nxt("a")
                a1, a2 = t1[i_], t2[i_]
                S.tt("pool", (a1, a1.ap[:, 0:n]), (q_, q_.ap[:, 0:n]), cos_, ALU.mult)
                S.tt("dve", (a2, a2.ap[:, 0:n]), (pr, pr.ap[:, 0:n]), sin_, ALU.mult)
                S.tt("pool", dst, (a1, a1.ap[:, 0:n]), (a2, a2.ap[:, 0:n]), ALU.add)

            if kind in ("ctx", "own"):
                for ti in range(6):
                    ps = proj_fm(256 + ti * 128)
                    qb_ = qst[nxt("qst", 3)]
                    if kind == "ctx":
                        S.copy("act", (qb_, qb_.ap[:, 0:n]), (ps, ps.ap[:, 0:n]))
                        S.dma("sp", QD0c[ti], (qb_, qb_.ap[:, 0:n]))
                    else:
                        rope_to(ps, (qb_, qb_.ap[:, 0:n]), (cb_, cb_.ap[:, 0, :]), (cb_, cb_.ap[:, 1, :]))
                        S.dma("sp", (QD0[ti], qd0[ti][:, bi * 512:bi * 512 + n]), (qb_, qb_.ap[:, 0:n]))
            if kind in ("ctx", "own", "halo"):
                for ti in range(2):
                    ps = proj_fm(1024 + ti * 128)
                    if kind == "ctx":
                        S.copy("act", KTc[ti], (ps, ps.ap[:, 0:n]))
                    elif kind == "own":
                        rope_to(ps, (KT[ti], KT[ti].ap[:, bi * 512:bi * 512 + n]), (cb_, cb_.ap[:, 0, :]), (cb_, cb_.ap[:, 1, :]))
                    else:
                        rope_to(ps, KTh[ti], cosH, sinH)
                for tt_ in range(n // 128):
                    pv = PS[4 + nxt("pv")]
                    S.mm((pv, pv.ap[:, 0:256]),
                         [((hb, hb.ap[:, kc, tt_ * 128:(tt_ + 1) * 128]), (Win, Win.ap[:, kc, 1280:1536])) for kc in range(8)])
                    if kind == "ctx":
                        vt = Vc[tt_]
                    elif kind == "own":
                        vt = Vo[bi * 4 + tt_]
                    else:
                        vt = Vh[tt_]
                    pvv = pv.ap[:, 0:256].rearrange("p (g d) -> p g d", g=4)
                    if kind == "halo":
                        S.ts("dve", (vt, vt.ap[:, :, 0:64]), (pv, pvv), (vh, vh.ap[:, tt_:tt_ + 1]), None, ALU.mult)
                        S.copy("pool", (vt, vt.ap[:, :, 64]), (vh, vh.ap[:, tt_:tt_ + 1].to_broadcast([128, 4])))
                    else:
                        S.copy("act", (vt, vt.ap[:, :, 0:64]), (pv, pvv))
                        S.memset("pool", (vt, vt.ap[:, :, 64]), 1.0)
            if kind in ("ctx", "own", "par"):
                ab = aTb[nxt("a2")]
                for cc in range(2):
                    ps = proj_fm(cc * 128)
                    S.copy("act" if cc == 0 else "dve", (ab, ab.ap[:, cc, 0:n]), (ps, ps.ap[:, 0:n]))
                for tt_ in range(n // 128):
                    pa = PS[6 + nxt("pa")]
                    for cc in range(2):
                        S.mm((pa, pa.ap[:, cc * 256:(cc + 1) * 256]), [((ab, ab.ap[:, cc, tt_ * 128:(tt_ + 1) * 128]), CS2)])
                    if kind == "ctx":
                        dst = AcAs_c[tt_]
                    elif kind == "own":
                        dst = AcAs[bi * 4 + tt_]
                    else:
                        dst = AcAs[NT_OWN + bi * 4 + tt_]
                    S.copy("act" if tt_ % 2 == 0 else "dve", dst, pa)
        self.dbg_out("KT0", KT[0], KT[0].ap, [128, NOWN], BF16)
        self.dbg_out("KTh0", KTh[0], KTh[0].ap, [128, 256], BF16)
        self.dbg_out("Vo0", Vo[0], Vo[0].ap, [128, 4, 65], BF16)
        self.dbg_out("Vh0", Vh[0], Vh[0].ap, [128, 4, 65], BF16)
        self.dbg_out("AcAs0", AcAs[0], AcAs[0].ap, [128, 512], BF16)
        if self.stop_after == "proj0":
            return
        S.barrier()
        W.release(m_proj)

        oT = W.alloc([8, cfg.NCOL], BF16, "oT", top=True)
        masks = W.alloc([768], BF16, "masks")
        esink = W.alloc([12], F32, "esink")
        o_tok = [W.alloc([768], BF16, f"otok{i}") for i in range(2)]
        PTl = [W.alloc([3, 384], BF16, f"PTl{i}") for i in range(2)]
        PTc = [W.alloc([2, 384], BF16, f"PTc{i}") for i in range(2)]
        zs = [W.alloc([4], F32, f"zs{i}") for i in range(2)]
        S.dma("pool", masks, DT["masks"])
        S.dma("sp", esink, (DT["sink"], self.din["sink"].partition_broadcast(128)))
        S.act(esink, esink, ACTF.Exp)
        psO = PS[5]
        psT = T(PS[6].ap.bitcast(BF16), "psT_bf")
        ac = {"pt": 0, "o": 0, "z": 0, "ot": 0}

        oT_att = [T(oT.ap[:, 2:8, c0:c0 + n], f"oTatt{bi}") for bi, (c0, n, v) in enumerate(cfg.blocks)]
        self.oT_att = oT_att
        def kfn(tiles, c0):
            return lambda ti: tiles[ti].ap[:, c0:c0 + 128]
        ctx_keys = [((KTc[0], KTc[1]), kfn(KTc, t * 128), Vc[t], "ctx") for t in range(2)]

        def attend_impl(Qsrc, klist, dst):
            ot = o_tok[ac["ot"] % 2]
            ac["ot"] += 1
            loc = [k for k in klist if k[3] != "ctx"]
            ctxk = [k for k in klist if k[3] == "ctx"]
            for g in range(4):
                half = g % 2
                rows = slice(half * 64, half * 64 + 64)
                ti = g // 2
                q_tiles = [(0 if g < 2 else 3) + j for j in range(3)]
                pl = PTl[ac["pt"] % 2]
                pc = PTc[ac["pt"] % 2]
                ac["pt"] += 1
                for grp, banks, ptile in ((loc, (0, 1, 2), pl), (ctxk, (3, 4), pc)):
                    for ci, (kTs, kap, vt, mt) in enumerate(grp):
                        ps = PS[banks[ci]]
                        for j in range(3):
                            qT_, qap = Qsrc[q_tiles[j]]
                            S.mm((ps, ps.ap[:, j * 128:(j + 1) * 128]), [((kTs[ti], kap(ti)[rows, :]), (qT_, qap[rows, :]))])
                        S.act((ptile, ptile.ap[:, ci, :]), (ps, ps.ap[:, 0:384]), ACTF.Exp, scale=0.125)
                        if mt == "lo":
                            S.tt("pool", (ptile, ptile.ap[:, ci, :]), (ptile, ptile.ap[:, ci, :]), (masks, masks.ap[:, 0:384]), ALU.mult)
                        elif mt == "hi":
                            S.tt("pool", (ptile, ptile.ap[:, ci, :]), (ptile, ptile.ap[:, ci, :]), (masks, masks.ap[:, 384:768]), ALU.mult)
                oo = (ac["o"] % 2) * 256
                ac["o"] += 1
                allk = [(pl, ci, k) for ci, k in enumerate(loc)] + [(pc, ci, k) for ci, k in enumerate(ctxk)]
                for j in range(3):
                    S.mm((psO, psO.ap[:, oo + j * 65: oo + (j + 1) * 65]),
                         [((pt_, pt_.ap[:, ci, j * 128:(j + 1) * 128]), (k[2], k[2].ap[:, g, :])) for (pt_, ci, k) in allk])
                z = zs[ac["z"] % 2]
                ac["z"] += 1
                ov = psO.ap[:, oo:oo + 195].rearrange("p (j d) -> p j d", j=3)
                S.tt("dve", (z, z.ap[:, 0:3]), (psO, ov[:, :, 64]), (esink, esink.ap[:, 3 * g:3 * g + 3]), ALU.add)
                S.recip((z, z.ap[:, 0:3]), (z, z.ap[:, 0:3]))
                S.tt("dve", (ot, ot.ap[:, 192 * g:192 * g + 192].rearrange("p (j d) -> p j d", j=3)), (psO, ov[:, :, 0:64]),
                     (z, z.ap[:, 0:3].unsqueeze(2).to_broadcast([128, 3, 64])), ALU.mult)
            for c in range(6):
                S.transpose((psT, psT.ap[:, c * 128:(c + 1) * 128]), (ot, ot.ap[:, c * 128:(c + 1) * 128]), self.ident_b)
            dstT, dap = dst
            S.copy("act", (dstT, dap), (psT, psT.ap[:, 0:768].rearrange("p (c q) -> p c q", c=6)))

        Qb = [W.alloc([6, 512], BF16, f"Qb{i}") for i in range(2)]
        for i in range(6):
            S.dma("sp" if i % 2 == 0 else "act", (Qb[1], Qb[1].ap[:, i, 0:NCTX]), QD0c[i])
        for t in range(2):
            attend_impl([(Qb[1], Qb[1].ap[:, i, t * 128:(t + 1) * 128]) for i in range(6)], ctx_keys,
                        (oT_att[0], oT.ap[:, 2:8, t * 128:(t + 1) * 128]))
        for n_ in range(NT_OWN):
            if n_ % 4 == 0:
                qb_ = Qb[(n_ // 4) % 2]
                for i in range(6):
                    S.dma("sp" if i % 2 == 0 else "act", (qb_, qb_.ap[:, i, :]), (QD0[i], qd0[i][:, n_ * 128:n_ * 128 + 512]))
            kl = []
            if n_ == 0:
                kl.append(((KTh[0], KTh[1]), kfn(KTh, 0), Vh[0], "lo"))
            else:
                kl.append(((KT[0], KT[1]), kfn(KT, (n_ - 1) * 128), Vo[n_ - 1], "lo"))
            kl.append(((KT[0], KT[1]), kfn(KT, n_ * 128), Vo[n_], "mid"))
            if n_ == NT_OWN - 1:
                kl.append(((KTh[0], KTh[1]), kfn(KTh, 128), Vh[1], "hi"))
            else:
                kl.append(((KT[0], KT[1]), kfn(KT, (n_ + 1) * 128), Vo[n_ + 1], "hi"))
            kl += ctx_keys
            bi = 1 + n_ // 4
            c0 = NCTX + n_ * 128
            attend_impl([(qb_, qb_.ap[:, i, (n_ % 4) * 128:(n_ % 4 + 1) * 128]) for i in range(6)], kl,
                        (oT_att[bi], oT.ap[:, 2:8, c0:c0 + 128]))
        self.dbg_out("oT_att", oT, oT.ap[:, 2:8, :], [128, 6, cfg.NCOL], BF16)
        if self.stop_after == "attn0":
            return
        S.barrier()
        W.release(m_attn)
        NPIECE = 8
        tb = [[W.alloc([NPIECE, 512], BF16, f"tab{cs}{i}") for i in range(2)] for cs in range(2)]
        tcx = [W.alloc([2, NCTX], BF16, f"tabc{cs}") for cs in range(2)]
        S.dma("sp", tcx[0], DT["tabCc"]); S.dma("act", tcx[1], DT["tabSc"])
        oT_f = [[T(oT.ap[:, cc, c0:c0 + n], f"oTf{cc}_{bi}") for bi, (c0, n, v) in enumerate(cfg.blocks)] for cc in range(2)]
        self.oT_f = oT_f
        for cc in range(2):
            ps = PS[cc]
            S.mm((ps, ps.ap[:, 0:NCTX]),
                 [((AcAs_c[t], AcAs_c[t].ap[:, cc * 256 + cs * 128: cc * 256 + cs * 128 + 128]), (tcx[cs], tcx[cs].ap[:, t, :]))
                  for t in range(2) for cs in range(2)])
            S.copy("act", oT_f[cc][0], (ps, ps.ap[:, 0:NCTX]))
        pi = 0
        for b in range(cfg.NB):
            npieces = NT_ALL // NPIECE
            for p_ in range(npieces):
                bufs = [tb[0][pi % 2], tb[1][pi % 2]]
                pi += 1
                for cs, nm in ((0, "tabC"), (1, "tabS")):
                    S.dma("sp" if cs == 0 else "act", bufs[cs], (DT[nm], self.din[nm][b, :, p_ * NPIECE:(p_ + 1) * NPIECE, :]))
                for cc in range(2):
                    ps = PS[cc]
                    pairs = []
                    for tl in range(NPIECE):
                        t = p_ * NPIECE + tl
                        for cs in range(2):
                            pairs.append(((AcAs[t], AcAs[t].ap[:, cc * 256 + cs * 128: cc * 256 + cs * 128 + 128]),
                                          (bufs[cs], bufs[cs].ap[:, tl, :])))
                    S.mm(ps, pairs, start=(p_ == 0), stop=(p_ == npieces - 1))
            for cc in range(2):
                S.copy("act" if cc == 0 else "dve", oT_f[cc][1 + b], PS[cc])
        self.dbg_out("oT_f", oT, oT.ap[:, 0:2, :], [128, 2, cfg.NCOL], BF16)
        if self.stop_after == "fourier0":
            return
        S.barrier()
        W.release(m_attn)
        self.oT = oT
        self.outproj_ln(0, "w_out0")
        if self.stop_after == "mix0":
            return
        S.barrier()
        W.release(m_layer)
        W.release_top()
        self.moe(0)
        if self.stop_after == "moe0":
            return

    def outproj_ln(self, l, wname):
        cfg, S, W, DT, PS = self.cfg, self.S, self.work, self.DT, self.PS
        oT = self.oT
        m0 = W.mark()
        Wo = W.alloc([8, D], BF16, "Wo")
        self.load_w_bf16(Wo, wname, self.din[wname], D)
        oTall = T(oT.ap, "oTall")
        k = 0
        for bi, (c0, n, v) in enumerate(cfg.blocks):
            for dc in range(8):
                ps = PS[k % 2]
                k += 1
                S.mm((ps, ps.ap[:, 0:n]), [((Wo, Wo.ap[:, oc, dc * 128:(dc + 1) * 128]), (oTall, oT.ap[:, oc, c0:c0 + n])) for oc in range(8)])
                gT_, gA = self.sc(("gp", l, 0, v))
                xt = self.xT[dc][bi]
                S.stt("dve", xt, (ps, ps.ap[:, 0:n]), (gT_, gA[:, dc:dc + 1]), xt, ALU.mult, ALU.add)
        self.dbg_out(f"z{l}0", self.xall, self.xall.ap, [128, 8, cfg.NCOL])
        self.layernorm(l, 0, self.hT2b)
        W.release(m0)

    def layernorm(self, l, i, hdst, skip_ctx=False):
        cfg, S, W, PS = self.cfg, self.S, self.work, self.PS
        m0 = W.mark()
        zsq = [W.alloc([512], F32, f"zsq{j}") for j in range(2)]
        mean = W.alloc([512], F32, "mean")
        var = W.alloc([512], F32, "var")
        rstd = W.alloc([512], F32, "rstd")
        mr = W.alloc([512], F32, "mr")
        u = [W.alloc([512], F32, f"u{j}") for j in range(2)]
        eps = LN_EPS / (ALPHA * ALPHA)
        last = (l == cfg.L - 1 and i == 1)
        for bi, (c0, n, v) in enumerate(cfg.blocks):
            if (last or skip_ctx) and v == 1:
                continue
            s1, s2 = PS[2], PS[3]
            S.mm((s1, s1.ap[:, 0:n]), [(self.ones_f, self.xT[c][bi]) for c in range(8)])
            for c in range(8):
                zq = zsq[c % 2]
                S.act((zq, zq.ap[:, 0:n]), self.xT[c][bi], ACTF.Square)
                S.mm((s2, s2.ap[:, 0:n]), [(self.ones_f, (zq, zq.ap[:, 0:n]))], start=(c == 0), stop=(c == 7))
            S.act((mean, mean.ap[:, 0:n]), (s1, s1.ap[:, 0:n]), ACTF.Copy, scale=1.0 / D)
            S.tt("pool", (mr, mr.ap[:, 0:n]), (mean, mean.ap[:, 0:n]), (mean, mean.ap[:, 0:n]), ALU.mult)
            S.stt("dve", (var, var.ap[:, 0:n]), (s2, s2.ap[:, 0:n]), 1.0 / D, (mr, mr.ap[:, 0:n]), ALU.mult, ALU.subtract)
            S.ts("dve", (var, var.ap[:, 0:n]), (var, var.ap[:, 0:n]), eps, None, ALU.add)
            S.act((var, var.ap[:, 0:n]), (var, var.ap[:, 0:n]), ACTF.Sqrt)
            S.recip((rstd, rstd.ap[:, 0:n]), (var, var.ap[:, 0:n]))
            S.tt("pool", (mr, mr.ap[:, 0:n]), (mean, mean.ap[:, 0:n]), (rstd, rstd.ap[:, 0:n]), ALU.mult)
            for c in range(8):
                uu = u[c % 2]
                eng = "dve" if c % 2 == 0 else "pool"
                S.tt(eng, (uu, uu.ap[:, 0:n]), self.xT[c][bi], (rstd, rstd.ap[:, 0:n]), ALU.mult)
                S.tt(eng, (uu, uu.ap[:, 0:n]), (uu, uu.ap[:, 0:n]), (mr, mr.ap[:, 0:n]), ALU.subtract)
                gT_, gA = self.lnp(l, i, 0)
                bT_, bA = self.lnp(l, i, 1)
                S.act(self.xT[c][bi], (uu, uu.ap[:, 0:n]), ACTF.Identity, bias=(bT_, bA[:, c:c + 1]), scale=(gT_, gA[:, c:c + 1]))
                if hdst is not None and not last:
                    hgT, hgA = self.sc(("hG", l, i, v))
                    hbT, hbA = self.sc(("hB", l, i, v))
                    S.act(hdst[c][bi], (uu, uu.ap[:, 0:n]), ACTF.Identity, bias=(hbT, hbA[:, c:c + 1]), scale=(hgT, hgA[:, c:c + 1]))
        self.dbg_out(f"x{l}{i}", self.xall, self.xall.ap, [128, 8, cfg.NCOL])
        S.barrier()
        W.release(m0)

    def moe(self, l):
        cfg, S, W, DT, PS = self.cfg, self.S, self.work, self.DT, self.PS
        NE, NCOL = cfg.NE, cfg.NCOL
        PL = "dve" if l == 1 else "pool"
        last = (l == cfg.L - 1)
        hT2, hT2b = self.hT2, self.hT2b
        m0 = W.mark()
        rw = W.alloc([8, NE], BF16, "rw")
        rb = W.alloc([NE], F32, "rb")
        GT = W.alloc([NCOL], F32, "GT")
        bgu = W.alloc([NE * 16], F32, "bgu")
        bd = W.alloc([D], F32, "bd")
        for kc in range(8):
            S.dma("pool", (rw, rw.ap[:, kc, :]), (DT["router_w"], self.din["router_w"][l, kc * 128:(kc + 1) * 128, :]))
        S.dma("sp", rb, (DT["router_b"], self.din["router_b"][l].partition_broadcast(128)))
        S.dma("sp", bgu, (DT["e_bguT"], self.din["e_bguT"][:, l * NE * 16:(l + 1) * NE * 16]))
        S.dma("sp", (bd, bd.ap[0:NE, :]), (DT["e_bd"], self.din["e_bd"][l]))
        Lg = [W.alloc([NE], F32, f"Lg{i}") for i in range(2)]
        Ex = [W.alloc([NE], F32, f"Ex{i}") for i in range(2)]
        Mk = [W.alloc([NE], F32, f"Mk{i}") for i in range(2)]
        t8 = [W.alloc([8], F32, f"t8{i}") for i in range(2)]
        sm = [W.alloc([4], F32, f"sm{i}") for i in range(2)]
        k = 0
        for bi, (c0, n, v) in enumerate(cfg.blocks):
            if last and v == 1:
                continue
            for tt_ in range(n // 128):
                i = k % 2
                k += 1
                pr, pt = PS[6], PS[7]
                S.mm((pr, pr.ap[:, 0:NE]), [((hT2b[kc][bi], hT2b[kc][bi].ap[:, tt_ * 128:(tt_ + 1) * 128]), (rw, rw.ap[:, kc, :])) for kc in range(8)])
                S.tt("dve", Lg[i], (pr, pr.ap[:, 0:NE]), rb, ALU.add)
                S.op("dve", lambda E, o=t8[i].ap, a=Lg[i].ap: E.max(out=o, in_=a), [Lg[i]], [t8[i]])
                S.ts("dve", Mk[i], Lg[i], (t8[i], t8[i].ap[:, 3:4]), None, ALU.is_ge)
                S.ts("dve", (sm[i], sm[i].ap[:, 0:1]), (t8[i], t8[i].ap[:, 0:1]), -1.0, None, ALU.mult)
                S.act(Ex[i], Lg[i], ACTF.Exp, bias=(sm[i], sm[i].ap[:, 0:1]))
                S.tt("dve", Ex[i], Ex[i], Mk[i], ALU.mult)
                S.op("dve", lambda E, o=sm[i].ap[:, 1:2], a=Ex[i].ap: E.reduce_sum(out=o, in_=a, axis=AX.X), [Ex[i]], [sm[i]])
                S.recip((sm[i], sm[i].ap[:, 2:3]), (sm[i], sm[i].ap[:, 1:2]))
                S.ts("dve", Ex[i], Ex[i], (sm[i], sm[i].ap[:, 2:3]), None, ALU.mult)
                S.transpose((pt, pt.ap[0:NE, 0:128]), Ex[i], self.ident_f)
                S.copy("act", (GT, GT.ap[0:NE, c0 + tt_ * 128:c0 + (tt_ + 1) * 128]), (pt, pt.ap[0:NE, 0:128]))
        self.dbg_out(f"GT{l}", GT, GT.ap[0:NE, :], [NE, NCOL])
        Wg = [[W.alloc([512], BF16, f"Wg{b}_{kc}") for kc in range(8)] for b in range(2)]
        Wu = [[W.alloc([512], BF16, f"Wu{b}_{kc}") for kc in range(8)] for b in range(2)]
        Wdn = [[W.alloc([D], BF16, f"Wd{b}_{jj}") for jj in range(4)] for b in range(2)]
        actT = [W.alloc([4, 512], BF16, f"actT{i}") for i in range(2)]
        g1 = [W.alloc([512], F32, f"g1{i}") for i in range(2)]
        u1 = [W.alloc([512], F32, f"u1{i}") for i in range(2)]
        sg = [W.alloc([512], BF16, f"sg{i}") for i in range(2)]
        gsb = [W.alloc([512], F32, f"gsb{i}") for i in range(2)]
        wgu = self.din[f"e_wgu{l}"]
        wd = self.din[f"e_wd{l}"]
        WGU, WD_ = DT[f"e_wgu{l}"], DT[f"e_wd{l}"]

        def load_half(e, hh, b):
            for kc in range(8):
                S.dma("pool", Wg[b][kc], (WGU, wgu[e, kc * 128:(kc + 1) * 128, hh * 512:(hh + 1) * 512]))
                S.dma("pool", Wu[b][kc], (WGU, wgu[e, kc * 128:(kc + 1) * 128, 1024 + hh * 512:1024 + (hh + 1) * 512]))
            for jj in range(4):
                S.dma("pool", Wdn[b][jj], (WD_, wd[e, (hh * 4 + jj) * 128:(hh * 4 + jj + 1) * 128, :]))

        halves = [(e, hh) for e in range(NE) for hh in range(2)]
        load_half(0, 0, 0)
        cq = {"q": 0, "a": 0, "y": 0, "g": 0}
        gpT, gpA = None, None
        for it, (e, hh) in enumerate(halves):
            b = it % 2
            if it + 1 < len(halves):
                load_half(halves[it + 1][0], halves[it + 1][1], (it + 1) % 2)
            for bi, (c0, n, v) in enumerate(cfg.blocks):
                if last and v == 1:
                    continue
                gpT, gpA = self.sc(("gp", l, 1, v))
                gps = PS[6]
                gs = gsb[cq["g"] % 2]
                cq["g"] += 1
                S.mm((gps, gps.ap[:, 0:n]), [((self.ident_f, self.ident_f.ap[0:NE, e:e + 1].to_broadcast([NE, 128])), (GT, GT.ap[0:NE, c0:c0 + n]))])
                S.copy("act", (gs, gs.ap[:, 0:n]), (gps, gps.ap[:, 0:n]))
                at = actT[cq["a"] % 2]
                cq["a"] += 1
                for jj in range(4):
                    j = hh * 4 + jj
                    q = cq["q"] % 2
                    cq["q"] += 1
                    pg, pu = PS[2 * q], PS[2 * q + 1]
                    S.mm((pg, pg.ap[:, 0:n]), [((Wg[b][kc], Wg[b][kc].ap[:, jj * 128:(jj + 1) * 128]), hT2b[kc][bi]) for kc in range(8)])
                    S.mm((pu, pu.ap[:, 0:n]), [((Wu[b][kc], Wu[b][kc].ap[:, jj * 128:(jj + 1) * 128]), hT2b[kc][bi]) for kc in range(8)])
                    G1, U1, SG = g1[q], u1[q], sg[q]
                    S.ts("dve", (G1, G1.ap[:, 0:n]), (pg, pg.ap[:, 0:n]), (bgu, bgu.ap[:, e * 16 + j:e * 16 + j + 1]), 7.0, ALU.add, ALU.min)
                    S.act((SG, SG.ap[:, 0:n]), (G1, G1.ap[:, 0:n]), ACTF.Sigmoid, scale=1.702)
                    S.ts("dve", (U1, U1.ap[:, 0:n]), (pu, pu.ap[:, 0:n]), (bgu, bgu.ap[:, e * 16 + 8 + j:e * 16 + 8 + j + 1]), 7.0, ALU.add, ALU.min)
                    S.ts(PL, (U1, U1.ap[:, 0:n]), (U1, U1.ap[:, 0:n]), -7.0, 1.0, ALU.max, ALU.add)
                    S.tt(PL, (G1, G1.ap[:, 0:n]), (G1, G1.ap[:, 0:n]), (SG, SG.ap[:, 0:n]), ALU.mult)
                    S.tt(PL, (G1, G1.ap[:, 0:n]), (G1, G1.ap[:, 0:n]), (U1, U1.ap[:, 0:n]), ALU.mult)
                    S.tt("dve", (at, at.ap[:, jj, 0:n]), (G1, G1.ap[:, 0:n]), (gs, gs.ap[:, 0:n]), ALU.mult)
                for dc in range(8):
                    py = PS[4 + cq["y"] % 2]
                    cq["y"] += 1
                    pairs = [((Wdn[b][jj], Wdn[b][jj].ap[:, dc * 128:(dc + 1) * 128]), (at, at.ap[:, jj, 0:n])) for jj in range(4)]
                    if it == 0:
                        pairs.append(((bd, bd.ap[0:NE, dc * 128:(dc + 1) * 128]), (GT, GT.ap[0:NE, c0:c0 + n])))
                    S.mm((py, py.ap[:, 0:n]), pairs)
                    xt = self.xT[dc][bi]
                    S.stt("dve", xt, (py, py.ap[:, 0:n]), (gpT, gpA[:, dc:dc + 1]), xt, ALU.mult, ALU.add)
        self.dbg_out(f"z{l}1", self.xall, self.xall.ap, [128, 8, NCOL])
        S.barrier()
        W.release(m0)
        self.layernorm(l, 1, None if last else hT2b)
        if not last:
            self.dbg_out(f"h1in", hT2, hT2.ap, [128, 8, NCOL], BF16)

    def layer1_proj(self, q1, kT_own, vc_own, vd_own, vcdst=None, vddst=None, kvT=None):
        cfg, S, W, DT, PS = self.cfg, self.S, self.work, self.DT, self.PS
        NOWN = cfg.NOWN
        m0 = W.mark()
        Win = W.alloc([8, 2304], BF16, "Win1")
        csb = [W.alloc([2, 512], F32, f"csb{i}") for i in range(2)]
        Rm = W.alloc([128], BF16, "Rm")
        blk1 = W.alloc([128], BF16, "blk1")
        qkng = W.alloc([2], F32, "qkng")
        qs = [W.alloc([512], BF16, f"qs{i}") for i in range(2)]
        sq = W.alloc([512], BF16, "sq")
        rs = W.alloc([512], F32, "rs")
        qn = [W.alloc([512], BF16, f"qn{i}") for i in range(2)]
        qst = [W.alloc([512], BF16, f"qst{i}") for i in range(3)]
        t1 = [W.alloc([512], F32, f"t1{i}") for i in range(2)]
        t2 = [W.alloc([512], F32, f"t2{i}") for i in range(2)]
        vcs = [W.alloc([4, 129], BF16, f"vcs{i}") for i in range(2)]
        vds = [W.alloc([2, 65], BF16, f"vds{i}") for i in range(2)]
        self.load_w_bf16(Win, "w_in1", self.din["w_in1"], 2304)
        S.dma("pool", Rm, DT["Rm"]); S.dma("pool", blk1, DT["blk1"])
        S.dma("sp", qkng, DT["qkng"])
        for i in range(2):
            S.memset("pool", (vcs[i], vcs[i].ap[:, :, 128]), 1.0)
            S.memset("pool", (vds[i], vds[i].ap[:, :, 64]), 1.0)
        Q1 = [T(q1[i], f"q1_{i}") for i in range(8)]
        if kvT is None:
            KTO = [T(kT_own[i], f"kTo_{i}") for i in range(5)]
            VCO, VDO = T(vc_own, "vco"), T(vd_own, "vdo")
            vcdst = lambda r0: vc_own[:, r0:r0 + 128, :].rearrange("h p d -> p h d")
            vddst = lambda r0: vd_own[:, r0:r0 + 128, :].rearrange("h p d -> p h d")
        else:
            KTO = [T(kT_own[i], f"kTo_{i}") for i in range(5)]
            VCO = VDO = None
        cnt = {}

        def nxt(k, m=2):
            v = cnt.get(k, 0)
            cnt[k] = v + 1
            return v % m

        for bi, (c0, n, v) in enumerate(cfg.blocks):
            own = (v == 0)
            k0 = c0
            if own:
                ob = bi - 1
                cb_ = csb[nxt("cs")]
                S.dma("sp", (cb_, cb_.ap[:, 0, :]), (DT["cos_own"], self.din["cos_own"][:, ob * 512:(ob + 1) * 512]))
                S.dma("act", (cb_, cb_.ap[:, 1, :]), (DT["sin_own"], self.din["sin_own"][:, ob * 512:(ob + 1) * 512]))

            def proj_fm(col0):
                ps = PS[nxt("pp")]
                S.mm((ps, ps.ap[:, 0:n]), [((Win, Win.ap[:, kc, col0:col0 + 128]), self.hT2b[kc][bi]) for kc in range(8)])
                return ps

            def finish(ps, dst_T, dst_ap, norm_col, rope):
                q_ = qs[nxt("q")]
                S.copy("act", (q_, q_.ap[:, 0:n]), (ps, ps.ap[:, 0:n]))
                cur = q_
                if norm_col is not None:
                    S.act((sq, sq.ap[:, 0:n]), (ps, ps.ap[:, 0:n]), ACTF.Square)
                    pn = PS[2 + nxt("pr")]
                    S.mm((pn, pn.ap[:, 0:n]), [(blk1, (sq, sq.ap[:, 0:n]))])
                    S.ts("dve", (rs, rs.ap[:, 0:n]), (pn, pn.ap[:, 0:n]), 1.0 / 64, RMS_EPS, ALU.mult, ALU.add)
                    S.act((rs, rs.ap[:, 0:n]), (rs, rs.ap[:, 0:n]), ACTF.Sqrt)
                    S.recip((rs, rs.ap[:, 0:n]), (rs, rs.ap[:, 0:n]))
                    qn_ = qn[nxt("qn")]
                    S.stt("dve", (qn_, qn_.ap[:, 0:n]), (q_, q_.ap[:, 0:n]), (qkng, qkng.ap[:, norm_col:norm_col + 1]),
                          (rs, rs.ap[:, 0:n]), ALU.mult, ALU.mult)
                    cur = qn_
                if rope:
                    pr = PS[2 + nxt("pr")]
                    S.mm((pr, pr.ap[:, 0:n]), [(Rm, (cur, cur.ap[:, 0:n]))])
                    i_ = nxt("a")
                    a1, a2 = t1[i_], t2[i_]
                    st_ = qst[nxt("qst", 3)]
                    S.tt("pool", (a1, a1.ap[:, 0:n]), (cur, cur.ap[:, 0:n]), (cb_, cb_.ap[:, 0, 0:n]), ALU.mult)
                    S.tt("dve", (a2, a2.ap[:, 0:n]), (pr, pr.ap[:, 0:n]), (cb_, cb_.ap[:, 1, 0:n]), ALU.mult)
                    S.tt("pool", (st_, st_.ap[:, 0:n]), (a1, a1.ap[:, 0:n]), (a2, a2.ap[:, 0:n]), ALU.add)
                    cur = st_
                S.dma("sp", (dst_T, dst_ap), (cur, cur.ap[:, 0:n]))

            if own:
                for ti in range(4):
                    finish(proj_fm(ti * 128), Q1[ti], q1[ti][:, ob * 512:ob * 512 + n], None, True)
                for ti in range(4):
                    finish(proj_fm(512 + ti * 128), Q1[4 + ti], q1[4 + ti][:, ob * 512:ob * 512 + n], 0, True)
            for ti in range(4):
                finish(proj_fm(1024 + ti * 128), KTO[ti], kT_own[ti][:, k0:k0 + n], None, own)
            finish(proj_fm(1536), KTO[4], kT_own[4][:, k0:k0 + n], 1, own)
            for tt_ in range(n // 128):
                hsl = [(self.hT2b[kc][bi], self.hT2b[kc][bi].ap[:, tt_ * 128:(tt_ + 1) * 128]) for kc in range(8)]
                pv, pd = PS[4 + nxt("pv")], PS[6 + nxt("pd")]
                S.mm(pv, [(hsl[kc], (Win, Win.ap[:, kc, 1664:2176])) for kc in range(8)])
                S.mm((pd, pd.ap[:, 0:128]), [(hsl[kc], (Win, Win.ap[:, kc, 2176:2304])) for kc in range(8)])
                vc_, vd_ = vcs[nxt("vc")], vds[nxt("vd")]
                S.copy("act", (vc_, vc_.ap[:, :, 0:128]), (pv, pv.ap.rearrange("p (h d) -> p h d", h=4)))
                S.copy("dve", (vd_, vd_.ap[:, :, 0:64]), (pd, pd.ap[:, 0:128].rearrange("p (h d) -> p h d", h=2)))
                r0 = k0 + tt_ * 128
                S.dma("sp", (VCO if VCO is not None else T(vcdst(r0)), vcdst(r0)), vc_)
                S.dma("act", (VDO if VDO is not None else T(vddst(r0)), vddst(r0)), vd_)
        S.barrier()
        W.release(m0)

    def layer1_attn(self, q1, kT_all, vc_all, vd_all, gather=None):
        cfg, S, W, DT, PS = self.cfg, self.S, self.work, self.DT, self.PS
        NOWN = cfg.NOWN
        NK = NCTX + 2 * NOWN
        NKC = NK // 128
        l = 1
        m0 = W.mark()
        Q1 = self.Q1T if gather is not None else [T(q1[i], f"q1r_{i}") for i in range(8)]
        if gather is None:
            KTA = [T(kT_all[i], f"kTa_{i}") for i in range(5)]
            VCA = [T(vc_all[i], f"vca_{i}") for i in range(4)]
            VDA = [T(vd_all[i], f"vda_{i}") for i in range(2)]
        NKOC = (NCTX + NOWN) // 128
        ROW = NKOC * 129
        Wo = W.alloc([8, D], BF16, "Wo1")
        self.load_w_bf16(Wo, "w_out1", self.din["w_out1"], D)
        if gather is None:
            Kt = [W.alloc([NK], BF16, f"Kt{i}") for i in range(2)]
            Vt = [W.alloc([NKC, 129], BF16, f"Vt{i}") for i in range(2)]
            chunks = list(range(NKC))
            kchunk = lambda kt, rows, kc: kt.ap[rows, kc * 128:(kc + 1) * 128]
            vchunk = lambda vt, kc, w: vt.ap[:, kc, 0:w]
        else:
            Kt = [W.alloc([2, ROW], BF16, f"Kt{i}") for i in range(2)]
            Vt = [W.alloc([2, ROW], BF16, f"Vt{i}") for i in range(2)]
            chunks = [(0, c) for c in range(NKOC)] + [(1, c) for c in range(2, NKOC)]
            kchunk = lambda kt, rows, kc: kt.ap[rows, kc[0], kc[1] * 128:(kc[1] + 1) * 128]
            vchunk = lambda vt, kc, w: vt.ap[:, kc[0], kc[1] * w:(kc[1] + 1) * w]
        Qt = [W.alloc([4, 512], BF16, f"Qt{i}") for i in range(2)]
        PT = [W.alloc([512], BF16, f"PT{i}") for i in range(4)]
        o_tok = W.alloc([4, D], BF16, "otok1")
        oTb = W.alloc([8, 512], BF16, "oTb1")
        lamt = W.alloc([256], F32, "lamt")
        lam = W.alloc([8], F32, "lam")
        sgb = W.alloc([128], F32, "sgb")
        oa = [W.alloc([128], F32, f"oa{i}") for i in range(2)]
        ob_ = [W.alloc([128], F32, f"ob{i}") for i in range(2)]
        rz = [W.alloc([8], F32, f"rz{i}") for i in range(2)]
        lam_init = 0.8 - 0.6 * math.exp(-0.3 * 1)
        S.dma("sp", lamt, (DT["lam"], self.din["lam"].partition_broadcast(128)))
        S.dma("sp", sgb, (DT["subln"], self.din["subln"].partition_broadcast(128)))
        S.tt("dve", (lamt, lamt.ap[:, 0:64]), (lamt, lamt.ap[:, 0:64]), (lamt, lamt.ap[:, 64:128]), ALU.mult)
        S.tt("dve", (lamt, lamt.ap[:, 128:192]), (lamt, lamt.ap[:, 128:192]), (lamt, lamt.ap[:, 192:256]), ALU.mult)
        S.op("dve", lambda E: E.reduce_sum(out=lam.ap[:, 0:1], in_=lamt.ap[:, 0:64], axis=AX.X), [lamt], [lam])
        S.op("dve", lambda E: E.reduce_sum(out=lam.ap[:, 1:2], in_=lamt.ap[:, 128:192], axis=AX.X), [lamt], [lam])
        S.act((lam, lam.ap[:, 0:2]), (lam, lam.ap[:, 0:2]), ACTF.Exp)
        S.tt("dve", (lam, lam.ap[:, 2:3]), (lam, lam.ap[:, 0:1]), (lam, lam.ap[:, 1:2]), ALU.subtract)
        S.ts("dve", (lam, lam.ap[:, 3:4]), (lam, lam.ap[:, 2:3]), lam_init, -1.0, ALU.add, ALU.mult)
        S.ts("dve", sgb, sgb, 1.0 - lam_init, None, ALU.mult)
        psT = T(PS[7].ap.bitcast(BF16), "psT1")
        cq = {}

        def nxt(k, m=2):
            v = cq.get(k, 0)
            cq[k] = v + 1
            return v % m

        for qb in range(cfg.NB):
            bi = 1 + qb
            units = [("C", h) for h in range(4)] + [("D", g) for g in range(2)]
            for (ut, ui) in units:
                kt, vt, qt = Kt[nxt("kt")], Vt[nxt("vt")], Qt[nxt("qt")]
                if ut == "C":
                    if gather is None:
                        S.dma("sp", kt, KTA[ui])
                        S.dma("act", vt, (VCA[ui], vc_all[ui].rearrange("(c p) d -> p c d", p=128)))
                    else:
                        for rl in range(2):
                            gather(kt, kt.ap[:, rl, :], rl, ui)
                            gather(vt, vt.ap[:, rl, :], rl, 5 + ui)
                    S.dma("sp", (qt, qt.ap[:, 0, :]), (Q1[ui], q1[ui][:, qb * 512:(qb + 1) * 512]))
                    maps = [(slice(64 * m, 64 * m + 64), 0) for m in range(2)]
                    dv = 128
                else:
                    if gather is None:
                        S.dma("sp", kt, KTA[4])
                        S.dma("act", (vt, vt.ap[:, :, 0:65]), (VDA[ui], vd_all[ui].rearrange("(c p) d -> p c d", p=128)))
                    else:
                        for rl in range(2):
                            gather(kt, kt.ap[:, rl, :], rl, 4)
                            gather(vt, vt.ap[:, rl, :], rl, 9 + ui)
                    for j in range(4):
                        S.dma("sp" if j % 2 == 0 else "act", (qt, qt.ap[:, j, :]), (Q1[4 + j], q1[4 + j][:, qb * 512:(qb + 1) * 512]))
                    maps = [(slice(64 * ui, 64 * ui + 64), j) for j in range(4)]
                    dv = 64
                nm = len(maps)
                w = dv + 1
                per_bank = 512 // w
                def acc(mi, st):
                    i = mi * 4 + st
                    return PS[4 + i // per_bank], (i % per_bank) * w
                for kci, kc in enumerate(chunks):
                    pts = []
                    for mi, (rows, qi) in enumerate(maps):
                        ps = PS[(nxt("sc", 4) if nm == 2 else mi)]
                        S.mm(ps, [((kt, kchunk(kt, rows, kc)), (qt, qt.ap[rows, qi, :]))])
                        pt = PT[nxt("pt", 4)]
                        S.act(pt, ps, ACTF.Exp, scale=0.125)
                        pts.append(pt)
                    for mi in range(nm):
                        for st in range(4):
                            pb, off = acc(mi, st)
                            S.mm((pb, pb.ap[:, off:off + w]), [((pts[mi], pts[mi].ap[:, st * 128:(st + 1) * 128]), (vt, vchunk(vt, kc, w)))],
                                 start=(kci == 0), stop=(kci == len(chunks) - 1))
                for st in range(4):
                    if ut == "C":
                        (p1, o1), (p2, o2) = acc(0, st), acc(1, st)
                        r = rz[nxt("rz")]
                        S.recip((r, r.ap[:, 0:1]), (p1, p1.ap[:, o1 + 128:o1 + 129]))
                        S.recip((r, r.ap[:, 1:2]), (p2, p2.ap[:, o2 + 128:o2 + 129]))
                        S.tt("dve", (r, r.ap[:, 1:2]), (r, r.ap[:, 1:2]), (lam, lam.ap[:, 3:4]), ALU.mult)
                        a_, b_ = oa[nxt("oa")], ob_[nxt("ob")]
                        S.ts("dve", a_, (p1, p1.ap[:, o1:o1 + 128]), (r, r.ap[:, 0:1]), None, ALU.mult)
                        S.stt("dve", a_, (p2, p2.ap[:, o2:o2 + 128]), (r, r.ap[:, 1:2]), a_, ALU.mult, ALU.add)
                        S.act(b_, a_, ACTF.Square, accum_out=(r, r.ap[:, 2:3]))
                        S.ts("dve", (r, r.ap[:, 2:3]), (r, r.ap[:, 2:3]), 1.0 / 128, RMS_EPS, ALU.mult, ALU.add)
                        S.act((r, r.ap[:, 2:3]), (r, r.ap[:, 2:3]), ACTF.Sqrt)
                        S.recip((r, r.ap[:, 2:3]), (r, r.ap[:, 2:3]))
                        S.stt("dve", (o_tok, o_tok.ap[:, st, ui * 128:(ui + 1) * 128]), a_, (r, r.ap[:, 2:3]), sgb, ALU.mult, ALU.mult)
                    else:
                        for j in range(4):
                            pb, off = acc(j, st)
                            r = rz[nxt("rz")]
                            S.recip((r, r.ap[:, 0:1]), (pb, pb.ap[:, off + 64:off + 65]))
                            hq = 4 * ui + j
                            S.ts("dve", (o_tok, o_tok.ap[:, st, 512 + 64 * hq:512 + 64 * hq + 64]), (pb, pb.ap[:, off:off + 64]),
                                 (r, r.ap[:, 0:1]), None, ALU.mult)
            for st in range(4):
                for c in range(8):
                    S.transpose((psT, psT.ap[:, c * 128:(c + 1) * 128]), (o_tok, o_tok.ap[:, st, c * 128:(c + 1) * 128]), self.ident_b)
                S.copy("act", (oTb, oTb.ap[:, :, st * 128:(st + 1) * 128]), (psT, psT.ap.rearrange("p (c q) -> p c q", c=8)))
            c0, n, v = cfg.blocks[bi]
            for dc in range(8):
                ps = PS[nxt("op")]
                S.mm(ps, [((Wo, Wo.ap[:, oc, dc * 128:(dc + 1) * 128]), (oTb, oTb.ap[:, oc, :])) for oc in range(8)])
                gT_, gA = self.sc(("gp", l, 0, 0))
                xt = self.xT[dc][bi]
                S.stt("dve", xt, ps, (gT_, gA[:, dc:dc + 1]), xt, ALU.mult, ALU.add)
        self.dbg_out("z10", self.xall, self.xall.ap, [128, 8, cfg.NCOL])
        S.barrier()
        W.release(m0)
        self.layernorm(l, 0, self.hT2b, skip_ctx=True)


BF = ml_dtypes.bfloat16
D = 1024
NCTX = 256
Q0_PERM = [0, 3, 1, 4, 2, 5, 6, 9, 7, 10, 8, 11]
QD_PERM = [0, 4, 1, 5, 2, 6, 3, 7]


def rope_tables(S):
    t = np.arange(S)
    row = (t // 64).astype(np.float32)
    col = (t % 64).astype(np.float32)
    nf = 16
    inv = (np.float32(10000.0) ** (-np.arange(nf, dtype=np.float32) / nf)).astype(np.float32)
    ar = row[:, None] * inv[None, :]
    ac = col[:, None] * inv[None, :]
    ang = np.concatenate([ar, ar, ac, ac], axis=-1).astype(np.float32)
    return np.cos(ang).astype(np.float32), np.sin(ang).astype(np.float32)


def consts(cfg):
    S = cfg.S
    c = {}
    Rm = np.zeros((128, 128), np.float32)
    for h in range(2):
        o = 64 * h
        for m in range(64):
            if m < 16:
                Rm[o + m + 16, o + m] = -1
            elif m < 32:
                Rm[o + m - 16, o + m] = 1
            elif m < 48:
                Rm[o + m + 16, o + m] = -1
            else:
                Rm[o + m - 16, o + m] = 1
    c["Rm"] = Rm
    kl = np.arange(128)[:, None]
    ql = np.arange(128)[None, :]
    lo = (kl >= ql).astype(np.float32)
    hi = (kl <= ql).astype(np.float32)
    c["masks"] = np.concatenate([lo, lo, lo, hi, hi, hi], axis=1)
    cc = np.arange(64)
    m = (cc[:, None] * cc[None, :]) % 64
    C64 = np.cos(2 * np.pi * m / 64)
    S64 = np.sin(2 * np.pi * m / 64)
    Z = np.zeros((64, 64))
    Cc2 = np.block([[C64, Z], [Z, C64]])
    Sc2 = np.block([[S64, Z], [Z, S64]])
    c["CS2"] = np.concatenate([Cc2, Sc2], axis=1).astype(np.float32)
    b1 = np.zeros((128, 128), np.float32)
    b1[:64, :64] = 1
    b1[64:, 64:] = 1
    c["blk1"] = b1
    c["ident"] = np.eye(128, dtype=np.float32)
    t = np.arange(NCTX)
    mm = (t[:, None] * t[None, :]) % NCTX
    nrm = 1.0 / np.sqrt(NCTX * 64.0)
    Cc = (np.cos(2 * np.pi * mm / NCTX) * nrm)
    Sc = (-np.sin(2 * np.pi * mm / NCTX) * nrm)
    c["tabCc"] = np.ascontiguousarray(Cc.reshape(2, 128, NCTX).transpose(1, 0, 2)).astype(BF)
    c["tabSc"] = np.ascontiguousarray(Sc.reshape(2, 128, NCTX).transpose(1, 0, 2)).astype(BF)
    return c


def core_consts(cfg, s):
    S, NOWN = cfg.S, cfg.NOWN
    c = {}
    cos, sin = rope_tables(S)
    own0 = s * NOWN
    def fm(tab, pos):
        valid = (pos >= 0) & (pos < S)
        p = np.clip(pos, 0, S - 1)
        a = tab[p].T * valid[None, :]
        return np.ascontiguousarray(np.concatenate([a, a], axis=0)).astype(np.float32)
    pos_own = np.arange(own0, own0 + NOWN)
    pos_halo = np.concatenate([np.arange(own0 - 128, own0), np.arange(own0 + NOWN, own0 + NOWN + 128)])
    c["cos_own"], c["sin_own"] = fm(cos, pos_own), fm(sin, pos_own)
    c["cos_halo"], c["sin_halo"] = fm(cos, pos_halo), fm(sin, pos_halo)
    vh = np.zeros((128, 2), np.float32)
    vh[:, 0] = 1.0 if own0 - 128 >= 0 else 0.0
    vh[:, 1] = 1.0 if own0 + NOWN + 128 <= S else 0.0
    c["valid_halo"] = vh
    par0 = (1 - s) * NOWN
    pos_all = np.concatenate([pos_own, np.arange(par0, par0 + NOWN)])
    nrm = 1.0 / np.sqrt(S * 64.0)
    tabC = np.empty((cfg.NB, 128, cfg.NT_ALL, 512), BF)
    tabS = np.empty((cfg.NB, 128, cfg.NT_ALL, 512), BF)
    for b in range(cfg.NB):
        tp = own0 + b * 512 + np.arange(512)
        mm = (pos_all[:, None].astype(np.int64) * tp[None, :]) % S
        ang = (2 * np.pi / S) * mm
        Cm = (np.cos(ang) * nrm).astype(np.float32).reshape(cfg.NT_ALL, 128, 512).transpose(1, 0, 2)
        Sm = (-np.sin(ang) * nrm).astype(np.float32).reshape(cfg.NT_ALL, 128, 512).transpose(1, 0, 2)
        tabC[b] = Cm.astype(BF)
        tabS[b] = Sm.astype(BF)
    c["tabC"], c["tabS"] = tabC, tabS
    return c


def pl(vec):
    v = np.asarray(vec, np.float32)
    lead = v.shape[:-1]
    n = v.shape[-1] // 128
    v = v.reshape(*lead, n, 128)
    v = np.moveaxis(v, -1, 0)
    return np.ascontiguousarray(v.reshape(128, -1))


def prep_inputs(inp, cfg, n_batch):
    L, NE, NOWN, S = cfg.L, cfg.NE, cfg.NOWN, cfg.S
    f32 = np.float32
    x = np.asarray(inp["x"], f32)
    ctx = np.asarray(inp["ctx"], f32)
    c = np.asarray(inp["c"], f32)
    c_ctx = np.asarray(inp["c_ctx"], f32)
    shared = {}
    shared["mod_w"] = np.ascontiguousarray(np.asarray(inp["mod_w"], f32))
    shared["mod_bT"] = pl(np.asarray(inp["mod_b"], f32))
    lnT = np.zeros((128, L * 32), f32)
    for l in range(L):
        for i in range(2):
            for gb, arr in enumerate((inp["ln_g"], inp["ln_b"])):
                o = ((l * 2 + i) * 2 + gb) * 8
                lnT[:, o:o + 8] = np.asarray(arr, f32)[l, i].reshape(8, 128).T
    shared["lnT"] = lnT
    w0 = np.asarray(inp["ab_w_in"], f32)[0]
    qcols = np.concatenate([256 + 64 * h + np.arange(64) for h in Q0_PERM])
    shared["w_in0"] = np.ascontiguousarray(np.concatenate([w0[:, :256], w0[:, qcols], w0[:, 1024:]], axis=1))
    shared["w_out0"] = np.ascontiguousarray(np.asarray(inp["ab_w_out"], f32)[0])
    shared["sink"] = np.asarray(inp["ab_sink"], f32)[0].reshape(1, 12)
    w1 = np.asarray(inp["cd_w_in"], f32)[0]
    qd = np.concatenate([512 + 64 * h + np.arange(64) for h in QD_PERM])
    shared["w_in1"] = np.ascontiguousarray(np.concatenate([w1[:, :512], w1[:, qd], w1[:, 1024:]], axis=1))
    shared["w_out1"] = np.ascontiguousarray(np.asarray(inp["cd_w_out"], f32)[0])
    shared["lam"] = np.asarray(inp["cd_lambda"], f32)[0].reshape(1, 256)
    shared["subln"] = np.asarray(inp["cd_subln_g"], f32)[0].reshape(1, 128)
    qn = np.asarray(inp["cd_q_norm_g"], f32)[0]
    kn = np.asarray(inp["cd_k_norm_g"], f32)[0]
    shared["qkng"] = np.ascontiguousarray(np.stack([np.concatenate([qn, qn]), np.concatenate([kn, kn])], axis=1))
    shared["router_w"] = np.ascontiguousarray(np.asarray(inp["router_w"], f32))
    shared["router_b"] = np.asarray(inp["router_b"], f32).reshape(L, 1, NE)
    wgu_ = np.asarray(inp["expert_w_gu"], f32)
    shared["e_wgu0"], shared["e_wgu1"] = wgu_[0], wgu_[1]
    bgu = np.asarray(inp["expert_b_gu"], f32)
    shared["e_bguT"] = np.ascontiguousarray(bgu.reshape(L * NE * 16, 128).T)
    wd_ = np.asarray(inp["expert_w_down"], f32)
    shared["e_wd0"], shared["e_wd1"] = wd_[0], wd_[1]
    shared["e_bd"] = np.asarray(inp["expert_b_down"], f32)
    shared.update(consts(cfg))
    cc = [core_consts(cfg, s) for s in range(2)]
    maps = []
    for b in range(n_batch):
        for s in range(2):
            m = dict(shared)
            own0 = s * NOWN
            par0 = (1 - s) * NOWN
            m["xT_own"] = np.ascontiguousarray(x[b, own0:own0 + NOWN].T)
            m["xT_par"] = np.ascontiguousarray(x[b, par0:par0 + NOWN].T)
            halo = np.zeros((256, D), f32)
            if own0 - 128 >= 0:
                halo[:128] = x[b, own0 - 128:own0]
            if own0 + NOWN + 128 <= S:
                halo[128:] = x[b, own0 + NOWN:own0 + NOWN + 128]
            m["xT_halo"] = np.ascontiguousarray(halo.T)
            m["cT"] = np.ascontiguousarray(ctx[b].T)
            cv = np.zeros((128, 16), f32)
            cv[:, 0:8] = c[b].reshape(8, 128).T
            cv[:, 8:16] = c_ctx.reshape(8, 128).T
            m["cvec"] = cv
            m.update(cc[s])
            gi = np.zeros((128, 22), np.uint32)
            for rl in range(2):
                for ti in range(11):
                    gi[:, rl * 11 + ti] = ((2 * b + rl) * 11 + ti) * 128 + np.arange(128)
            m["gidx"] = gi
            maps.append(m)
    return maps


def glue_b(maps_a, res_a, cfg, n_batch):
    NOWN = cfg.NOWN
    maps = []
    for b in range(n_batch):
        for s in range(2):
            me, par = res_a[2 * b + s], res_a[2 * b + (1 - s)]
            m = dict(maps_a[2 * b + s])
            m["xstate"] = me["xstate"]
            m["q1"] = me["q1"]
            m["kT_all"] = np.concatenate([me["kT_own"], par["kT_own"][:, :, 256:]], axis=2)
            m["vc_all"] = np.concatenate([me["vc_own"], par["vc_own"][:, 256:]], axis=1)
            m["vd_all"] = np.concatenate([me["vd_own"], par["vd_own"][:, 256:]], axis=1)
            maps.append(m)
    return maps


def select(m, names):
    return {k: m[k] for k in names}


def kernel(**inputs):
    cfg = Cfg(S=4096, NE=32, L=2)
    inp = {k: np.asarray(v) for k, v in inputs.items()}
    nb = inp["x"].shape[0]
    ncores = 2 * nb
    maps = prep_inputs(inp, cfg, nb)
    nc = Prog(cfg, mode="F").build()
    names = list(dram_inputs(cfg, "F").keys())
    res = run_bass_kernel_spmd(nc, [select(m, names) for m in maps], core_ids=list(range(ncores)))
    NOWN = cfg.NOWN
    out = np.empty((nb, cfg.S, D), np.float32)
    for b in range(nb):
        for s in range(2):
            o = np.asarray(res.results[2 * b + s]["outT"])
            out[b, s * NOWN:(s + 1) * NOWN] = o.transpose(2, 1, 0).reshape(NOWN, D)
    return out
```

```python
import numpy as np
import ml_dtypes
from contextlib import ExitStack
import concourse.bass as bass
import concourse.mybir as mybir
from concourse.bass_utils import run_bass_kernel_spmd

F32 = mybir.dt.float32
BF16 = mybir.dt.bfloat16
ALU = mybir.AluOpType
ACTF = mybir.ActivationFunctionType
AX = mybir.AxisListType

ENGS = ["sp", "act", "dve", "pool", "pe"]
DQ = ("sp", "act", "pool")
NDSEM = 8


class T:
    __slots__ = ("ap", "w", "r", "name")

    def __init__(self, ap, name=""):
        self.ap = ap
        self.w = None
        self.r = []
        self.name = name


def _ta(x):
    if isinstance(x, T):
        return x, x.ap
    if isinstance(x, tuple):
        return x
    return None, x


def _key(ev):
    return ev[:2] if ev[0] == "e" else ev[:3]


class Sched:
    def __init__(self, nc, stack):
        self.nc = nc
        self.q = {e: [] for e in ENGS}
        self.cnt = {e: 0 for e in ENGS}
        self.sem = {e: stack.enter_context(nc.semaphore(f"s_{e}")) for e in ENGS}
        self.dsem = {e: [stack.enter_context(nc.semaphore(f"d_{e}{k}")) for k in range(NDSEM)] for e in DQ}
        self.dcnt = {e: [0] * NDSEM for e in DQ}
        self.dnext = {e: 0 for e in DQ}
        self.seen = {e: {} for e in ENGS}
        self.ninstr = 0

    def _wait(self, eng, ev):
        if ev[0] == "e":
            _, f, n = ev
            if f == eng and eng == "pe":
                return
            key, val, sem = ("e", f), n, self.sem[f]
        else:
            _, f, k, n = ev
            key, val, sem = ("d", f, k), 16 * n, self.dsem[f][k]
        if self.seen[eng].get(key, 0) >= val:
            return
        self.seen[eng][key] = val
        self.q[eng].append(lambda E, sem=sem, val=val: E.wait_ge(sem, val))
        self.ninstr += 1

    def _deps(self, eng, reads, writes):
        best = {}
        for t in reads:
            if t.w is not None:
                k = _key(t.w)
                if k not in best or best[k][-1] < t.w[-1]:
                    best[k] = t.w
        for t in writes:
            for ev in ([t.w] if t.w is not None else []) + t.r:
                k = _key(ev)
                if k not in best or best[k][-1] < ev[-1]:
                    best[k] = ev
        for ev in best.values():
            self._wait(eng, ev)

    def _commit(self, ev, reads, writes):
        for t in reads:
            t.r.append(ev)
            if len(t.r) > 16:
                best = {}
                for e2 in t.r:
                    k = _key(e2)
                    if k not in best or best[k][-1] < e2[-1]:
                        best[k] = e2
                t.r = list(best.values())
        for t in writes:
            t.w = ev
            t.r = []

    def op(self, eng, fns, reads=(), writes=()):
        if callable(fns):
            fns = [fns]
        reads = [t for t in reads if t is not None]
        writes = [t for t in writes if t is not None]
        self._deps(eng, reads, writes)
        self.cnt[eng] += 1
        n = self.cnt[eng]
        sem = self.sem[eng]
        last = len(fns) - 1
        for i, fn in enumerate(fns):
            if i == last:
                self.q[eng].append(lambda E, fn=fn, sem=sem: fn(E).then_inc(sem, 1))
            else:
                self.q[eng].append(lambda E, fn=fn: fn(E))
            self.ninstr += 1
        ev = ("e", eng, n)
        self._commit(ev, reads, writes)
        return ev

    def dma(self, eng, out, in_):
        ot, oa = _ta(out)
        it, ia = _ta(in_)
        k = self.dnext[eng]
        self.dnext[eng] = (k + 1) % NDSEM
        if self.dcnt[eng][k] > 0:
            self._wait(eng, ("d", eng, k, self.dcnt[eng][k]))
        reads = [t for t in [it] if t is not None]
        writes = [t for t in [ot] if t is not None]
        self._deps(eng, reads, writes)
        self.dcnt[eng][k] += 1
        n = self.dcnt[eng][k]
        sem = self.dsem[eng][k]
        self.q[eng].append(lambda E, oa=oa, ia=ia, sem=sem: E.dma_start(out=oa, in_=ia).then_inc(sem, 16))
        self.ninstr += 1
        ev = ("d", eng, k, n)
        self._commit(ev, reads, writes)
        return ev

    def _all_events(self):
        evs = [("e", f, self.cnt[f]) for f in ENGS if self.cnt[f] > 0]
        for f in DQ:
            for k in range(NDSEM):
                if self.dcnt[f][k] > 0:
                    evs.append(("d", f, k, self.dcnt[f][k]))
        return evs

    def barrier(self):
        evs = self._all_events()
        for e in ENGS:
            for ev in evs:
                self._wait(e, ev)

    def wait_all_on(self, eng):
        for ev in self._all_events():
            self._wait(eng, ev)

    def mm(self, out, pairs, start=True, stop=True, extra_reads=()):
        ot, oa = _ta(out)
        reads = list(extra_reads)
        fns = []
        n = len(pairs)
        for i, (l, r) in enumerate(pairs):
            lt, la = _ta(l)
            rt, ra = _ta(r)
            reads += [lt, rt]
            fns.append(lambda E, la=la, ra=ra, st=(start and i == 0), sp=(stop and i == n - 1):
                       E.matmul(oa, la, ra, start=st, stop=sp))
        return self.op("pe", fns, reads, [ot])

    def transpose(self, out, in_, ident):
        ot, oa = _ta(out)
        it, ia = _ta(in_)
        dt, da = _ta(ident)
        return self.op("pe", lambda E: E.transpose(oa, ia, da), [it, dt], [ot])

    def act(self, out, in_, func, bias=0.0, scale=1.0, accum_out=None, eng="act"):
        ot, oa = _ta(out)
        it, ia = _ta(in_)
        bt, ba = _ta(bias)
        st_, sa = _ta(scale)
        at, aa = _ta(accum_out) if accum_out is not None else (None, None)
        if aa is None:
            fn = lambda E: E.activation(out=oa, in_=ia, func=func, bias=ba, scale=sa)
        else:
            fn = lambda E: E.activation(out=oa, in_=ia, func=func, bias=ba, scale=sa, accum_out=aa)
        return self.op("act", fn, [it, bt, st_], [ot, at])

    def tt(self, eng, out, in0, in1, op):
        ot, oa = _ta(out)
        at, aa = _ta(in0)
        bt, ba = _ta(in1)
        return self.op(eng, lambda E: E.tensor_tensor(out=oa, in0=aa, in1=ba, op=op), [at, bt], [ot])

    def ts(self, eng, out, in0, s1, s2, op0, op1=None):
        ot, oa = _ta(out)
        at, aa = _ta(in0)
        t1, a1 = _ta(s1)
        t2, a2 = _ta(s2)
        if op1 is None:
            fn = lambda E: E.tensor_scalar(out=oa, in0=aa, scalar1=a1, scalar2=None, op0=op0)
        else:
            fn = lambda E: E.tensor_scalar(out=oa, in0=aa, scalar1=a1, scalar2=a2, op0=op0, op1=op1)
        return self.op(eng, fn, [at, t1, t2], [ot])

    def stt(self, eng, out, in0, scalar, in1, op0, op1):
        ot, oa = _ta(out)
        at, aa = _ta(in0)
        st_, sa = _ta(scalar)
        bt, ba = _ta(in1)
        return self.op(eng, lambda E: E.scalar_tensor_tensor(out=oa, in0=aa, scalar=sa, in1=ba, op0=op0, op1=op1),
                       [at, st_, bt], [ot])

    def copy(self, eng, out, in_):
        ot, oa = _ta(out)
        it, ia = _ta(in_)
        if eng == "act":
            return self.op("act", lambda E: E.activation(out=oa, in_=ia, func=ACTF.Copy), [it], [ot])
        return self.op(eng, lambda E: E.tensor_copy(out=oa, in_=ia), [it], [ot])

    def recip(self, out, in_):
        ot, oa = _ta(out)
        it, ia = _ta(in_)
        return self.op("dve", lambda E: E.reciprocal(out=oa, in_=ia), [it], [ot])

    def memset(self, eng, out, val):
        ot, oa = _ta(out)
        return self.op(eng, lambda E: E.memset(oa, val), [], [ot])

    def emit(self):
        nc = self.nc
        q = self.q
        with nc.Block() as block:
            @block.sync
            def _(E):
                for f in q["sp"]:
                    f(E)

            @block.scalar
            def _(E):
                for f in q["act"]:
                    f(E)

            @block.vector
            def _(E):
                for f in q["dve"]:
                    f(E)

            @block.gpsimd
            def _(E):
                for f in q["pool"]:
                    f(E)

            @block.tensor
            def _(E):
                for f in q["pe"]:
                    f(E)


class Arena:
    def __init__(self, nc, stack, name, nbytes):
        self.n = nbytes // 4
        self.t = stack.enter_context(nc.sbuf_tensor(name, [128, self.n], F32))
        self.off = 0
        self.peak = 0
        self.top = self.n

    def alloc(self, free_shape, dt, name="", top=False):
        nel = int(np.prod(free_shape))
        nw = nel if dt == F32 else (nel + 1) // 2
        if top:
            self.top = (self.top - nw) // 8 * 8
            assert self.off <= self.top, f"arena overflow (top) allocating {name}"
            ap = self.t[:, self.top:self.top + nw]
        else:
            self.off = (self.off + 7) // 8 * 8
            assert self.off + nw <= self.top, f"arena overflow allocating {name}: {self.off + nw} > {self.top}"
            ap = self.t[:, self.off:self.off + nw]
            self.off += nw
        self.peak = max(self.peak, self.off + (self.n - self.top))
        if dt != F32:
            ap = ap.bitcast(dt)
            if nel != 2 * nw:
                ap = ap[:, 0:nel]
        if len(free_shape) == 2:
            ap = ap.rearrange("p (a b) -> p a b", a=free_shape[0])
        elif len(free_shape) == 3:
            ap = ap.rearrange("p (a b c) -> p a b c", a=free_shape[0], b=free_shape[1])
        return T(ap, name)

    def mark(self):
        return self.off

    def release(self, m):
        self.off = m

    def release_top(self):
        self.top = self.n


import math

D = 1024
HD = 64
ALPHA = 4.0 ** 0.25
LN_EPS = 1e-6
RMS_EPS = 1e-6
NCTX = 256


class Cfg:
    def __init__(self, S=4096, NE=32, L=2):
        self.S, self.NE, self.L = S, NE, L
        self.NOWN = S // 2
        self.NB = self.NOWN // 512
        self.NCOL = NCTX + self.NOWN
        self.NT_OWN = self.NOWN // 128
        self.NT_ALL = S // 128
        self.blocks = [(0, NCTX, 1)] + [(NCTX + 512 * i, 512, 0) for i in range(self.NB)]


def dram_inputs(cfg, mode="A"):
    L, NE, NOWN = cfg.L, cfg.NE, cfg.NOWN
    NKO, NK = NCTX + NOWN, NCTX + 2 * NOWN
    common = {
        "cvec": ([128, 16], F32), "mod_w": ([L, D, 6 * D], F32), "mod_bT": ([128, L * 48], F32),
        "lnT": ([128, L * 2 * 2 * 8], F32),
        "router_w": ([L, D, NE], F32), "router_b": ([L, 1, NE], F32),
        "e_bguT": ([128, L * NE * 16], F32), "e_bd": ([L, NE, D], F32), "ident": ([128, 128], F32),
    }
    a_only = {
        "xT_own": ([D, NOWN], F32), "xT_par": ([D, NOWN], F32), "xT_halo": ([D, 256], F32), "cT": ([D, NCTX], F32),
        "w_in0": ([D, 1536], F32), "w_out0": ([D, D], F32), "sink": ([1, 12], F32),
        "w_in1": ([D, 2304], F32), "qkng": ([128, 2], F32),
        "e_wgu0": ([NE, D, 2 * D], F32), "e_wd0": ([NE, D, D], F32),
        "cos_own": ([128, NOWN], F32), "sin_own": ([128, NOWN], F32),
        "cos_halo": ([128, 256], F32), "sin_halo": ([128, 256], F32), "valid_halo": ([128, 2], F32),
        "Rm": ([128, 128], F32), "masks": ([128, 768], F32), "CS2": ([128, 256], F32),
        "blk1": ([128, 128], F32),
        "tabC": ([cfg.NB, 128, cfg.NT_ALL, 512], BF16), "tabS": ([cfg.NB, 128, cfg.NT_ALL, 512], BF16),
        "tabCc": ([128, 2, NCTX], BF16), "tabSc": ([128, 2, NCTX], BF16),
    }
    b_only = {
        "w_out1": ([D, D], F32), "lam": ([1, 256], F32), "subln": ([1, 128], F32),
        "e_wgu1": ([NE, D, 2 * D], F32), "e_wd1": ([NE, D, D], F32),
    }
    b_state = {
        "xstate": ([128, 8, cfg.NCOL], F32), "q1": ([8, 128, NOWN], BF16), "kT_all": ([5, 128, NK], BF16),
        "vc_all": ([4, NK, 129], BF16), "vd_all": ([2, NK, 65], BF16),
    }
    d = dict(common)
    if mode in ("A", "F"):
        d.update(a_only)
    if mode in ("B", "F"):
        d.update(b_only)
    if mode == "B":
        d.update(b_state)
    if mode == "F":
        d["gidx"] = ([128, 22], mybir.dt.uint32)
    return d


class Prog:
    def __init__(self, cfg, mode="A", stop_after=None, dbg=()):
        self.cfg = cfg
        self.mode = mode
        self.stop_after = stop_after
        self.dbg = dbg
        self.nc = bass.Bass("TRN2", target_bir_lowering=False)
        self.st = ExitStack()

    def build(self):
        cfg, nc, st, mode = self.cfg, self.nc, self.st, self.mode
        NOWN = cfg.NOWN
        NKO, NK = NCTX + NOWN, NCTX + 2 * NOWN
        with st:
            self.S = S = Sched(nc, st)
            self.din = {}
            for name, (shape, dt) in dram_inputs(cfg, mode).items():
                self.din[name] = nc.dram_tensor(name, shape, dt, kind="ExternalInput").ap()
            self.dbg_aps = {}
            self.DT = {k: T(v, k) for k, v in self.din.items()}
            pers_b = 8 * cfg.NCOL * 6 + 6144
            self.pers = Arena(nc, st, "pers", pers_b)
            self.work = Arena(nc, st, "work", (210000 - pers_b) // 32 * 32)
            self.PS = [T(st.enter_context(nc.psum_tensor(f"ps{i}", [128, 512], F32))[:], f"ps{i}") for i in range(8)]
            self.setup_persistent()
            self.phase0_mod()
            done = False
            if mode in ("A", "F"):
                self.layer0()
                done = self.stop_after is not None and self.stop_after != "cc"
            if mode == "A" and not done:
                outs = {}
                for name, shape, dt in (("xstate", [128, 8, cfg.NCOL], F32), ("q1", [8, 128, NOWN], BF16),
                                        ("kT_own", [5, 128, NKO], BF16), ("vc_own", [4, NKO, 129], BF16), ("vd_own", [2, NKO, 65], BF16)):
                    outs[name] = nc.dram_tensor(name, shape, dt, kind="ExternalOutput").ap()
                self.layer1_proj(outs["q1"], outs["kT_own"], outs["vc_own"], outs["vd_own"])
                XS = T(outs["xstate"], "xstate_o")
                for c in range(8):
                    for bi, (c0, n, v) in enumerate(cfg.blocks):
                        S.dma("sp" if c % 2 == 0 else "act", (XS, outs["xstate"][:, c, c0:c0 + n]), self.xT[c][bi])
            if mode == "F" and not done:
                NKOC = NKO // 128
                ROW = NKOC * 129
                snd_t = nc.dram_tensor("kv_snd", [11 * 128, ROW], BF16)
                rcv_t = nc.dram_tensor("kv_rcv", [8 * 11 * 128, ROW], BF16)
                q1_t = nc.dram_tensor("q1_scr", [8, 128, NOWN], BF16)
                snd, rcv, q1 = snd_t.ap(), rcv_t.ap(), q1_t.ap()
                SND, RCV = T(snd, "kv_snd"), T(rcv, "kv_rcv")
                self.Q1T = [T(q1[i], f"q1_{i}") for i in range(8)]
                kdst = [snd[i * 128:(i + 1) * 128, 0:NKO] for i in range(5)]
                vcdst = lambda r0: snd[5 * 128:9 * 128, (r0 // 128) * 129:(r0 // 128 + 1) * 129].rearrange("(h p) d -> p h d", p=128)
                vddst = lambda r0: snd[9 * 128:11 * 128, (r0 // 128) * 65:(r0 // 128 + 1) * 65].rearrange("(h p) d -> p h d", p=128)
                self.layer1_proj(q1, kdst, None, None, vcdst=vcdst, vddst=vddst, kvT=SND)
                gix = self.pers.alloc([22], mybir.dt.uint32, "gidx") if False else None
                gi_sb = T(st.enter_context(nc.sbuf_tensor("gidx_sb", [128, 22], mybir.dt.uint32))[:], "gidx_sb")
                S.dma("sp", gi_sb, self.DT["gidx"])
                cc_sem = st.enter_context(nc.semaphore("cc_sem"))
                S.barrier()
                CCSTEP = 9
                if CCSTEP >= 1:
                    S.q["pool"].append(lambda E: E.collective_compute("AllGather", ALU.bypass, replica_groups=[list(range(8))],
                                                                      ins=[snd_t.ap().opt()], outs=[rcv_t.ap().opt()]).then_inc(cc_sem))
                    S.q["pool"].append(lambda E: E.wait_ge(cc_sem, 1))

                last_g = [None]

                def gather(dst_T, dst_ap, rl, tile):
                    eng = "pool"
                    if last_g[0] is not None:
                        S._wait(eng, last_g[0])
                    k = S.dnext[eng]
                    S.dnext[eng] = (k + 1) % NDSEM
                    if S.dcnt[eng][k] > 0:
                        S._wait(eng, ("d", eng, k, S.dcnt[eng][k]))
                    S._deps(eng, [gi_sb], [dst_T])
                    S.dcnt[eng][k] += 1
                    n_ = S.dcnt[eng][k]
                    sem = S.dsem[eng][k]
                    col = rl * 11 + tile
                    S.q[eng].append(lambda E, dst_ap=dst_ap, col=col, sem=sem: E.indirect_dma_start(
                        out=dst_ap, out_offset=None, in_=rcv,
                        in_offset=bass.IndirectOffsetOnAxis(ap=gi_sb.ap[:, col:col + 1], axis=0)).then_inc(sem, 16))
                    S.ninstr += 1
                    S._commit(("d", eng, k, n_), [gi_sb], [dst_T])
                    last_g[0] = ("d", eng, k, n_)

                if self.stop_after == "cc":
                    tmpk = self.work.alloc([2, ROW], BF16, "tmpk")
                    if CCSTEP >= 2:
                        gather(tmpk, tmpk.ap[:, 0, :], 0, 0)
                        gather(tmpk, tmpk.ap[:, 1, :], 1, 5)
                    done = True
                else:
                    self.layer1_attn(q1, None, None, None, gather=gather)
            if mode == "B":
                self.layer1_attn(self.din["q1"], self.din["kT_all"], self.din["vc_all"], self.din["vd_all"])
            if mode in ("B", "F") and not done:
                self.moe(1)
            if mode in ("B", "F"):
                out_ap = nc.dram_tensor("outT", [128, 8, NOWN], F32, kind="ExternalOutput").ap()
                OUT = T(out_ap, "outT")
                for c in range(8):
                    for bi, (c0, n, v) in enumerate(cfg.blocks):
                        if v == 1:
                            continue
                        S.dma("sp" if c % 2 == 0 else "act", (OUT, out_ap[:, c, c0 - NCTX:c0 - NCTX + n]), self.xT[c][bi])
            S.wait_all_on("sp")
            S.wait_all_on("pool")
            S.wait_all_on("act")
            print("mode", mode, "instructions:", S.ninstr, "pers peak", self.pers.peak * 4, "work peak", self.work.peak * 4)
            S.emit()
        return nc

    def dbg_out(self, name, src_T, src_ap, shape, dt=F32):
        if name not in self.dbg:
            return
        ap = self.nc.dram_tensor("dbg_" + name, list(shape), dt, kind="ExternalOutput").ap()
        self.dbg_aps[name] = ap
        self.S.barrier()
        self.S.dma("sp", (T(ap), ap), (src_T, src_ap))

    def setup_persistent(self):
        cfg, S, P = self.cfg, self.S, self.pers
        DT = self.DT
        self.xT = [[None] * len(cfg.blocks) for _ in range(8)]
        xall = P.alloc([8, cfg.NCOL], F32, "xT")
        self.xall = xall
        for c in range(8):
            for bi, (c0, n, v) in enumerate(cfg.blocks):
                self.xT[c][bi] = T(xall.ap[:, c, c0:c0 + n], f"xT{c}_{bi}")
        self.hT2 = P.alloc([8, cfg.NCOL], BF16, "hbuf")
        self.hT2b = [[T(self.hT2.ap[:, c, c0:c0 + n], f"h2_{c}_{bi}") for bi, (c0, n, v) in enumerate(cfg.blocks)] for c in range(8)]
        self.ident_f = P.alloc([128], F32, "ident_f")
        self.ident_b = P.alloc([128], BF16, "ident_b")
        self.ones_f = P.alloc([128], F32, "ones_f")
        self.lnT = P.alloc([cfg.L * 32], F32, "lnT")
        self.mv = [P.alloc([cfg.L * 48], F32, f"mv{v}") for v in range(2)]
        self.scal = P.alloc([cfg.L * 2 * 2 * 3 * 8 + 32], F32, "scal")
        self._scal_off = 0
        self._scal_map = {}
        S.dma("sp", self.ident_f, DT["ident"])
        S.copy("dve", self.ident_b, self.ident_f)
        S.memset("dve", self.ones_f, 1.0)
        S.dma("sp", self.lnT, DT["lnT"])
        for bi, (c0, n, v) in enumerate(cfg.blocks):
            if self.mode == "B":
                for c in range(8):
                    S.dma("sp" if c % 2 == 0 else "act", self.xT[c][bi], (DT["xstate"], self.din["xstate"][:, c, c0:c0 + n]))
                continue
            src = self.din["cT"] if v == 1 else self.din["xT_own"][:, c0 - NCTX:c0 - NCTX + n]
            srcT = DT["cT"] if v == 1 else DT["xT_own"]
            for c in range(8):
                S.dma("sp", self.xT[c][bi], (srcT, src[c * 128:(c + 1) * 128, :]))

    def sc(self, key):
        if key not in self._scal_map:
            self._scal_map[key] = self._scal_off
            self._scal_off += 8
        o = self._scal_map[key]
        return (self.scal, self.scal.ap[:, o:o + 8])

    def lnp(self, l, i, gb):
        o = ((l * 2 + i) * 2 + gb) * 8
        return (self.lnT, self.lnT.ap[:, o:o + 8])

    def mcol(self, v, l, j):
        o = l * 48 + j * 8
        return (self.mv[v], self.mv[v].ap[:, o:o + 8])

    def phase0_mod(self):
        cfg, S, W, DT = self.cfg, self.S, self.work, self.DT
        m0 = W.mark()
        cv = W.alloc([16], F32, "cv")
        sT = W.alloc([16], F32, "sT")
        mbT = W.alloc([cfg.L * 48], F32, "mbT")
        mw = [W.alloc([8, 512], F32, f"mw{i}") for i in range(2)]
        S.dma("sp", cv, DT["cvec"])
        S.dma("sp", mbT, DT["mod_bT"])
        S.act(sT, cv, ACTF.Silu)
        sview = sT.ap.rearrange("p (v k) -> p k v", v=2)
        mps = self.PS[0]
        it = 0
        for l in range(cfg.L):
            for cb in range(12):
                buf = mw[it % 2]
                it += 1
                for kc in range(8):
                    S.dma("sp" if kc % 2 == 0 else "act", (buf, buf.ap[:, kc, :]),
                          (DT["mod_w"], self.din["mod_w"][l, kc * 128:(kc + 1) * 128, cb * 512:(cb + 1) * 512]))
                for sub in range(4):
                    ch = l * 48 + cb * 4 + sub
                    S.mm((mps, mps.ap[:, ch * 2:ch * 2 + 2]),
                         [((buf, buf.ap[:, kc, sub * 128:(sub + 1) * 128]), (sT, sview[:, kc, :])) for kc in range(8)])
        nch = cfg.L * 48
        for v in range(2):
            S.tt("dve", self.mv[v], (mps, mps.ap[:, 0:2 * nch].rearrange("p (c v) -> p c v", v=2)[:, :, v]), mbT, ALU.add)
        for v in range(2):
            S.ts("dve", self.sc(("A0s", v)), self.mcol(v, 0, 1), 1.0, None, ALU.add)
            S.copy("dve", self.sc(("A0b", v)), self.mcol(v, 0, 0))
            for l in range(cfg.L):
                for i in range(2):
                    S.ts("dve", self.sc(("gp", l, i, v)), self.mcol(v, l, 2 + 3 * i), 1.0 / ALPHA, None, ALU.mult)
                    if i == 0:
                        nsc, nsh = self.mcol(v, l, 4), self.mcol(v, l, 3)
                    elif l + 1 < cfg.L:
                        nsc, nsh = self.mcol(v, l + 1, 1), self.mcol(v, l + 1, 0)
                    else:
                        continue
                    tmp = self.sc(("tmp", v))
                    S.ts("dve", tmp, nsc, 1.0, None, ALU.add)
                    S.tt("dve", self.sc(("hG", l, i, v)), self.lnp(l, i, 0), tmp, ALU.mult)
                    S.tt("dve", self.sc(("hB", l, i, v)), self.lnp(l, i, 1), tmp, ALU.mult)
                    S.tt("dve", self.sc(("hB", l, i, v)), self.sc(("hB", l, i, v)), nsh, ALU.add)
        self.dbg_out("mv0", self.mv[0], self.mv[0].ap, [128, cfg.L * 48])
        self.dbg_out("mv1", self.mv[1], self.mv[1].ap, [128, cfg.L * 48])
        S.barrier()
        W.release(m0)

    def load_w_bf16(self, dst, src_name, src_ap, ncols, nk=8):
        S = self.S
        for kc in range(nk):
            S.dma("pool", (dst, dst.ap[:, kc, :]), (self.DT[src_name], src_ap[kc * 128:(kc + 1) * 128, :]))

    def layer0(self):
        cfg, S, W, DT, PS = self.cfg, self.S, self.work, self.DT, self.PS
        NOWN, NT_OWN, NT_ALL = cfg.NOWN, cfg.NT_OWN, cfg.NT_ALL
        m_layer = W.mark()
        hflat = self.hT2.ap.rearrange("p c n -> p (c n)")
        AcAs = [T(hflat[:, t * 512:(t + 1) * 512], f"AcAs{t}") for t in range(NT_ALL)]
        AcAs_c = [T(hflat[:, (NT_ALL + t) * 512:(NT_ALL + t + 1) * 512], f"AcAsc{t}") for t in range(2)]
        qd0 = self.nc.dram_tensor("qd0", [6, 128, NOWN], BF16, kind="Internal").ap()
        qd0c = self.nc.dram_tensor("qd0c", [6, 128, NCTX], BF16, kind="Internal").ap()
        QD0 = [T(qd0[i], f"qd0_{i}") for i in range(6)]
        QD0c = [T(qd0c[i], f"qd0c_{i}") for i in range(6)]
        m_attn = W.mark()
        KT = [W.alloc([NOWN], BF16, f"KT{i}") for i in range(2)]
        KTh = [W.alloc([256], BF16, f"KTh{i}") for i in range(2)]
        KTc = [W.alloc([NCTX], BF16, f"KTc{i}") for i in range(2)]
        Vo = [W.alloc([4, 65], BF16, f"Vo{t}") for t in range(NT_OWN)]
        Vh = [W.alloc([4, 65], BF16, f"Vh{t}") for t in range(2)]
        Vc = [W.alloc([4, 65], BF16, f"Vc{t}") for t in range(2)]
        m_proj = W.mark()
        Win = W.alloc([8, 1536], BF16, "Win")
        csb = [W.alloc([2, 512], F32, f"csb{i}") for i in range(2)]
        cosH = W.alloc([256], F32, "cosH")
        sinH = W.alloc([256], F32, "sinH")
        vh = W.alloc([2], F32, "vh")
        Rm = W.alloc([128], BF16, "Rm")
        CS2 = W.alloc([256], BF16, "CS2")
        xs = [W.alloc([512], F32, f"xs{i}") for i in range(3)]
        hT = [W.alloc([8, 512], BF16, "hT0")]
        qs = [W.alloc([512], BF16, "qs0")]
        qst = [W.alloc([512], BF16, f"qst{i}") for i in range(3)]
        t1 = [W.alloc([512], F32, f"t1{i}") for i in range(2)]
        t2 = [W.alloc([512], F32, f"t2{i}") for i in range(2)]
        aTb = [W.alloc([2, 512], BF16, f"aTb{i}") for i in range(2)]
        self.load_w_bf16(Win, "w_in0", self.din["w_in0"], 1536)
        S.dma("sp", cosH, DT["cos_halo"]); S.dma("sp", sinH, DT["sin_halo"])
        S.dma("sp", vh, DT["valid_halo"])
        S.dma("pool", Rm, DT["Rm"]); S.dma("pool", CS2, DT["CS2"])

        tblocks = [("ctx", 0, NCTX)] + [("own", i, 512) for i in range(cfg.NB)] + [("halo", 0, 256)] + \
                  [("par", i, 512) for i in range(cfg.NB)]
        cnt = {"h": 0, "q": 0, "a": 0, "a2": 0, "pp": 0, "pr": 0, "pv": 0, "pa": 0, "xs": 0, "cs": 0, "qst": 0}

        def nxt(k, m=2):
            v = cnt[k] % m
            cnt[k] += 1
            return v

        for (kind, bi, n) in tblocks:
            v = 1 if kind == "ctx" else 0
            hb = hT[0]
            if kind == "own":
                cb_ = csb[nxt("cs")]
                S.dma("sp", (cb_, cb_.ap[:, 0, :]), (DT["cos_own"], self.din["cos_own"][:, bi * 512:(bi + 1) * 512]))
                S.dma("act", (cb_, cb_.ap[:, 1, :]), (DT["sin_own"], self.din["sin_own"][:, bi * 512:(bi + 1) * 512]))
            for c in range(8):
                if kind == "ctx":
                    src = self.xT[c][0]
                elif kind == "own":
                    src = self.xT[c][1 + bi]
                else:
                    dn = "xT_halo" if kind == "halo" else "xT_par"
                    dap = self.din[dn][c * 128:(c + 1) * 128, (0 if kind == "halo" else bi * 512):(0 if kind == "halo" else bi * 512) + n]
                    xb_ = xs[nxt("xs", 3)]
                    S.dma("sp" if c % 2 == 0 else "act", (xb_, xb_.ap[:, 0:n]), (DT[dn], dap))
                    src = (xb_, xb_.ap[:, 0:n])
                sT_, sA = self.sc(("A0s", v))
                bT_, bA = self.sc(("A0b", v))
                S.act((hb, hb.ap[:, c, 0:n]), src, ACTF.Identity, bias=(bT_, bA[:, c:c + 1]), scale=(sT_, sA[:, c:c + 1]))

            def proj_fm(col0):
                ps = PS[nxt("pp")]
                S.mm((ps, ps.ap[:, 0:n]), [((Win, Win.ap[:, kc, col0:col0 + 128]), (hb, hb.ap[:, kc, 0:n])) for kc in range(8)])
                return ps

            def rope_to(ps, dst, cos_, sin_):
                q_ = qs[0]
                S.copy("act", (q_, q_.ap[:, 0:n]), (ps, ps.ap[:, 0:n]))
                pr = PS[2 + nxt("pr")]
                S.mm((pr, pr.ap[:, 0:n]), [(Rm, (q_, q_.ap[:, 0:n]))])
                i_ = nxt("a")
                a1, a2 = t1[i_], t2[i_]
                S.tt("pool", (a1, a1.ap[:, 0:n]), (q_, q_.ap[:, 0:n]), cos_, ALU.mult)
                S.tt("dve", (a2, a2.ap[:, 0:n]), (pr, pr.ap[:, 0:n]), sin_, ALU.mult)
                S.tt("pool", dst, (a1, a1.ap[:, 0:n]), (a2, a2.ap[:, 0:n]), ALU.add)

            if kind in ("ctx", "own"):
                for ti in range(6):
                    ps = proj_fm(256 + ti * 128)
                    qb_ = qst[nxt("qst", 3)]
                    if kind == "ctx":
                        S.copy("act", (qb_, qb_.ap[:, 0:n]), (ps, ps.ap[:, 0:n]))
                        S.dma("sp", QD0c[ti], (qb_, qb_.ap[:, 0:n]))
                    else:
                        rope_to(ps, (qb_, qb_.ap[:, 0:n]), (cb_, cb_.ap[:, 0, :]), (cb_, cb_.ap[:, 1, :]))
                        S.dma("sp", (QD0[ti], qd0[ti][:, bi * 512:bi * 512 + n]), (qb_, qb_.ap[:, 0:n]))
            if kind in ("ctx", "own", "halo"):
                for ti in range(2):
                    ps = proj_fm(1024 + ti * 128)
                    if kind == "ctx":
                        S.copy("act", KTc[ti], (ps, ps.ap[:, 0:n]))
                    elif kind == "own":
                        rope_to(ps, (KT[ti], KT[ti].ap[:, bi * 512:bi * 512 + n]), (cb_, cb_.ap[:, 0, :]), (cb_, cb_.ap[:, 1, :]))
                    else:
                        rope_to(ps, KTh[ti], cosH, sinH)
                for tt_ in range(n // 128):
                    pv = PS[4 + nxt("pv")]
                    S.mm((pv, pv.ap[:, 0:256]),
                         [((hb, hb.ap[:, kc, tt_ * 128:(tt_ + 1) * 128]), (Win, Win.ap[:, kc, 1280:1536])) for kc in range(8)])
                    if kind == "ctx":
                        vt = Vc[tt_]
                    elif kind == "own":
                        vt = Vo[bi * 4 + tt_]
                    else:
                        vt = Vh[tt_]
                    pvv = pv.ap[:, 0:256].rearrange("p (g d) -> p g d", g=4)
                    if kind == "halo":
                        S.ts("dve", (vt, vt.ap[:, :, 0:64]), (pv, pvv), (vh, vh.ap[:, tt_:tt_ + 1]), None, ALU.mult)
                        S.copy("pool", (vt, vt.ap[:, :, 64]), (vh, vh.ap[:, tt_:tt_ + 1].to_broadcast([128, 4])))
                    else:
                        S.copy("act", (vt, vt.ap[:, :, 0:64]), (pv, pvv))
                        S.memset("pool", (vt, vt.ap[:, :, 64]), 1.0)
            if kind in ("ctx", "own", "par"):
                ab = aTb[nxt("a2")]
                for cc in range(2):
                    ps = proj_fm(cc * 128)
                    S.copy("act" if cc == 0 else "dve", (ab, ab.ap[:, cc, 0:n]), (ps, ps.ap[:, 0:n]))
                for tt_ in range(n // 128):
                    pa = PS[6 + nxt("pa")]
                    for cc in range(2):
                        S.mm((pa, pa.ap[:, cc * 256:(cc + 1) * 256]), [((ab, ab.ap[:, cc, tt_ * 128:(tt_ + 1) * 128]), CS2)])
                    if kind == "ctx":
                        dst = AcAs_c[tt_]
                    elif kind == "own":
                        dst = AcAs[bi * 4 + tt_]
                    else:
                        dst = AcAs[NT_OWN + bi * 4 + tt_]
                    S.copy("act" if tt_ % 2 == 0 else "dve", dst, pa)
        self.dbg_out("KT0", KT[0], KT[0].ap, [128, NOWN], BF16)
        self.dbg_out("KTh0", KTh[0], KTh[0].ap, [128, 256], BF16)
        self.dbg_out("Vo0", Vo[0], Vo[0].ap, [128, 4, 65], BF16)
        self.dbg_out("Vh0", Vh[0], Vh[0].ap, [128, 4, 65], BF16)
        self.dbg_out("AcAs0", AcAs[0], AcAs[0].ap, [128, 512], BF16)
        if self.stop_after == "proj0":
            return
        S.barrier()
        W.release(m_proj)

        oT = W.alloc([8, cfg.NCOL], BF16, "oT", top=True)
        masks = W.alloc([768], BF16, "masks")
        esink = W.alloc([12], F32, "esink")
        o_tok = [W.alloc([768], BF16, f"otok{i}") for i in range(2)]
        PTl = [W.alloc([3, 384], BF16, f"PTl{i}") for i in range(2)]
        PTc = [W.alloc([2, 384], BF16, f"PTc{i}") for i in range(2)]
        zs = [W.alloc([4], F32, f"zs{i}") for i in range(2)]
        S.dma("pool", masks, DT["masks"])
        S.dma("sp", esink, (DT["sink"], self.din["sink"].partition_broadcast(128)))
        S.act(esink, esink, ACTF.Exp)
        psO = PS[5]
        psT = T(PS[6].ap.bitcast(BF16), "psT_bf")
        ac = {"pt": 0, "o": 0, "z": 0, "ot": 0}

        oT_att = [T(oT.ap[:, 2:8, c0:c0 + n], f"oTatt{bi}") for bi, (c0, n, v) in enumerate(cfg.blocks)]
        self.oT_att = oT_att
        def kfn(tiles, c0):
            return lambda ti: tiles[ti].ap[:, c0:c0 + 128]
        ctx_keys = [((KTc[0], KTc[1]), kfn(KTc, t * 128), Vc[t], "ctx") for t in range(2)]

        def attend_impl(Qsrc, klist, dst):
            ot = o_tok[ac["ot"] % 2]
            ac["ot"] += 1
            loc = [k for k in klist if k[3] != "ctx"]
            ctxk = [k for k in klist if k[3] == "ctx"]
            for g in range(4):
                half = g % 2
                rows = slice(half * 64, half * 64 + 64)
                ti = g // 2
                q_tiles = [(0 if g < 2 else 3) + j for j in range(3)]
                pl = PTl[ac["pt"] % 2]
                pc = PTc[ac["pt"] % 2]
                ac["pt"] += 1
                for grp, banks, ptile in ((loc, (0, 1, 2), pl), (ctxk, (3, 4), pc)):
                    for ci, (kTs, kap, vt, mt) in enumerate(grp):
                        ps = PS[banks[ci]]
                        for j in range(3):
                            qT_, qap = Qsrc[q_tiles[j]]
                            S.mm((ps, ps.ap[:, j * 128:(j + 1) * 128]), [((kTs[ti], kap(ti)[rows, :]), (qT_, qap[rows, :]))])
                        S.act((ptile, ptile.ap[:, ci, :]), (ps, ps.ap[:, 0:384]), ACTF.Exp, scale=0.125)
                        if mt == "lo":
                            S.tt("pool", (ptile, ptile.ap[:, ci, :]), (ptile, ptile.ap[:, ci, :]), (masks, masks.ap[:, 0:384]), ALU.mult)
                        elif mt == "hi":
                            S.tt("pool", (ptile, ptile.ap[:, ci, :]), (ptile, ptile.ap[:, ci, :]), (masks, masks.ap[:, 384:768]), ALU.mult)
                oo = (ac["o"] % 2) * 256
                ac["o"] += 1
                allk = [(pl, ci, k) for ci, k in enumerate(loc)] + [(pc, ci, k) for ci, k in enumerate(ctxk)]
                for j in range(3):
                    S.mm((psO, psO.ap[:, oo + j * 65: oo + (j + 1) * 65]),
                         [((pt_, pt_.ap[:, ci, j * 128:(j + 1) * 128]), (k[2], k[2].ap[:, g, :])) for (pt_, ci, k) in allk])
                z = zs[ac["z"] % 2]
                ac["z"] += 1
                ov = psO.ap[:, oo:oo + 195].rearrange("p (j d) -> p j d", j=3)
                S.tt("dve", (z, z.ap[:, 0:3]), (psO, ov[:, :, 64]), (esink, esink.ap[:, 3 * g:3 * g + 3]), ALU.add)
                S.recip((z, z.ap[:, 0:3]), (z, z.ap[:, 0:3]))
                S.tt("dve", (ot, ot.ap[:, 192 * g:192 * g + 192].rearrange("p (j d) -> p j d", j=3)), (psO, ov[:, :, 0:64]),
                     (z, z.ap[:, 0:3].unsqueeze(2).to_broadcast([128, 3, 64])), ALU.mult)
            for c in range(6):
                S.transpose((psT, psT.ap[:, c * 128:(c + 1) * 128]), (ot, ot.ap[:, c * 128:(c + 1) * 128]), self.ident_b)
            dstT, dap = dst
            S.copy("act", (dstT, dap), (psT, psT.ap[:, 0:768].rearrange("p (c q) -> p c q", c=6)))

        Qb = [W.alloc([6, 512], BF16, f"Qb{i}") for i in range(2)]
        for i in range(6):
            S.dma("sp" if i % 2 == 0 else "act", (Qb[1], Qb[1].ap[:, i, 0:NCTX]), QD0c[i])
        for t in range(2):
            attend_impl([(Qb[1], Qb[1].ap[:, i, t * 128:(t + 1) * 128]) for i in range(6)], ctx_keys,
                        (oT_att[0], oT.ap[:, 2:8, t * 128:(t + 1) * 128]))
        for n_ in range(NT_OWN):
            if n_ % 4 == 0:
                qb_ = Qb[(n_ // 4) % 2]
                for i in range(6):
                    S.dma("sp" if i % 2 == 0 else "act", (qb_, qb_.ap[:, i, :]), (QD0[i], qd0[i][:, n_ * 128:n_ * 128 + 512]))
            kl = []
            if n_ == 0:
                kl.append(((KTh[0], KTh[1]), kfn(KTh, 0), Vh[0], "lo"))
            else:
                kl.append(((KT[0], KT[1]), kfn(KT, (n_ - 1) * 128), Vo[n_ - 1], "lo"))
            kl.append(((KT[0], KT[1]), kfn(KT, n_ * 128), Vo[n_], "mid"))
            if n_ == NT_OWN - 1:
                kl.append(((KTh[0], KTh[1]), kfn(KTh, 128), Vh[1], "hi"))
            else:
                kl.append(((KT[0], KT[1]), kfn(KT, (n_ + 1) * 128), Vo[n_ + 1], "hi"))
            kl += ctx_keys
            bi = 1 + n_ // 4
            c0 = NCTX + n_ * 128
            attend_impl([(qb_, qb_.ap[:, i, (n_ % 4) * 128:(n_ % 4 + 1) * 128]) for i in range(6)], kl,
                        (oT_att[bi], oT.ap[:, 2:8, c0:c0 + 128]))
        self.dbg_out("oT_att", oT, oT.ap[:, 2:8, :], [128, 6, cfg.NCOL], BF16)
        if self.stop_after == "attn0":
            return
        S.barrier()
        W.release(m_attn)
        NPIECE = 8
        tb = [[W.alloc([NPIECE, 512], BF16, f"tab{cs}{i}") for i in range(2)] for cs in range(2)]
        tcx = [W.alloc([2, NCTX], BF16, f"tabc{cs}") for cs in range(2)]
        S.dma("sp", tcx[0], DT["tabCc"]); S.dma("act", tcx[1], DT["tabSc"])
        oT_f = [[T(oT.ap[:, cc, c0:c0 + n], f"oTf{cc}_{bi}") for bi, (c0, n, v) in enumerate(cfg.blocks)] for cc in range(2)]
        self.oT_f = oT_f
        for cc in range(2):
            ps = PS[cc]
            S.mm((ps, ps.ap[:, 0:NCTX]),
                 [((AcAs_c[t], AcAs_c[t].ap[:, cc * 256 + cs * 128: cc * 256 + cs * 128 + 128]), (tcx[cs], tcx[cs].ap[:, t, :]))
                  for t in range(2) for cs in range(2)])
            S.copy("act", oT_f[cc][0], (ps, ps.ap[:, 0:NCTX]))
        pi = 0
        for b in range(cfg.NB):
            npieces = NT_ALL // NPIECE
            for p_ in range(npieces):
                bufs = [tb[0][pi % 2], tb[1][pi % 2]]
                pi += 1
                for cs, nm in ((0, "tabC"), (1, "tabS")):
                    S.dma("sp" if cs == 0 else "act", bufs[cs], (DT[nm], self.din[nm][b, :, p_ * NPIECE:(p_ + 1) * NPIECE, :]))
                for cc in range(2):
                    ps = PS[cc]
                    pairs = []
                    for tl in range(NPIECE):
                        t = p_ * NPIECE + tl
                        for cs in range(2):
                            pairs.append(((AcAs[t], AcAs[t].ap[:, cc * 256 + cs * 128: cc * 256 + cs * 128 + 128]),
                                          (bufs[cs], bufs[cs].ap[:, tl, :])))
                    S.mm(ps, pairs, start=(p_ == 0), stop=(p_ == npieces - 1))
            for cc in range(2):
                S.copy("act" if cc == 0 else "dve", oT_f[cc][1 + b], PS[cc])
        self.dbg_out("oT_f", oT, oT.ap[:, 0:2, :], [128, 2, cfg.NCOL], BF16)
        if self.stop_after == "fourier0":
            return
        S.barrier()
        W.release(m_attn)
        self.oT = oT
        self.outproj_ln(0, "w_out0")
        if self.stop_after == "mix0":
            return
        S.barrier()
        W.release(m_layer)
        W.release_top()
        self.moe(0)
        if self.stop_after == "moe0":
            return

    def outproj_ln(self, l, wname):
        cfg, S, W, DT, PS = self.cfg, self.S, self.work, self.DT, self.PS
        oT = self.oT
        m0 = W.mark()
        Wo = W.alloc([8, D], BF16, "Wo")
        self.load_w_bf16(Wo, wname, self.din[wname], D)
        oTall = T(oT.ap, "oTall")
        k = 0
        for bi, (c0, n, v) in enumerate(cfg.blocks):
            for dc in range(8):
                ps = PS[k % 2]
                k += 1
                S.mm((ps, ps.ap[:, 0:n]), [((Wo, Wo.ap[:, oc, dc * 128:(dc + 1) * 128]), (oTall, oT.ap[:, oc, c0:c0 + n])) for oc in range(8)])
                gT_, gA = self.sc(("gp", l, 0, v))
                xt = self.xT[dc][bi]
                S.stt("dve", xt, (ps, ps.ap[:, 0:n]), (gT_, gA[:, dc:dc + 1]), xt, ALU.mult, ALU.add)
        self.dbg_out(f"z{l}0", self.xall, self.xall.ap, [128, 8, cfg.NCOL])
        self.layernorm(l, 0, self.hT2b)
        W.release(m0)

    def layernorm(self, l, i, hdst, skip_ctx=False):
        cfg, S, W, PS = self.cfg, self.S, self.work, self.PS
        m0 = W.mark()
        zsq = [W.alloc([512], F32, f"zsq{j}") for j in range(2)]
        mean = W.alloc([512], F32, "mean")
        var = W.alloc([512], F32, "var")
        rstd = W.alloc([512], F32, "rstd")
        mr = W.alloc([512], F32, "mr")
        u = [W.alloc([512], F32, f"u{j}") for j in range(2)]
        eps = LN_EPS / (ALPHA * ALPHA)
        last = (l == cfg.L - 1 and i == 1)
        for bi, (c0, n, v) in enumerate(cfg.blocks):
            if (last or skip_ctx) and v == 1:
                continue
            s1, s2 = PS[2], PS[3]
            S.mm((s1, s1.ap[:, 0:n]), [(self.ones_f, self.xT[c][bi]) for c in range(8)])
            for c in range(8):
                zq = zsq[c % 2]
                S.act((zq, zq.ap[:, 0:n]), self.xT[c][bi], ACTF.Square)
                S.mm((s2, s2.ap[:, 0:n]), [(self.ones_f, (zq, zq.ap[:, 0:n]))], start=(c == 0), stop=(c == 7))
            S.act((mean, mean.ap[:, 0:n]), (s1, s1.ap[:, 0:n]), ACTF.Copy, scale=1.0 / D)
            S.tt("pool", (mr, mr.ap[:, 0:n]), (mean, mean.ap[:, 0:n]), (mean, mean.ap[:, 0:n]), ALU.mult)
            S.stt("dve", (var, var.ap[:, 0:n]), (s2, s2.ap[:, 0:n]), 1.0 / D, (mr, mr.ap[:, 0:n]), ALU.mult, ALU.subtract)
            S.ts("dve", (var, var.ap[:, 0:n]), (var, var.ap[:, 0:n]), eps, None, ALU.add)
            S.act((var, var.ap[:, 0:n]), (var, var.ap[:, 0:n]), ACTF.Sqrt)
            S.recip((rstd, rstd.ap[:, 0:n]), (var, var.ap[:, 0:n]))
            S.tt("pool", (mr, mr.ap[:, 0:n]), (mean, mean.ap[:, 0:n]), (rstd, rstd.ap[:, 0:n]), ALU.mult)
            for c in range(8):
                uu = u[c % 2]
                eng = "dve" if c % 2 == 0 else "pool"
                S.tt(eng, (uu, uu.ap[:, 0:n]), self.xT[c][bi], (rstd, rstd.ap[:, 0:n]), ALU.mult)
                S.tt(eng, (uu, uu.ap[:, 0:n]), (uu, uu.ap[:, 0:n]), (mr, mr.ap[:, 0:n]), ALU.subtract)
                gT_, gA = self.lnp(l, i, 0)
                bT_, bA = self.lnp(l, i, 1)
                S.act(self.xT[c][bi], (uu, uu.ap[:, 0:n]), ACTF.Identity, bias=(bT_, bA[:, c:c + 1]), scale=(gT_, gA[:, c:c + 1]))
                if hdst is not None and not last:
                    hgT, hgA = self.sc(("hG", l, i, v))
                    hbT, hbA = self.sc(("hB", l, i, v))
                    S.act(hdst[c][bi], (uu, uu.ap[:, 0:n]), ACTF.Identity, bias=(hbT, hbA[:, c:c + 1]), scale=(hgT, hgA[:, c:c + 1]))
        self.dbg_out(f"x{l}{i}", self.xall, self.xall.ap, [128, 8, cfg.NCOL])
        S.barrier()
        W.release(m0)

    def moe(self, l):
        cfg, S, W, DT, PS = self.cfg, self.S, self.work, self.DT, self.PS
        NE, NCOL = cfg.NE, cfg.NCOL
        PL = "dve" if l == 1 else "pool"
        last = (l == cfg.L - 1)
        hT2, hT2b = self.hT2, self.hT2b
        m0 = W.mark()
        rw = W.alloc([8, NE], BF16, "rw")
        rb = W.alloc([NE], F32, "rb")
        GT = W.alloc([NCOL], F32, "GT")
        bgu = W.alloc([NE * 16], F32, "bgu")
        bd = W.alloc([D], F32, "bd")
        for kc in range(8):
            S.dma("pool", (rw, rw.ap[:, kc, :]), (DT["router_w"], self.din["router_w"][l, kc * 128:(kc + 1) * 128, :]))
        S.dma("sp", rb, (DT["router_b"], self.din["router_b"][l].partition_broadcast(128)))
        S.dma("sp", bgu, (DT["e_bguT"], self.din["e_bguT"][:, l * NE * 16:(l + 1) * NE * 16]))
        S.dma("sp", (bd, bd.ap[0:NE, :]), (DT["e_bd"], self.din["e_bd"][l]))
        bsg = W.alloc([NE * 8], F32, "bsg")
        bview = bgu.ap.rearrange("p (e t j) -> p e t j", t=2, j=8)
        S.ts("dve", (bsg, bsg.ap.rearrange("p (e j) -> p e j", j=8)), (bgu, bview[:, :, 0, :]), 1.702, None, ALU.mult)
        NEWCH = (l == 0)
        if NEWCH:
            S.ts("dve", (bgu, bview[:, :, 1, :]), (bgu, bview[:, :, 1, :]), 1.0, None, ALU.add)
        Lg = [W.alloc([NE], F32, f"Lg{i}") for i in range(2)]
        Ex = [W.alloc([NE], F32, f"Ex{i}") for i in range(2)]
        Mk = [W.alloc([NE], F32, f"Mk{i}") for i in range(2)]
        t8 = [W.alloc([8], F32, f"t8{i}") for i in range(2)]
        sm = [W.alloc([4], F32, f"sm{i}") for i in range(2)]
        k = 0
        for bi, (c0, n, v) in enumerate(cfg.blocks):
            if last and v == 1:
                continue
            for tt_ in range(n // 128):
                i = k % 2
                k += 1
                pr, pt = PS[6], PS[7]
                S.mm((pr, pr.ap[:, 0:NE]), [((hT2b[kc][bi], hT2b[kc][bi].ap[:, tt_ * 128:(tt_ + 1) * 128]), (rw, rw.ap[:, kc, :])) for kc in range(8)])
                S.tt("dve", Lg[i], (pr, pr.ap[:, 0:NE]), rb, ALU.add)
                S.op("dve", lambda E, o=t8[i].ap, a=Lg[i].ap: E.max(out=o, in_=a), [Lg[i]], [t8[i]])
                S.ts("dve", Mk[i], Lg[i], (t8[i], t8[i].ap[:, 3:4]), None, ALU.is_ge)
                S.ts("dve", (sm[i], sm[i].ap[:, 0:1]), (t8[i], t8[i].ap[:, 0:1]), -1.0, None, ALU.mult)
                S.act(Ex[i], Lg[i], ACTF.Exp, bias=(sm[i], sm[i].ap[:, 0:1]))
                S.tt("dve", Ex[i], Ex[i], Mk[i], ALU.mult)
                S.op("dve", lambda E, o=sm[i].ap[:, 1:2], a=Ex[i].ap: E.reduce_sum(out=o, in_=a, axis=AX.X), [Ex[i]], [sm[i]])
                S.recip((sm[i], sm[i].ap[:, 2:3]), (sm[i], sm[i].ap[:, 1:2]))
                S.ts("dve", Ex[i], Ex[i], (sm[i], sm[i].ap[:, 2:3]), None, ALU.mult)
                S.transpose((pt, pt.ap[0:NE, 0:128]), Ex[i], self.ident_f)
                S.copy("act", (GT, GT.ap[0:NE, c0 + tt_ * 128:c0 + (tt_ + 1) * 128]), (pt, pt.ap[0:NE, 0:128]))
        self.dbg_out(f"GT{l}", GT, GT.ap[0:NE, :], [NE, NCOL])
        Wg = [[W.alloc([512], BF16, f"Wg{b}_{kc}") for kc in range(8)] for b in range(2)]
        Wu = [[W.alloc([512], BF16, f"Wu{b}_{kc}") for kc in range(8)] for b in range(2)]
        Wdn = [[W.alloc([D], BF16, f"Wd{b}_{jj}") for jj in range(4)] for b in range(2)]
        actT = [W.alloc([4, 512], BF16, f"actT{i}") for i in range(2)]
        g1 = [W.alloc([512], F32, f"g1{i}") for i in range(2)]
        u1 = [W.alloc([512], F32, f"u1{i}") for i in range(2)]
        sg = [W.alloc([512], BF16, f"sg{i}") for i in range(2)]
        gsb = [W.alloc([512], F32, f"gsb{i}") for i in range(2)]
        wgu = self.din[f"e_wgu{l}"]
        wd = self.din[f"e_wd{l}"]
        WGU, WD_ = DT[f"e_wgu{l}"], DT[f"e_wd{l}"]

        def load_half(e, hh, b):
            for kc in range(8):
                S.dma("pool", Wg[b][kc], (WGU, wgu[e, kc * 128:(kc + 1) * 128, hh * 512:(hh + 1) * 512]))
                S.dma("pool", Wu[b][kc], (WGU, wgu[e, kc * 128:(kc + 1) * 128, 1024 + hh * 512:1024 + (hh + 1) * 512]))
            for jj in range(4):
                S.dma("pool", Wdn[b][jj], (WD_, wd[e, (hh * 4 + jj) * 128:(hh * 4 + jj + 1) * 128, :]))

        halves = [(e, hh) for e in range(NE) for hh in range(2)]
        load_half(0, 0, 0)
        cq = {"q": 0, "a": 0, "y": 0, "g": 0}
        gpT, gpA = None, None
        for it, (e, hh) in enumerate(halves):
            b = it % 2
            if it + 1 < len(halves):
                load_half(halves[it + 1][0], halves[it + 1][1], (it + 1) % 2)
            for bi, (c0, n, v) in enumerate(cfg.blocks):
                if last and v == 1:
                    continue
                gpT, gpA = self.sc(("gp", l, 1, v))
                gps = PS[6]
                gs = gsb[cq["g"] % 2]
                cq["g"] += 1
                S.mm((gps, gps.ap[:, 0:n]), [((self.ident_f, self.ident_f.ap[0:NE, e:e + 1].to_broadcast([NE, 128])), (GT, GT.ap[0:NE, c0:c0 + n]))])
                S.copy("act", (gs, gs.ap[:, 0:n]), (gps, gps.ap[:, 0:n]))
                at = actT[cq["a"] % 2]
                cq["a"] += 1
                for jj in range(4):
                    j = hh * 4 + jj
                    q = cq["q"] % 2
                    cq["q"] += 1
                    pg, pu = PS[2 * q], PS[2 * q + 1]
                    S.mm((pg, pg.ap[:, 0:n]), [((Wg[b][kc], Wg[b][kc].ap[:, jj * 128:(jj + 1) * 128]), hT2b[kc][bi]) for kc in range(8)])
                    S.mm((pu, pu.ap[:, 0:n]), [((Wu[b][kc], Wu[b][kc].ap[:, jj * 128:(jj + 1) * 128]), hT2b[kc][bi]) for kc in range(8)])
                    G1, U1, SG = g1[q], u1[q], sg[q]
                    S.ts("dve", (G1, G1.ap[:, 0:n]), (pg, pg.ap[:, 0:n]), (bgu, bgu.ap[:, e * 16 + j:e * 16 + j + 1]), 7.0, ALU.add, ALU.min)
                    S.act((SG, SG.ap[:, 0:n]), (G1, G1.ap[:, 0:n]), ACTF.Sigmoid, scale=1.702)
                    if NEWCH:
                        S.ts("dve", (U1, U1.ap[:, 0:n]), (pu, pu.ap[:, 0:n]), (bgu, bgu.ap[:, e * 16 + 8 + j:e * 16 + 8 + j + 1]), 8.0, ALU.add, ALU.min)
                        S.tt("dve", (G1, G1.ap[:, 0:n]), (G1, G1.ap[:, 0:n]), (SG, SG.ap[:, 0:n]), ALU.mult)
                        S.stt("dve", (G1, G1.ap[:, 0:n]), (U1, U1.ap[:, 0:n]), -6.0, (G1, G1.ap[:, 0:n]), ALU.max, ALU.mult)
                    else:
                        S.ts("dve", (U1, U1.ap[:, 0:n]), (pu, pu.ap[:, 0:n]), (bgu, bgu.ap[:, e * 16 + 8 + j:e * 16 + 8 + j + 1]), 7.0, ALU.add, ALU.min)
                        S.ts("dve", (U1, U1.ap[:, 0:n]), (U1, U1.ap[:, 0:n]), -7.0, 1.0, ALU.max, ALU.add)
                        S.tt("dve", (G1, G1.ap[:, 0:n]), (G1, G1.ap[:, 0:n]), (SG, SG.ap[:, 0:n]), ALU.mult)
                        S.tt("dve", (G1, G1.ap[:, 0:n]), (G1, G1.ap[:, 0:n]), (U1, U1.ap[:, 0:n]), ALU.mult)
                    S.tt("dve", (at, at.ap[:, jj, 0:n]), (G1, G1.ap[:, 0:n]), (gs, gs.ap[:, 0:n]), ALU.mult)
                for dc in range(8):
                    py = PS[4 + cq["y"] % 2]
                    cq["y"] += 1
                    pairs = [((Wdn[b][jj], Wdn[b][jj].ap[:, dc * 128:(dc + 1) * 128]), (at, at.ap[:, jj, 0:n])) for jj in range(4)]
                    if it == 0:
                        pairs.append(((bd, bd.ap[0:NE, dc * 128:(dc + 1) * 128]), (GT, GT.ap[0:NE, c0:c0 + n])))
                    S.mm((py, py.ap[:, 0:n]), pairs)
                    xt = self.xT[dc][bi]
                    S.stt("dve", xt, (py, py.ap[:, 0:n]), (gpT, gpA[:, dc:dc + 1]), xt, ALU.mult, ALU.add)
        self.dbg_out(f"z{l}1", self.xall, self.xall.ap, [128, 8, NCOL])
        S.barrier()
        W.release(m0)
        self.layernorm(l, 1, None if last else hT2b)
        if not last:
            self.dbg_out(f"h1in", hT2, hT2.ap, [128, 8, NCOL], BF16)

    def layer1_proj(self, q1, kT_own, vc_own, vd_own, vcdst=None, vddst=None, kvT=None):
        cfg, S, W, DT, PS = self.cfg, self.S, self.work, self.DT, self.PS
        NOWN = cfg.NOWN
        m0 = W.mark()
        Win = W.alloc([8, 2304], BF16, "Win1")
        csb = [W.alloc([2, 512], F32, f"csb{i}") for i in range(2)]
        Rm = W.alloc([128], BF16, "Rm")
        blk1 = W.alloc([128], BF16, "blk1")
        qkng = W.alloc([2], F32, "qkng")
        qs = [W.alloc([512], BF16, f"qs{i}") for i in range(2)]
        sq = W.alloc([512], BF16, "sq")
        rs = W.alloc([512], F32, "rs")
        qn = [W.alloc([512], BF16, f"qn{i}") for i in range(2)]
        qst = [W.alloc([512], BF16, f"qst{i}") for i in range(3)]
        t1 = [W.alloc([512], F32, f"t1{i}") for i in range(2)]
        t2 = [W.alloc([512], F32, f"t2{i}") for i in range(2)]
        vcs = [W.alloc([4, 129], BF16, f"vcs{i}") for i in range(2)]
        vds = [W.alloc([2, 65], BF16, f"vds{i}") for i in range(2)]
        self.load_w_bf16(Win, "w_in1", self.din["w_in1"], 2304)
        S.dma("pool", Rm, DT["Rm"]); S.dma("pool", blk1, DT["blk1"])
        S.dma("sp", qkng, DT["qkng"])
        for i in range(2):
            S.memset("pool", (vcs[i], vcs[i].ap[:, :, 128]), 1.0)
            S.memset("pool", (vds[i], vds[i].ap[:, :, 64]), 1.0)
        Q1 = [T(q1[i], f"q1_{i}") for i in range(8)]
        if kvT is None:
            KTO = [T(kT_own[i], f"kTo_{i}") for i in range(5)]
            VCO, VDO = T(vc_own, "vco"), T(vd_own, "vdo")
            vcdst = lambda r0: vc_own[:, r0:r0 + 128, :].rearrange("h p d -> p h d")
            vddst = lambda r0: vd_own[:, r0:r0 + 128, :].rearrange("h p d -> p h d")
        else:
            KTO = [T(kT_own[i], f"kTo_{i}") for i in range(5)]
            VCO = VDO = None
        cnt = {}

        def nxt(k, m=2):
            v = cnt.get(k, 0)
            cnt[k] = v + 1
            return v % m

        for bi, (c0, n, v) in enumerate(cfg.blocks):
            own = (v == 0)
            k0 = c0
            if own:
                ob = bi - 1
                cb_ = csb[nxt("cs")]
                S.dma("sp", (cb_, cb_.ap[:, 0, :]), (DT["cos_own"], self.din["cos_own"][:, ob * 512:(ob + 1) * 512]))
                S.dma("act", (cb_, cb_.ap[:, 1, :]), (DT["sin_own"], self.din["sin_own"][:, ob * 512:(ob + 1) * 512]))

            def proj_fm(col0):
                ps = PS[nxt("pp")]
                S.mm((ps, ps.ap[:, 0:n]), [((Win, Win.ap[:, kc, col0:col0 + 128]), self.hT2b[kc][bi]) for kc in range(8)])
                return ps

            def finish(ps, dst_T, dst_ap, norm_col, rope):
                q_ = qs[nxt("q")]
                S.copy("act", (q_, q_.ap[:, 0:n]), (ps, ps.ap[:, 0:n]))
                cur = q_
                if norm_col is not None:
                    S.act((sq, sq.ap[:, 0:n]), (ps, ps.ap[:, 0:n]), ACTF.Square)
                    pn = PS[2 + nxt("pr")]
                    S.mm((pn, pn.ap[:, 0:n]), [(blk1, (sq, sq.ap[:, 0:n]))])
                    S.ts("dve", (rs, rs.ap[:, 0:n]), (pn, pn.ap[:, 0:n]), 1.0 / 64, RMS_EPS, ALU.mult, ALU.add)
                    S.act((rs, rs.ap[:, 0:n]), (rs, rs.ap[:, 0:n]), ACTF.Sqrt)
                    S.recip((rs, rs.ap[:, 0:n]), (rs, rs.ap[:, 0:n]))
                    qn_ = qn[nxt("qn")]
                    S.stt("dve", (qn_, qn_.ap[:, 0:n]), (q_, q_.ap[:, 0:n]), (qkng, qkng.ap[:, norm_col:norm_col + 1]),
                          (rs, rs.ap[:, 0:n]), ALU.mult, ALU.mult)
                    cur = qn_
                if rope:
                    pr = PS[2 + nxt("pr")]
                    S.mm((pr, pr.ap[:, 0:n]), [(Rm, (cur, cur.ap[:, 0:n]))])
                    i_ = nxt("a")
                    a1, a2 = t1[i_], t2[i_]
                    st_ = qst[nxt("qst", 3)]
                    S.tt("pool", (a1, a1.ap[:, 0:n]), (cur, cur.ap[:, 0:n]), (cb_, cb_.ap[:, 0, 0:n]), ALU.mult)
                    S.tt("dve", (a2, a2.ap[:, 0:n]), (pr, pr.ap[:, 0:n]), (cb_, cb_.ap[:, 1, 0:n]), ALU.mult)
                    S.tt("pool", (st_, st_.ap[:, 0:n]), (a1, a1.ap[:, 0:n]), (a2, a2.ap[:, 0:n]), ALU.add)
                    cur = st_
                S.dma("sp", (dst_T, dst_ap), (cur, cur.ap[:, 0:n]))

            if own:
                for ti in range(4):
                    finish(proj_fm(ti * 128), Q1[ti], q1[ti][:, ob * 512:ob * 512 + n], None, True)
                for ti in range(4):
                    finish(proj_fm(512 + ti * 128), Q1[4 + ti], q1[4 + ti][:, ob * 512:ob * 512 + n], 0, True)
            for ti in range(4):
                finish(proj_fm(1024 + ti * 128), KTO[ti], kT_own[ti][:, k0:k0 + n], None, own)
            finish(proj_fm(1536), KTO[4], kT_own[4][:, k0:k0 + n], 1, own)
            for tt_ in range(n // 128):
                hsl = [(self.hT2b[kc][bi], self.hT2b[kc][bi].ap[:, tt_ * 128:(tt_ + 1) * 128]) for kc in range(8)]
                pv, pd = PS[4 + nxt("pv")], PS[6 + nxt("pd")]
                S.mm(pv, [(hsl[kc], (Win, Win.ap[:, kc, 1664:2176])) for kc in range(8)])
                S.mm((pd, pd.ap[:, 0:128]), [(hsl[kc], (Win, Win.ap[:, kc, 2176:2304])) for kc in range(8)])
                vc_, vd_ = vcs[nxt("vc")], vds[nxt("vd")]
                S.copy("act", (vc_, vc_.ap[:, :, 0:128]), (pv, pv.ap.rearrange("p (h d) -> p h d", h=4)))
                S.copy("dve", (vd_, vd_.ap[:, :, 0:64]), (pd, pd.ap[:, 0:128].rearrange("p (h d) -> p h d", h=2)))
                r0 = k0 + tt_ * 128
                S.dma("sp", (VCO if VCO is not None else T(vcdst(r0)), vcdst(r0)), vc_)
                S.dma("act", (VDO if VDO is not None else T(vddst(r0)), vddst(r0)), vd_)
        S.barrier()
        W.release(m0)

    def layer1_attn(self, q1, kT_all, vc_all, vd_all, gather=None):
        cfg, S, W, DT, PS = self.cfg, self.S, self.work, self.DT, self.PS
        NOWN = cfg.NOWN
        NK = NCTX + 2 * NOWN
        NKC = NK // 128
        l = 1
        m0 = W.mark()
        Q1 = self.Q1T if gather is not None else [T(q1[i], f"q1r_{i}") for i in range(8)]
        if gather is None:
            KTA = [T(kT_all[i], f"kTa_{i}") for i in range(5)]
            VCA = [T(vc_all[i], f"vca_{i}") for i in range(4)]
            VDA = [T(vd_all[i], f"vda_{i}") for i in range(2)]
        NKOC = (NCTX + NOWN) // 128
        ROW = NKOC * 129
        Wo = W.alloc([8, D], BF16, "Wo1")
        self.load_w_bf16(Wo, "w_out1", self.din["w_out1"], D)
        if gather is None:
            Kt = [W.alloc([NK], BF16, f"Kt{i}") for i in range(2)]
            Vt = [W.alloc([NKC, 129], BF16, f"Vt{i}") for i in range(2)]
            chunks = list(range(NKC))
            kchunk = lambda kt, rows, kc: kt.ap[rows, kc * 128:(kc + 1) * 128]
            vchunk = lambda vt, kc, w: vt.ap[:, kc, 0:w]
        else:
            Kt = [W.alloc([2, ROW], BF16, f"Kt{i}") for i in range(2)]
            Vt = [W.alloc([2, ROW], BF16, f"Vt{i}") for i in range(2)]
            chunks = [(0, c) for c in range(NKOC)] + [(1, c) for c in range(2, NKOC)]
            kchunk = lambda kt, rows, kc: kt.ap[rows, kc[0], kc[1] * 128:(kc[1] + 1) * 128]
            vchunk = lambda vt, kc, w: vt.ap[:, kc[0], kc[1] * w:(kc[1] + 1) * w]
        Qt = [W.alloc([4, 512], BF16, f"Qt{i}") for i in range(2)]
        PT = [W.alloc([512], BF16, f"PT{i}") for i in range(4)]
        o_tok = W.alloc([4, D], BF16, "otok1")
        oTb = W.alloc([8, 512], BF16, "oTb1")
        lamt = W.alloc([256], F32, "lamt")
        lam = W.alloc([8], F32, "lam")
        sgb = W.alloc([128], F32, "sgb")
        oa = [W.alloc([128], F32, f"oa{i}") for i in range(2)]
        ob_ = [W.alloc([128], F32, f"ob{i}") for i in range(2)]
        rz = [W.alloc([8], F32, f"rz{i}") for i in range(2)]
        lam_init = 0.8 - 0.6 * math.exp(-0.3 * 1)
        S.dma("sp", lamt, (DT["lam"], self.din["lam"].partition_broadcast(128)))
        S.dma("sp", sgb, (DT["subln"], self.din["subln"].partition_broadcast(128)))
        S.tt("dve", (lamt, lamt.ap[:, 0:64]), (lamt, lamt.ap[:, 0:64]), (lamt, lamt.ap[:, 64:128]), ALU.mult)
        S.tt("dve", (lamt, lamt.ap[:, 128:192]), (lamt, lamt.ap[:, 128:192]), (lamt, lamt.ap[:, 192:256]), ALU.mult)
        S.op("dve", lambda E: E.reduce_sum(out=lam.ap[:, 0:1], in_=lamt.ap[:, 0:64], axis=AX.X), [lamt], [lam])
        S.op("dve", lambda E: E.reduce_sum(out=lam.ap[:, 1:2], in_=lamt.ap[:, 128:192], axis=AX.X), [lamt], [lam])
        S.act((lam, lam.ap[:, 0:2]), (lam, lam.ap[:, 0:2]), ACTF.Exp)
        S.tt("dve", (lam, lam.ap[:, 2:3]), (lam, lam.ap[:, 0:1]), (lam, lam.ap[:, 1:2]), ALU.subtract)
        S.ts("dve", (lam, lam.ap[:, 3:4]), (lam, lam.ap[:, 2:3]), lam_init, -1.0, ALU.add, ALU.mult)
        S.ts("dve", sgb, sgb, 1.0 - lam_init, None, ALU.mult)
        psT = T(PS[7].ap.bitcast(BF16), "psT1")
        cq = {}

        def nxt(k, m=2):
            v = cq.get(k, 0)
            cq[k] = v + 1
            return v % m

        for qb in range(cfg.NB):
            bi = 1 + qb
            units = [("C", h) for h in range(4)] + [("D", g) for g in range(2)]
            for (ut, ui) in units:
                kt, vt, qt = Kt[nxt("kt")], Vt[nxt("vt")], Qt[nxt("qt")]
                if ut == "C":
                    if gather is None:
                        S.dma("sp", kt, KTA[ui])
                        S.dma("act", vt, (VCA[ui], vc_all[ui].rearrange("(c p) d -> p c d", p=128)))
                    else:
                        for rl in range(2):
                            gather(kt, kt.ap[:, rl, :], rl, ui)
                            gather(vt, vt.ap[:, rl, :], rl, 5 + ui)
                    S.dma("sp", (qt, qt.ap[:, 0, :]), (Q1[ui], q1[ui][:, qb * 512:(qb + 1) * 512]))
                    maps = [(slice(64 * m, 64 * m + 64), 0) for m in range(2)]
                    dv = 128
                else:
                    if gather is None:
                        S.dma("sp", kt, KTA[4])
                        S.dma("act", (vt, vt.ap[:, :, 0:65]), (VDA[ui], vd_all[ui].rearrange("(c p) d -> p c d", p=128)))
                    else:
                        for rl in range(2):
                            gather(kt, kt.ap[:, rl, :], rl, 4)
                            gather(vt, vt.ap[:, rl, :], rl, 9 + ui)
                    for j in range(4):
                        S.dma("sp" if j % 2 == 0 else "act", (qt, qt.ap[:, j, :]), (Q1[4 + j], q1[4 + j][:, qb * 512:(qb + 1) * 512]))
                    maps = [(slice(64 * ui, 64 * ui + 64), j) for j in range(4)]
                    dv = 64
                nm = len(maps)
                w = dv + 1
                per_bank = 512 // w
                def acc(mi, st):
                    i = mi * 4 + st
                    return PS[4 + i // per_bank], (i % per_bank) * w
                for kci, kc in enumerate(chunks):
                    pts = []
                    for mi, (rows, qi) in enumerate(maps):
                        ps = PS[(nxt("sc", 4) if nm == 2 else mi)]
                        S.mm(ps, [((kt, kchunk(kt, rows, kc)), (qt, qt.ap[rows, qi, :]))])
                        pt = PT[nxt("pt", 4)]
                        S.act(pt, ps, ACTF.Exp, scale=0.125)
                        pts.append(pt)
                    for mi in range(nm):
                        for st in range(4):
                            pb, off = acc(mi, st)
                            S.mm((pb, pb.ap[:, off:off + w]), [((pts[mi], pts[mi].ap[:, st * 128:(st + 1) * 128]), (vt, vchunk(vt, kc, w)))],
                                 start=(kci == 0), stop=(kci == len(chunks) - 1))
                for st in range(4):
                    if ut == "C":
                        (p1, o1), (p2, o2) = acc(0, st), acc(1, st)
                        r = rz[nxt("rz")]
                        S.recip((r, r.ap[:, 0:1]), (p1, p1.ap[:, o1 + 128:o1 + 129]))
                        S.recip((r, r.ap[:, 1:2]), (p2, p2.ap[:, o2 + 128:o2 + 129]))
                        S.tt("dve", (r, r.ap[:, 1:2]), (r, r.ap[:, 1:2]), (lam, lam.ap[:, 3:4]), ALU.mult)
                        a_, b_ = oa[nxt("oa")], ob_[nxt("ob")]
                        S.ts("dve", a_, (p1, p1.ap[:, o1:o1 + 128]), (r, r.ap[:, 0:1]), None, ALU.mult)
                        S.stt("dve", a_, (p2, p2.ap[:, o2:o2 + 128]), (r, r.ap[:, 1:2]), a_, ALU.mult, ALU.add)
                        S.act(b_, a_, ACTF.Square, accum_out=(r, r.ap[:, 2:3]))
                        S.ts("dve", (r, r.ap[:, 2:3]), (r, r.ap[:, 2:3]), 1.0 / 128, RMS_EPS, ALU.mult, ALU.add)
                        S.act((r, r.ap[:, 2:3]), (r, r.ap[:, 2:3]), ACTF.Sqrt)
                        S.recip((r, r.ap[:, 2:3]), (r, r.ap[:, 2:3]))
                        S.stt("dve", (o_tok, o_tok.ap[:, st, ui * 128:(ui + 1) * 128]), a_, (r, r.ap[:, 2:3]), sgb, ALU.mult, ALU.mult)
                    else:
                        for j in range(4):
                            pb, off = acc(j, st)
                            r = rz[nxt("rz")]
                            S.recip((r, r.ap[:, 0:1]), (pb, pb.ap[:, off + 64:off + 65]))
                            hq = 4 * ui + j
                            S.ts("dve", (o_tok, o_tok.ap[:, st, 512 + 64 * hq:512 + 64 * hq + 64]), (pb, pb.ap[:, off:off + 64]),
                                 (r, r.ap[:, 0:1]), None, ALU.mult)
            for st in range(4):
                for c in range(8):
                    S.transpose((psT, psT.ap[:, c * 128:(c + 1) * 128]), (o_tok, o_tok.ap[:, st, c * 128:(c + 1) * 128]), self.ident_b)
                S.copy("act", (oTb, oTb.ap[:, :, st * 128:(st + 1) * 128]), (psT, psT.ap.rearrange("p (c q) -> p c q", c=8)))
            c0, n, v = cfg.blocks[bi]
            for dc in range(8):
                ps = PS[nxt("op")]
                S.mm(ps, [((Wo, Wo.ap[:, oc, dc * 128:(dc + 1) * 128]), (oTb, oTb.ap[:, oc, :])) for oc in range(8)])
                gT_, gA = self.sc(("gp", l, 0, 0))
                xt = self.xT[dc][bi]
                S.stt("dve", xt, ps, (gT_, gA[:, dc:dc + 1]), xt, ALU.mult, ALU.add)
        self.dbg_out("z10", self.xall, self.xall.ap, [128, 8, cfg.NCOL])
        S.barrier()
        W.release(m0)
        self.layernorm(l, 0, self.hT2b, skip_ctx=True)


BF = ml_dtypes.bfloat16
D = 1024
NCTX = 256
Q0_PERM = [0, 3, 1, 4, 2, 5, 6, 9, 7, 10, 8, 11]
QD_PERM = [0, 4, 1, 5, 2, 6, 3, 7]


def rope_tables(S):
    t = np.arange(S)
    row = (t // 64).astype(np.float32)
    col = (t % 64).astype(np.float32)
    nf = 16
    inv = (np.float32(10000.0) ** (-np.arange(nf, dtype=np.float32) / nf)).astype(np.float32)
    ar = row[:, None] * inv[None, :]
    ac = col[:, None] * inv[None, :]
    ang = np.concatenate([ar, ar, ac, ac], axis=-1).astype(np.float32)
    return np.cos(ang).astype(np.float32), np.sin(ang).astype(np.float32)


def consts(cfg):
    S = cfg.S
    c = {}
    Rm = np.zeros((128, 128), np.float32)
    for h in range(2):
        o = 64 * h
        for m in range(64):
            if m < 16:
                Rm[o + m + 16, o + m] = -1
            elif m < 32:
                Rm[o + m - 16, o + m] = 1
            elif m < 48:
                Rm[o + m + 16, o + m] = -1
            else:
                Rm[o + m - 16, o + m] = 1
    c["Rm"] = Rm
    kl = np.arange(128)[:, None]
    ql = np.arange(128)[None, :]
    lo = (kl >= ql).astype(np.float32)
    hi = (kl <= ql).astype(np.float32)
    c["masks"] = np.concatenate([lo, lo, lo, hi, hi, hi], axis=1)
    cc = np.arange(64)
    m = (cc[:, None] * cc[None, :]) % 64
    C64 = np.cos(2 * np.pi * m / 64)
    S64 = np.sin(2 * np.pi * m / 64)
    Z = np.zeros((64, 64))
    Cc2 = np.block([[C64, Z], [Z, C64]])
    Sc2 = np.block([[S64, Z], [Z, S64]])
    c["CS2"] = np.concatenate([Cc2, Sc2], axis=1).astype(np.float32)
    b1 = np.zeros((128, 128), np.float32)
    b1[:64, :64] = 1
    b1[64:, 64:] = 1
    c["blk1"] = b1
    c["ident"] = np.eye(128, dtype=np.float32)
    t = np.arange(NCTX)
    mm = (t[:, None] * t[None, :]) % NCTX
    nrm = 1.0 / np.sqrt(NCTX * 64.0)
    Cc = (np.cos(2 * np.pi * mm / NCTX) * nrm)
    Sc = (-np.sin(2 * np.pi * mm / NCTX) * nrm)
    c["tabCc"] = np.ascontiguousarray(Cc.reshape(2, 128, NCTX).transpose(1, 0, 2)).astype(BF)
    c["tabSc"] = np.ascontiguousarray(Sc.reshape(2, 128, NCTX).transpose(1, 0, 2)).astype(BF)
    return c


def core_consts(cfg, s):
    S, NOWN = cfg.S, cfg.NOWN
    c = {}
    cos, sin = rope_tables(S)
    own0 = s * NOWN
    def fm(tab, pos):
        valid = (pos >= 0) & (pos < S)
        p = np.clip(pos, 0, S - 1)
        a = tab[p].T * valid[None, :]
        return np.ascontiguousarray(np.concatenate([a, a], axis=0)).astype(np.float32)
    pos_own = np.arange(own0, own0 + NOWN)
    pos_halo = np.concatenate([np.arange(own0 - 128, own0), np.arange(own0 + NOWN, own0 + NOWN + 128)])
    c["cos_own"], c["sin_own"] = fm(cos, pos_own), fm(sin, pos_own)
    c["cos_halo"], c["sin_halo"] = fm(cos, pos_halo), fm(sin, pos_halo)
    vh = np.zeros((128, 2), np.float32)
    vh[:, 0] = 1.0 if own0 - 128 >= 0 else 0.0
    vh[:, 1] = 1.0 if own0 + NOWN + 128 <= S else 0.0
    c["valid_halo"] = vh
    par0 = (1 - s) * NOWN
    pos_all = np.concatenate([pos_own, np.arange(par0, par0 + NOWN)])
    nrm = 1.0 / np.sqrt(S * 64.0)
    tabC = np.empty((cfg.NB, 128, cfg.NT_ALL, 512), BF)
    tabS = np.empty((cfg.NB, 128, cfg.NT_ALL, 512), BF)
    for b in range(cfg.NB):
        tp = own0 + b * 512 + np.arange(512)
        mm = (pos_all[:, None].astype(np.int64) * tp[None, :]) % S
        ang = (2 * np.pi / S) * mm
        Cm = (np.cos(ang) * nrm).astype(np.float32).reshape(cfg.NT_ALL, 128, 512).transpose(1, 0, 2)
        Sm = (-np.sin(ang) * nrm).astype(np.float32).reshape(cfg.NT_ALL, 128, 512).transpose(1, 0, 2)
        tabC[b] = Cm.astype(BF)
        tabS[b] = Sm.astype(BF)
    c["tabC"], c["tabS"] = tabC, tabS
    return c


def pl(vec):
    v = np.asarray(vec, np.float32)
    lead = v.shape[:-1]
    n = v.shape[-1] // 128
    v = v.reshape(*lead, n, 128)
    v = np.moveaxis(v, -1, 0)
    return np.ascontiguousarray(v.reshape(128, -1))


def prep_inputs(inp, cfg, n_batch):
    L, NE, NOWN, S = cfg.L, cfg.NE, cfg.NOWN, cfg.S
    f32 = np.float32
    x = np.asarray(inp["x"], f32)
    ctx = np.asarray(inp["ctx"], f32)
    c = np.asarray(inp["c"], f32)
    c_ctx = np.asarray(inp["c_ctx"], f32)
    shared = {}
    shared["mod_w"] = np.ascontiguousarray(np.asarray(inp["mod_w"], f32))
    shared["mod_bT"] = pl(np.asarray(inp["mod_b"], f32))
    lnT = np.zeros((128, L * 32), f32)
    for l in range(L):
        for i in range(2):
            for gb, arr in enumerate((inp["ln_g"], inp["ln_b"])):
                o = ((l * 2 + i) * 2 + gb) * 8
                lnT[:, o:o + 8] = np.asarray(arr, f32)[l, i].reshape(8, 128).T
    shared["lnT"] = lnT
    w0 = np.asarray(inp["ab_w_in"], f32)[0]
    qcols = np.concatenate([256 + 64 * h + np.arange(64) for h in Q0_PERM])
    shared["w_in0"] = np.ascontiguousarray(np.concatenate([w0[:, :256], w0[:, qcols], w0[:, 1024:]], axis=1))
    shared["w_out0"] = np.ascontiguousarray(np.asarray(inp["ab_w_out"], f32)[0])
    shared["sink"] = np.asarray(inp["ab_sink"], f32)[0].reshape(1, 12)
    w1 = np.asarray(inp["cd_w_in"], f32)[0]
    qd = np.concatenate([512 + 64 * h + np.arange(64) for h in QD_PERM])
    shared["w_in1"] = np.ascontiguousarray(np.concatenate([w1[:, :512], w1[:, qd], w1[:, 1024:]], axis=1))
    shared["w_out1"] = np.ascontiguousarray(np.asarray(inp["cd_w_out"], f32)[0])
    shared["lam"] = np.asarray(inp["cd_lambda"], f32)[0].reshape(1, 256)
    shared["subln"] = np.asarray(inp["cd_subln_g"], f32)[0].reshape(1, 128)
    qn = np.asarray(inp["cd_q_norm_g"], f32)[0]
    kn = np.asarray(inp["cd_k_norm_g"], f32)[0]
    shared["qkng"] = np.ascontiguousarray(np.stack([np.concatenate([qn, qn]), np.concatenate([kn, kn])], axis=1))
    shared["router_w"] = np.ascontiguousarray(np.asarray(inp["router_w"], f32))
    shared["router_b"] = np.asarray(inp["router_b"], f32).reshape(L, 1, NE)
    wgu_ = np.asarray(inp["expert_w_gu"], f32)
    shared["e_wgu0"], shared["e_wgu1"] = wgu_[0], wgu_[1]
    bgu = np.asarray(inp["expert_b_gu"], f32)
    shared["e_bguT"] = np.ascontiguousarray(bgu.reshape(L * NE * 16, 128).T)
    wd_ = np.asarray(inp["expert_w_down"], f32)
    shared["e_wd0"], shared["e_wd1"] = wd_[0], wd_[1]
    shared["e_bd"] = np.asarray(inp["expert_b_down"], f32)
    shared.update(consts(cfg))
    cc = [core_consts(cfg, s) for s in range(2)]
    maps = []
    for b in range(n_batch):
        for s in range(2):
            m = dict(shared)
            own0 = s * NOWN
            par0 = (1 - s) * NOWN
            m["xT_own"] = np.ascontiguousarray(x[b, own0:own0 + NOWN].T)
            m["xT_par"] = np.ascontiguousarray(x[b, par0:par0 + NOWN].T)
            halo = np.zeros((256, D), f32)
            if own0 - 128 >= 0:
                halo[:128] = x[b, own0 - 128:own0]
            if own0 + NOWN + 128 <= S:
                halo[128:] = x[b, own0 + NOWN:own0 + NOWN + 128]
            m["xT_halo"] = np.ascontiguousarray(halo.T)
            m["cT"] = np.ascontiguousarray(ctx[b].T)
            cv = np.zeros((128, 16), f32)
            cv[:, 0:8] = c[b].reshape(8, 128).T
            cv[:, 8:16] = c_ctx.reshape(8, 128).T
            m["cvec"] = cv
            m.update(cc[s])
            gi = np.zeros((128, 22), np.uint32)
            for rl in range(2):
                for ti in range(11):
                    gi[:, rl * 11 + ti] = ((2 * b + rl) * 11 + ti) * 128 + np.arange(128)
            m["gidx"] = gi
            maps.append(m)
    return maps


def glue_b(maps_a, res_a, cfg, n_batch):
    NOWN = cfg.NOWN
    maps = []
    for b in range(n_batch):
        for s in range(2):
            me, par = res_a[2 * b + s], res_a[2 * b + (1 - s)]
            m = dict(maps_a[2 * b + s])
            m["xstate"] = me["xstate"]
            m["q1"] = me["q1"]
            m["kT_all"] = np.concatenate([me["kT_own"], par["kT_own"][:, :, 256:]], axis=2)
            m["vc_all"] = np.concatenate([me["vc_own"], par["vc_own"][:, 256:]], axis=1)
            m["vd_all"] = np.concatenate([me["vd_own"], par["vd_own"][:, 256:]], axis=1)
            maps.append(m)
    return maps


def select(m, names):
    return {k: m[k] for k in names}


def kernel(**inputs):
    cfg = Cfg(S=4096, NE=32, L=2)
    inp = {k: np.asarray(v) for k, v in inputs.items()}
    nb = inp["x"].shape[0]
    ncores = 2 * nb
    maps = prep_inputs(inp, cfg, nb)
    nc = Prog(cfg, mode="F").build()
    names = list(dram_inputs(cfg, "F").keys())
    res = run_bass_kernel_spmd(nc, [select(m, names) for m in maps], core_ids=list(range(ncores)))
    NOWN = cfg.NOWN
    out = np.empty((nb, cfg.S, D), np.float32)
    for b in range(nb):
        for s in range(2):
            o = np.asarray(res.results[2 * b + s]["outT"])
            out[b, s * NOWN:(s + 1) * NOWN] = o.transpose(2, 1, 0).reshape(NOWN, D)
    return out
```
